# Optimizing a Trainium2 kernel written in Bass

```python
import math
import jax, jax.numpy as jnp
from jax import lax
import numpy as np

D_MODEL = 2048
BATCH = 8
SEQ = 2048
DEPTH = 2
DEC_BATCH = 2
DEC_SEQ = 16384
PAST_LEN = 128

D_CONV = 3 * D_MODEL // 8
D_ATT = 3 * D_MODEL // 8
D_MEM = D_MODEL - D_CONV - D_ATT
ATT_HEAD_DIM = 64
N_ATT_HEADS = D_ATT // ATT_HEAD_DIM
N_MEM_HEADS = 4
MEM_HEAD_DIM = D_MEM // N_MEM_HEADS
N_MEM_TOKENS = 256
D_IN = 2 * D_CONV + 3 * D_ATT + D_MEM
CONV_WIDTH = 31
CONV_PAD = CONV_WIDTH // 2
DILATED_CONFIGS = ((128, 1), (512, 4), (2048, 16))
ROT_DIM = ATT_HEAD_DIM // 4
ROPE_THETA = 500000.0
D_FF = 5632
N_EXPERTS = 8
TOP_K = 2
EXPERT_FF = 7 * D_MODEL // 2
N_DENSE = (DEPTH + 1) // 2
N_MOE = DEPTH // 2
ALPHA = (2 * DEPTH) ** 0.25
BETA = (8 * DEPTH) ** -0.25
LN_EPS = 1e-5
NEG_INF = -1e30

kernel_name = 'hybrid_conv_dilated_memory_encoder'


def _layernorm(x, g, b):
    xf = x.astype(jnp.float32)
    mu = jnp.mean(xf, axis=-1, keepdims=True)
    var = jnp.mean(jnp.square(xf - mu), axis=-1, keepdims=True)
    y = (xf - mu) * lax.rsqrt(var + LN_EPS)
    return (y * g.astype(jnp.float32) + b.astype(jnp.float32)).astype(x.dtype)


def _partial_rotary(t, positions):
    half = ROT_DIM // 2
    inv_freq = jnp.power(ROPE_THETA, -jnp.arange(0, ROT_DIM, 2, dtype=jnp.float32) / ROT_DIM)
    ang = positions[:, None] * inv_freq[None, :]
    cos = jnp.cos(ang)[None, :, None, :]
    sin = jnp.sin(ang)[None, :, None, :]
    tf = t.astype(jnp.float32)
    t1 = tf[..., :half]
    t2 = tf[..., half:ROT_DIM]
    out = jnp.concatenate([t1 * cos - t2 * sin, t2 * cos + t1 * sin, tf[..., ROT_DIM:]], axis=-1)
    return out.astype(t.dtype)


def _conformer_conv(a, g, conv_w, conv_b, ln_g, ln_b):
    u = a * jax.nn.sigmoid(g)
    y = lax.conv_general_dilated(
        u, conv_w[:, None, :], window_strides=(1,), padding=((CONV_PAD, CONV_PAD),),
        dimension_numbers=('NWC', 'WIO', 'NWC'), feature_group_count=u.shape[-1])
    y = _layernorm(y + conv_b, ln_g, ln_b)
    return jax.nn.silu(y)


def _banded_attention(q, k, v, n_side):
    N, L, H, Dh = q.shape
    blk = n_side
    nb = -(-L // blk)
    Lp = nb * blk
    tail = Lp - L
    qb = jnp.pad(q, ((0, 0), (0, tail), (0, 0), (0, 0))).reshape(N, nb, blk, H, Dh)
    kb = jnp.pad(k, ((0, 0), (blk, blk + tail), (0, 0), (0, 0))).reshape(N, nb + 2, blk, H, Dh)
    vb = jnp.pad(v, ((0, 0), (blk, blk + tail), (0, 0), (0, 0))).reshape(N, nb + 2, blk, H, Dh)
    kw = jnp.concatenate([kb[:, :-2], kb[:, 1:-1], kb[:, 2:]], axis=2)
    vw = jnp.concatenate([vb[:, :-2], vb[:, 1:-1], vb[:, 2:]], axis=2)
    s = jnp.einsum('nbqhd,nbkhd->nbhqk', qb, kw, preferred_element_type=jnp.float32)
    s = s * (Dh ** -0.5)
    blocks = jnp.arange(nb)
    qpos = blocks[:, None] * blk + jnp.arange(blk)[None, :]
    kpos = (blocks[:, None] - 1) * blk + jnp.arange(3 * blk)[None, :]
    rel = kpos[:, None, :] - qpos[:, :, None]
    valid = (jnp.abs(rel) <= n_side) & (kpos[:, None, :] >= 0) & (kpos[:, None, :] < L)
    s = jnp.where(valid[None, :, None, :, :], s, NEG_INF)
    m = jnp.max(s, axis=-1, keepdims=True)
    p = jnp.exp(s - m)
    den = jnp.sum(p, axis=-1)
    num = jnp.einsum('nbhqk,nbkhd->nbqhd', p, vw.astype(jnp.float32))
    num = num.reshape(N, Lp, H, Dh)[:, :L]
    den = den.transpose(0, 1, 3, 2).reshape(N, Lp, H)[:, :L]
    m = m[..., 0].transpose(0, 1, 3, 2).reshape(N, Lp, H)[:, :L]
    return num, den, m


def _to_residue_major(t, dil):
    B, S = t.shape[:2]
    rest = t.shape[2:]
    t = t.reshape((B, S // dil, dil) + rest)
    t = jnp.moveaxis(t, 2, 1)
    return t.reshape((B * dil, S // dil) + rest)


def _from_residue_major(t, B, dil):
    L = t.shape[1]
    rest = t.shape[2:]
    t = t.reshape((B, dil, L) + rest)
    t = jnp.moveaxis(t, 1, 2)
    return t.reshape((B, L * dil) + rest)


def _dilated_attention(q, k, v):
    B = q.shape[0]
    nums, dens, maxs = [], [], []
    for window, dil in DILATED_CONFIGS:
        n_side = window // (2 * dil)
        num, den, m = _banded_attention(_to_residue_major(q, dil), _to_residue_major(k, dil),
                                        _to_residue_major(v, dil), n_side)
        nums.append(_from_residue_major(num, B, dil))
        dens.append(_from_residue_major(den, B, dil))
        maxs.append(_from_residue_major(m, B, dil))
    m_all = jnp.maximum(jnp.maximum(maxs[0], maxs[1]), maxs[2])
    scales = [jnp.exp(mg - m_all) for mg in maxs]
    numer = sum(n * w[..., None] for n, w in zip(nums, scales))
    denom = sum(d * w for d, w in zip(dens, scales))
    return (numer / denom[..., None]).astype(q.dtype)


def _memory_attention(qm, mem, w_mem_kv):
    B, S, _ = qm.shape
    M = mem.shape[1]
    kv = mem @ w_mem_kv
    km = kv[..., :D_MEM].reshape(B, M, N_MEM_HEADS, MEM_HEAD_DIM)
    vm = kv[..., D_MEM:].reshape(B, M, N_MEM_HEADS, MEM_HEAD_DIM)
    q = qm.reshape(B, S, N_MEM_HEADS, MEM_HEAD_DIM)
    s = jnp.einsum('bshd,bmhd->bhsm', q, km, preferred_element_type=jnp.float32) * (MEM_HEAD_DIM ** -0.5)
    p = jax.nn.softmax(s, axis=-1)
    o = jnp.einsum('bhsm,bmhd->bshd', p, vm.astype(jnp.float32))
    return o.reshape(B, S, D_MEM).astype(qm.dtype)


def _mixer(x, mem, w_in, conv_w, conv_b, conv_ln_g, conv_ln_b, w_mem_kv, w_out):
    B, S, _ = x.shape
    h = x @ w_in
    cuts = np.cumsum([D_CONV, D_CONV, D_ATT, D_ATT, D_ATT]).tolist()
    a, g, q, k, v, qm = jnp.split(h, cuts, axis=-1)
    conv_out = _conformer_conv(a, g, conv_w, conv_b, conv_ln_g, conv_ln_b)
    positions = jnp.arange(S, dtype=jnp.float32)
    q = _partial_rotary(q.reshape(B, S, N_ATT_HEADS, ATT_HEAD_DIM), positions)
    k = _partial_rotary(k.reshape(B, S, N_ATT_HEADS, ATT_HEAD_DIM), positions)
    v = v.reshape(B, S, N_ATT_HEADS, ATT_HEAD_DIM)
    att_out = _dilated_attention(q, k, v).reshape(B, S, D_ATT)
    mem_out = _memory_attention(qm, mem, w_mem_kv)
    return jnp.concatenate([conv_out, att_out, mem_out], axis=-1) @ w_out


def _swiglu(x, w1, w3, w2):
    return (jax.nn.silu(x @ w1) * (x @ w3)) @ w2


def _moe_swiglu(x, router, w1, w3, w2):
    B, S, D = x.shape
    t = x.reshape(B * S, D)
    logits = (t @ router).astype(jnp.float32)
    top_vals, top_idx = lax.top_k(logits, TOP_K)
    gates = jax.nn.softmax(top_vals, axis=-1)
    combine = jnp.sum(jax.nn.one_hot(top_idx, N_EXPERTS, dtype=jnp.float32) * gates[..., None], axis=1)
    combine = combine.astype(t.dtype)
    out = jnp.zeros_like(t)
    for e in range(N_EXPERTS):
        out = out + combine[:, e:e + 1] * _swiglu(t, w1[e], w3[e], w2[e])
    return out.reshape(B, S, D)


def _trunk(x, mem, w_in, conv_w, conv_b, conv_ln_g, conv_ln_b, w_mem_kv, w_out, ln1_g, ln1_b,
           ffn_w1, ffn_w3, ffn_w2, moe_router, moe_w1, moe_w3, moe_w2, ln2_g, ln2_b):
    for l in range(DEPTH):
        y = _mixer(x, mem, w_in[l], conv_w[l], conv_b[l], conv_ln_g[l], conv_ln_b[l], w_mem_kv[l], w_out[l])
        x = _layernorm(ALPHA * x + y, ln1_g[l], ln1_b[l])
        if l % 2 == 0:
            i = l // 2
            f = _swiglu(x, ffn_w1[i], ffn_w3[i], ffn_w2[i])
        else:
            i = l // 2
            f = _moe_swiglu(x, moe_router[i], moe_w1[i], moe_w3[i], moe_w2[i])
        x = _layernorm(ALPHA * x + f, ln2_g[l], ln2_b[l])
    return x


def setup_inputs(seed: int = 0) -> dict:
    key = jax.random.key(seed)
    ks = jax.random.split(key, 24)
    f32 = jnp.float32
    nrm = lambda k, shape, scale: jax.random.normal(k, shape, f32) * scale
    return {
        'x_prompt': nrm(ks[0], (BATCH, SEQ, D_MODEL), 1.0),
        'x_sample': nrm(ks[1], (DEC_BATCH, DEC_SEQ, D_MODEL), 1.0),
        'mem_prompt': nrm(ks[2], (BATCH, N_MEM_TOKENS, D_MODEL), 1.0),
        'mem_sample': nrm(ks[3], (DEC_BATCH, N_MEM_TOKENS, D_MODEL), 1.0),
        'w_in': nrm(ks[4], (DEPTH, D_MODEL, D_IN), D_MODEL ** -0.5),
        'conv_w': nrm(ks[5], (DEPTH, CONV_WIDTH, D_CONV), CONV_WIDTH ** -0.5),
        'conv_b': nrm(ks[6], (DEPTH, D_CONV), 0.02),
        'conv_ln_g': 1.0 + nrm(ks[7], (DEPTH, D_CONV), 0.02),
        'conv_ln_b': nrm(ks[8], (DEPTH, D_CONV), 0.02),
        'w_mem_kv': nrm(ks[9], (DEPTH, D_MODEL, 2 * D_MEM), D_MODEL ** -0.5),
        'w_out': nrm(ks[10], (DEPTH, D_MODEL, D_MODEL), BETA * D_MODEL ** -0.5),
        'ln1_g': 1.0 + nrm(ks[11], (DEPTH, D_MODEL), 0.02),
        'ln1_b': nrm(ks[12], (DEPTH, D_MODEL), 0.02),
        'ffn_w1': nrm(ks[13], (N_DENSE, D_MODEL, D_FF), D_MODEL ** -0.5),
        'ffn_w3': nrm(ks[14], (N_DENSE, D_MODEL, D_FF), D_MODEL ** -0.5),
        'ffn_w2': nrm(ks[15], (N_DENSE, D_FF, D_MODEL), BETA * D_FF ** -0.5),
        'moe_router': nrm(ks[16], (N_MOE, D_MODEL, N_EXPERTS), D_MODEL ** -0.5),
        'moe_w1': nrm(ks[17], (N_MOE, N_EXPERTS, D_MODEL, EXPERT_FF), D_MODEL ** -0.5),
        'moe_w3': nrm(ks[18], (N_MOE, N_EXPERTS, D_MODEL, EXPERT_FF), D_MODEL ** -0.5),
        'moe_w2': nrm(ks[19], (N_MOE, N_EXPERTS, EXPERT_FF, D_MODEL), BETA * EXPERT_FF ** -0.5),
        'ln2_g': 1.0 + nrm(ks[20], (DEPTH, D_MODEL), 0.02),
        'ln2_b': nrm(ks[21], (DEPTH, D_MODEL), 0.02),
    }


def reference(x_prompt, x_sample, mem_prompt, mem_sample, w_in, conv_w, conv_b, conv_ln_g, conv_ln_b,
              w_mem_kv, w_out, ln1_g, ln1_b, ffn_w1, ffn_w3, ffn_w2, moe_router, moe_w1, moe_w3, moe_w2,
              ln2_g, ln2_b):
    y_prompt = _trunk(x_prompt, mem_prompt, w_in, conv_w, conv_b, conv_ln_g, conv_ln_b, w_mem_kv, w_out,
                      ln1_g, ln1_b, ffn_w1, ffn_w3, ffn_w2, moe_router, moe_w1, moe_w3, moe_w2, ln2_g, ln2_b)
    y_sample = _trunk(x_sample, mem_sample, w_in, conv_w, conv_b, conv_ln_g, conv_ln_b, w_mem_kv, w_out,
                      ln1_g, ln1_b, ffn_w1, ffn_w3, ffn_w2, moe_router, moe_w1, moe_w3, moe_w2, ln2_g, ln2_b)
    return (y_prompt, y_sample)
```

```python
import numpy as np
from contextlib import ExitStack
import concourse.bass as bass
import concourse.mybir as mybir
from concourse.bass_utils import run_bass_kernel_spmd

F32 = mybir.dt.float32
BF16 = mybir.dt.bfloat16
AF = mybir.ActivationFunctionType
ALU = mybir.AluOpType

D = 2048
DEPTH = 2
NCORE = 8
D_CONV = 768
D_ATT = 768
D_MEM = 512
D_IN = 4352
D_EXT = D_IN + 2 * D_ATT
NMT = D_EXT // 128
D_FF = 5632
E_FF = 7168
NE = 8
ALPHA = (2 * DEPTH) ** 0.25
LN_EPS = 1e-5
ROPE_THETA = 500000.0
MASK_D0 = 1408
MASK_J = 2944
SW = 8192
PW = 2048


def _mult_mask():
    kk = np.arange(128)[:, None]
    j = np.arange(MASK_J)[None, :]
    o = kk - j + MASK_D0
    ao = np.abs(o)
    c = (ao <= 64).astype(np.float32) + ((o % 4 == 0) & (ao <= 256)) + ((o % 16 == 0) & (ao <= 1024))
    return c.astype(np.float32)


def _rope_tables(pos):
    half = 8
    inv = np.power(np.float32(ROPE_THETA), -np.arange(0, 16, 2, dtype=np.float32) / np.float32(16)).astype(np.float32)
    ang = pos.astype(np.float32)[None, :] * inv[:, None]
    cos = np.cos(ang).astype(np.float32)
    sin = np.sin(ang).astype(np.float32)
    W = pos.shape[0]
    C = np.ones((128, W), np.float32)
    S = np.zeros((128, W), np.float32)
    for hh in range(2):
        b = hh * 64
        C[b:b + 8] = cos
        C[b + 8:b + 16] = cos
        S[b:b + 8] = -sin
        S[b + 8:b + 16] = sin
    return np.stack([C, S], axis=0)


class Sem:
    def __init__(self, nc, name):
        self.h = nc.semaphore(name).__enter__()
        self.n = 0
        self.name = name


class Buf:
    def __init__(self, tile, sem=None):
        self.t = tile
        self.rd = []
        self.wr = []
        self.sem = sem


class KB:
    def __init__(self):
        self.nc = bass.Bass("TRN2", target_bir_lowering=False)
        nc = self.nc
        self.eng = {'pe': nc.tensor, 'act': nc.scalar, 'dve': nc.vector, 'pool': nc.gpsimd, 'sp': nc.sync}
        self.esem = {e: Sem(nc, "e_" + e) for e in ('pe', 'act', 'dve', 'pool')}
        self.dsems = [Sem(nc, f"d{i}") for i in range(72)]
        self.dfree = list(self.dsems)
        self.waited = {}
        self.pending = []
        self.uid = 0

    def sig(self, e, ins):
        s = self.esem[e]
        ins.then_inc(s.h, 1)
        s.n += 1
        return (s, s.n, e)

    def wait(self, e, tok, force=False):
        if tok is None or (tok[2] == e and not force):
            return
        key = (e, tok[0].name)
        if self.waited.get(key, 0) >= tok[1]:
            return
        self.waited[key] = tok[1]
        self.eng[e].wait_ge(tok[0].h, tok[1])

    def dma(self, q, out, in_, sem):
        ins = self.eng[q].dma_start(out=out, in_=in_)
        ins.then_inc(sem.h, 16)
        sem.n += 16
        return (sem, sem.n, None)

    def getsem(self):
        return self.dfree.pop()

    def acq_w(self, e, b):
        for t in b.rd + b.wr:
            self.wait(e, t)

    def set_w(self, b, tok, fresh=True):
        if fresh:
            b.rd = []
            b.wr = [tok]
        else:
            b.wr.append(tok)

    def acq_r(self, e, b):
        for t in b.wr:
            self.wait(e, t)

    def add_r(self, b, tok):
        b.rd.append(tok)

    def load(self, q, b, out, in_, fresh=True):
        self.acq_w(q, b)
        tok = self.dma(q, out, in_, b.sem)
        self.set_w(b, tok, fresh)
        return tok

    def store(self, q, b, out, in_):
        self.acq_r(q, b)
        tok = self.dma(q, out, in_, b.sem)
        self.add_r(b, tok)
        self.pending.append((q, tok))
        return tok

    def phase_end(self):
        for q, tok in self.pending:
            self.wait(q, tok)
        self.pending = []
        self.nc.all_engine_barrier()


class Phase:
    def __init__(self, k):
        self.k = k
        self.es = ExitStack()
        self.sems = []

    def __enter__(self):
        self.es.__enter__()
        return self

    def __exit__(self, *a):
        self.k.phase_end()
        for s in self.sems:
            self.k.dfree.append(s)
        return self.es.__exit__(*a)

    def sb(self, shape, dt, dma=False, name=None):
        k = self.k
        k.uid += 1
        t = self.es.enter_context(k.nc.sbuf_tensor(f"{name or 't'}{k.uid}", list(shape), dt))
        s = None
        if dma:
            s = k.getsem()
            self.sems.append(s)
        return Buf(t, s)

    def ps(self, shape, dt, name=None):
        k = self.k
        k.uid += 1
        t = self.es.enter_context(k.nc.psum_tensor(f"{name or 'p'}{k.uid}", list(shape), dt))
        return Buf(t)

    def ring(self, n, shape, dt, dma=False, name=None):
        return [self.sb(shape, dt, dma, name) for _ in range(n)]

    def pring(self, n, shape, dt, name=None):
        return [self.ps(shape, dt, name) for _ in range(n)]


class Seg:
    pass


def build(dbg=False, stop=None, only_p=False):
    k = KB()
    nc = k.nc
    E = k.eng

    def din(name, shape, dt=F32):
        return nc.dram_tensor(name, list(shape), dt, kind="ExternalInput").ap()

    def dscr(name, shape, dt, out=False):
        return nc.dram_tensor(name, list(shape), dt, kind=("ExternalOutput" if out else "Internal")).ap()

    xp = din("xp", [PW, D])
    xs = din("xs", [SW, D])
    memp = din("memp", [256, D])
    mems = din("mems", [256, D])
    valid_s = din("valid_s", [128, SW // 128])
    valid_p = din("valid_p", [128, PW // 128])
    cs_p = din("cs_p", [2, 128, PW])
    cs_s = din("cs_s", [2, 128, SW])
    maskd = din("maskd", [128, MASK_J])
    identd = din("identd", [128, 128])
    w_in = din("w_in_ext", [DEPTH, D, D_EXT])
    cpar = din("cpar", [DEPTH, 128, 6, 34])
    w_memkv = din("w_mem_kv", [DEPTH, D, 1024])
    w_out = din("w_out", [DEPTH, D, D])
    ln1_g = din("ln1_g", [DEPTH, D]); ln1_b = din("ln1_b", [DEPTH, D])
    ln2_g = din("ln2_g", [DEPTH, D]); ln2_b = din("ln2_b", [DEPTH, D])
    ffn_w1 = din("ffn_w1", [1, D, D_FF]); ffn_w3 = din("ffn_w3", [1, D, D_FF]); ffn_w2 = din("ffn_w2", [1, D_FF, D])
    router = din("moe_router", [1, D, NE])
    moe_w1 = din("moe_w1", [1, NE, D, E_FF]); moe_w3 = din("moe_w3", [1, NE, D, E_FF]); moe_w2 = din("moe_w2", [1, NE, E_FF, D])
    yp = nc.dram_tensor("yp", [PW, D], F32, kind="ExternalOutput").ap()
    ys = nc.dram_tensor("ys", [4096, D], F32, kind="ExternalOutput").ap()

    win_b = [dscr(f"win_b{l}", [NMT, 128, 16 * 128], BF16) for l in range(DEPTH)]
    wkv_b = [dscr(f"wkv_b{l}", [8, 128, 16 * 128], BF16) for l in range(DEPTH)]
    wout_b = [dscr(f"wout_b{l}", [D, D], BF16) for l in range(DEPTH)]
    f1_b = dscr("f1_b", [D_FF // 128, 128, 2048], BF16)
    f3_b = dscr("f3_b", [D_FF // 128, 128, 2048], BF16)
    f2_b = dscr("f2_b", [D_FF, D], BF16)
    m1_b = dscr("m1_b", [NE, E_FF // 128, 128, 2048], BF16)
    m3_b = dscr("m3_b", [NE, E_FF // 128, 128, 2048], BF16)
    m2_b = dscr("m2_b", [NE, E_FF, D], BF16)

    segs = []
    for nm, W, xin, mem, valid, cs, hr, orr, yout, yoff in (
            ("p", PW, xp, memp, valid_p, cs_p, [(0, PW), (0, PW)], [(0, PW), (0, PW)], yp, 0),
            ("s", SW, xs, mems, valid_s, cs_s, [(0, SW), (1024, 7168)], [(1024, 7168), (2048, 6144)], ys, 2048)):
        s = Seg()
        s.nm, s.W, s.x0, s.mem, s.valid, s.cs, s.hr, s.orr, s.yout, s.yoff = nm, W, xin, mem, valid, cs, hr, orr, yout, yoff
        s.u = dscr(f"u_{nm}", [D_CONV, W], F32, out=dbg)
        s.q = dscr(f"q_{nm}", [D_ATT, W], BF16, out=dbg)
        s.kk = dscr(f"k_{nm}", [D_ATT, W], BF16, out=dbg)
        s.qm = dscr(f"qm_{nm}", [D_MEM, W], BF16, out=dbg)
        s.v = dscr(f"v_{nm}", [W, 12 * 128], BF16, out=dbg)
        s.mix = dscr(f"mix_{nm}", [D, W], BF16, out=dbg)
        s.x1 = dscr(f"x1_{nm}", [W, D], F32, out=dbg)
        s.x2 = dscr(f"x2_{nm}", [W, D], F32, out=dbg)
        s.comb = dscr(f"comb_{nm}", [W, NE], F32, out=dbg)
        segs.append(s)
    if only_p:
        segs = segs[:1]

    wsem = k.getsem()

    def conv_tiled(src, dst, ncols):
        for m in range(ncols // 128):
            s_ap = src[:, m * 128:(m + 1) * 128].rearrange("(k p) c -> p k c", p=128)
            d_ap = dst[m].rearrange("p (k c) -> p k c", c=128)
            k.pending.append(('pool', k.dma('pool', d_ap, s_ap, wsem)))

    def conv_plain(src, dst, nrows):
        for r in range(0, nrows, 128):
            k.pending.append(('pool', k.dma('pool', dst[r:r + 128, :], src[r:r + 128, :], wsem)))

    for l in range(DEPTH):
        conv_tiled(w_in[l], win_b[l], D_EXT)
        conv_tiled(w_memkv[l], wkv_b[l], 1024)
        conv_plain(w_out[l], wout_b[l], D)
    conv_tiled(ffn_w1[0], f1_b, D_FF)
    conv_tiled(ffn_w3[0], f3_b, D_FF)
    conv_plain(ffn_w2[0], f2_b, D_FF)
    if stop is None or stop > 10:
        for e in range(NE):
            conv_tiled(moe_w1[0, e], m1_b[e], E_FF)
            conv_tiled(moe_w3[0, e], m3_b[e], E_FF)
            conv_plain(moe_w2[0, e], m2_b[e], E_FF)
    k.pending = [k.pending[-1]]
    k.phase_end()

    cst = ExitStack()
    ident_b = Buf(cst.enter_context(nc.sbuf_tensor("ident_b", [128, 128], BF16)), k.getsem())
    ident_f = Buf(cst.enter_context(nc.sbuf_tensor("ident_f", [128, 128], F32)), k.getsem())
    ones_b = Buf(cst.enter_context(nc.sbuf_tensor("ones_b", [128, 128], BF16)))
    ones_f = Buf(cst.enter_context(nc.sbuf_tensor("ones_f", [128, 128], F32)))
    epsb = Buf(cst.enter_context(nc.sbuf_tensor("epsb", [128, 1], F32)))
    k.load('pool', ident_b, ident_b.t[:], identd[:, :])
    k.load('sp', ident_f, ident_f.t[:], identd[:, :])
    k.set_w(ones_b, k.sig('dve', nc.vector.memset(ones_b.t[:], 1.0)))
    k.set_w(ones_f, k.sig('dve', nc.vector.memset(ones_f.t[:], 1.0)))
    k.set_w(epsb, k.sig('dve', nc.vector.memset(epsb.t[:], LN_EPS)))
    for e in ('pe', 'act', 'dve', 'pool'):
        k.acq_r(e, ident_b); k.acq_r(e, ident_f); k.acq_r(e, ones_b); k.acq_r(e, ones_f); k.acq_r(e, epsb)

    def load_xT(ph, src_rows, xb, xT, tp_ring, cnt):
        k.load('pool', xb, xb.t[:], src_rows.rearrange("(t p) d -> p t d", p=128))
        k.acq_r('pe', xb)
        k.acq_w('act', xT); k.acq_w('dve', xT)
        first = True
        tokp_last = [None]
        for kg in range(4):
            tp = tp_ring[cnt[0] % len(tp_ring)]; cnt[0] += 1
            k.acq_w('pe', tp)
            for kk in range(4):
                for t in range(4):
                    ins = nc.tensor.transpose(tp.t[:, kk, t * 128:(t + 1) * 128], xb.t[:, t, (kg * 4 + kk) * 128:(kg * 4 + kk + 1) * 128], ident_b.t[:])
            tokp_last[0] = k.sig('pe', ins)
            k.set_w(tp, tokp_last[0])
            e = 'act' if kg % 2 == 0 else 'dve'
            k.acq_r(e, tp)
            if e == 'act':
                ins = nc.scalar.activation(out=xT.t[:, kg * 4:(kg + 1) * 4, :], in_=tp.t[:], func=AF.Copy)
            else:
                ins = nc.vector.tensor_copy(out=xT.t[:, kg * 4:(kg + 1) * 4, :], in_=tp.t[:])
            tok = k.sig(e, ins)
            k.add_r(tp, tok)
            k.set_w(xT, tok, fresh=first)
            first = False
        k.add_r(xb, tokp_last[0])

    def ln_inplace(yt, gb, rs_pool, valid_ap=None):
        st, mv, rs, nb = rs_pool
        for ch in range(4):
            ins = nc.vector.bn_stats(out=st.t[:, ch, :], in_=yt[:, ch * 512:(ch + 1) * 512])
        k.wait('dve', k.sig('dve', ins), force=True)
        tok = k.sig('dve', nc.vector.bn_aggr(out=mv.t[:], in_=st.t[:].rearrange("p a b -> p (a b)")))
        k.wait('act', tok)
        tok = k.sig('act', nc.scalar.activation(out=rs.t[:], in_=mv.t[:, 1:2], func=AF.Sqrt, bias=epsb.t[:, 0:1], scale=1.0))
        k.wait('dve', tok)
        tok = k.sig('dve', nc.vector.reciprocal(out=rs.t[:], in_=rs.t[:]))
        k.wait('dve', tok, force=True)
        tok = k.sig('dve', nc.vector.tensor_scalar(out=nb.t[:], in0=mv.t[:, 0:1], scalar1=rs.t[:, 0:1], scalar2=-1.0, op0=ALU.mult, op1=ALU.mult))
        k.wait('act', tok)
        tok = k.sig('act', nc.scalar.activation(out=yt, in_=yt, func=AF.Identity, bias=nb.t[:, 0:1], scale=rs.t[:, 0:1]))
        k.wait('dve', tok)
        nc.vector.tensor_tensor(out=yt, in0=yt, in1=gb.t[:, 0, :], op=ALU.mult)
        ins = nc.vector.tensor_tensor(out=yt, in0=yt, in1=gb.t[:, 1, :], op=ALU.add)
        if valid_ap is not None:
            ins = nc.vector.tensor_scalar(out=yt, in0=yt, scalar1=valid_ap, scalar2=None, op0=ALU.mult)
        return k.sig('dve', ins)

    for l in range(DEPTH):
        if stop is not None and stop <= l * 10:
            break
        for seg in segs:
            xin = seg.x0 if l == 0 else seg.x2
            hs, he = seg.hr[l]
            os_, oe = seg.orr[l]
            W = seg.W
            with Phase(k) as ph:
                xb = ph.sb([128, 4, 2048], BF16, dma=True, name="xb")
                xT = ph.sb([128, 16, 512], BF16, name="xT")
                wv = ph.sb([128, 6, 2048], BF16, dma=True, name="wv")
                wt = ph.ring(6, [128, 2048], BF16, dma=True, name="wt")
                cst_ = ph.ring(2, [128, 2, 512], F32, dma=True, name="cs")
                vt = ph.sb([128, W // 128], F32, dma=True, name="vt")
                onesv = ph.sb([128, 6, 64], BF16, name="onesv")
                sg = ph.ring(2, [128, 512], F32, name="sg")
                uo = ph.ring(2, [128, 512], F32, dma=True, name="uo")
                t1 = ph.ring(2, [128, 512], F32, name="t1")
                t2 = ph.ring(2, [128, 512], F32, name="t2")
                qo = ph.ring(2, [128, 512], BF16, dma=True, name="qo")
                qmo = ph.ring(2, [128, 512], BF16, dma=True, name="qmo")
                vx = ph.ring(2, [128, 12, 128], BF16, dma=True, name="vx")
                tpr = ph.pring(2, [128, 4, 512], BF16, name="tp")
                mm = ph.pring(4, [128, 512], F32, name="mm")
                cnt = [0]
                mmc = [0]
                wtc = [0]
                k.set_w(onesv, k.sig('pool', nc.gpsimd.memset(onesv.t[:], 1.0)))
                k.load('sp', vt, vt.t[:], seg.valid[:, :])
                for m in range(6):
                    k.load('sp', wv, wv.t[:, m, :], win_b[l][24 + m], fresh=(m == 0))
                k.acq_r('pe', wv)
                k.acq_r('pool', vt)

                def mtile(m, xTb):
                    wb = wt[wtc[0] % 6]; wtc[0] += 1
                    k.load('sp', wb, wb.t[:], win_b[l][m])
                    pb = mm[mmc[0] % 4]; mmc[0] += 1
                    k.acq_r('pe', wb); k.acq_w('pe', pb); k.acq_r('pe', xTb)
                    for kk in range(16):
                        ins = nc.tensor.matmul(pb.t[:], lhsT=wb.t[:, kk * 128:(kk + 1) * 128], rhs=xTb.t[:, kk, :], start=(kk == 0), stop=(kk == 15))
                    tok = k.sig('pe', ins)
                    k.set_w(pb, tok); k.add_r(wb, tok)
                    return pb, tok

                nblk = (he - hs) // 512
                for b in range(nblk):
                    t0 = hs + b * 512
                    load_xT(ph, xin[t0:t0 + 512, :], xb, xT, tpr, cnt)
                    csb = cst_[b % 2]
                    k.load('sp', csb, csb.t[:], seg.cs[:, :, t0:t0 + 512].rearrange("a p t -> p a t"))
                    lasttok = None
                    for c in range(6):
                        pa, _ = mtile(c, xT)
                        pg, _ = mtile(6 + c, xT)
                        sgb = sg[c % 2]; uob = uo[c % 2]
                        k.acq_r('act', pg); k.acq_w('act', sgb)
                        tok = k.sig('act', nc.scalar.activation(out=sgb.t[:], in_=pg.t[:], func=AF.Sigmoid))
                        k.add_r(pg, tok); k.set_w(sgb, tok)
                        k.acq_r('dve', pa); k.acq_r('dve', sgb); k.acq_w('dve', uob)
                        tok = k.sig('dve', nc.vector.tensor_tensor(out=uob.t[:], in0=pa.t[:], in1=sgb.t[:], op=ALU.mult))
                        k.add_r(pa, tok); k.add_r(sgb, tok); k.set_w(uob, tok)
                        k.store('pool', uob, seg.u[c * 128:(c + 1) * 128, t0:t0 + 512], uob.t[:])
                    k.acq_r('dve', csb)
                    for which, base, pbase, dst in (("q", 12, 34, seg.q), ("k", 18, 40, seg.kk)):
                        for hp in range(6):
                            pq, _ = mtile(base + hp, xT)
                            pp, _ = mtile(pbase + hp, xT)
                            i2 = hp % 2
                            k.acq_r('dve', pq); k.acq_w('dve', t1[i2])
                            tok = k.sig('dve', nc.vector.tensor_tensor(out=t1[i2].t[:], in0=pq.t[:], in1=csb.t[:, 0, :], op=ALU.mult))
                            k.add_r(pq, tok); k.set_w(t1[i2], tok)
                            k.acq_r('dve', pp); k.acq_w('dve', t2[i2])
                            tok = k.sig('dve', nc.vector.tensor_tensor(out=t2[i2].t[:], in0=pp.t[:], in1=csb.t[:, 1, :], op=ALU.mult))
                            k.add_r(pp, tok); k.set_w(t2[i2], tok); k.add_r(csb, tok)
                            k.acq_r('pool', t1[i2]); k.acq_r('pool', t2[i2]); k.acq_w('pool', qo[i2])
                            tok = k.sig('pool', nc.gpsimd.tensor_tensor(out=qo[i2].t[:], in0=t1[i2].t[:], in1=t2[i2].t[:], op=ALU.add))
                            k.add_r(t1[i2], tok); k.add_r(t2[i2], tok); k.set_w(qo[i2], tok)
                            k.store('pool', qo[i2], dst[hp * 128:(hp + 1) * 128, t0:t0 + 512], qo[i2].t[:])
                    for mh in range(4):
                        pq, _ = mtile(30 + mh, xT)
                        ob = qmo[mh % 2]
                        k.acq_r('act', pq); k.acq_w('act', ob)
                        tok = k.sig('act', nc.scalar.activation(out=ob.t[:], in_=pq.t[:], func=AF.Copy))
                        k.add_r(pq, tok); k.set_w(ob, tok)
                        k.store('pool', ob, seg.qm[mh * 128:(mh + 1) * 128, t0:t0 + 512], ob.t[:])
                    for t in range(4):
                        vb = vx[t % 2]
                        tile_idx = (t0 // 128) + t
                        k.acq_w('pool', vb)
                        v5 = vb.t[:].rearrange("p (a two) d -> p a two d", two=2)
                        nc.gpsimd.tensor_scalar(out=v5[:, :, 0, 64:128], in0=onesv.t[:], scalar1=vt.t[:, tile_idx:tile_idx + 1], scalar2=None, op0=ALU.mult)
                        tok = k.sig('pool', nc.gpsimd.tensor_scalar(out=v5[:, :, 1, 0:64], in0=onesv.t[:], scalar1=vt.t[:, tile_idx:tile_idx + 1], scalar2=None, op0=ALU.mult))
                        k.set_w(vb, tok)
                        for (c0, ncol, h0, nh) in ((0, 512, 0, 8), (512, 256, 8, 4)):
                            pb = mm[mmc[0] % 4]; mmc[0] += 1
                            k.acq_w('pe', pb); k.acq_r('pe', xT)
                            for kk in range(16):
                                ins = nc.tensor.matmul(pb.t[:, 0:ncol], lhsT=xT.t[:, kk, t * 128:(t + 1) * 128],
                                                       rhs=wv.t[:, c0 // 128:(c0 + ncol) // 128, kk * 128:(kk + 1) * 128],
                                                       start=(kk == 0), stop=(kk == 15))
                            tokp = k.sig('pe', ins)
                            lasttok = tokp
                            k.set_w(pb, tokp)
                            p4 = pb.t[:, 0:ncol].rearrange("p (a two d) -> p a two d", two=2, d=64)
                            d4 = vb.t[:, h0:h0 + nh, :].rearrange("p (a two) d -> p a two d", two=2)
                            k.acq_r('act', pb); k.acq_w('act', vb)
                            tok = k.sig('act', nc.scalar.activation(out=d4[:, :, 0, 0:64], in_=p4[:, :, 0, :], func=AF.Copy))
                            k.add_r(pb, tok); k.set_w(vb, tok, fresh=False)
                            k.acq_r('dve', pb); k.acq_w('dve', vb)
                            tok = k.sig('dve', nc.vector.tensor_copy(out=d4[:, :, 1, 64:128], in_=p4[:, :, 1, :]))
                            k.add_r(pb, tok); k.set_w(vb, tok, fresh=False)
                        k.store('pool', vb, seg.v[t0 + t * 128:t0 + (t + 1) * 128, :], vb.t[:].rearrange("p h d -> p (h d)"))
                    k.add_r(xT, lasttok)
            if stop is not None and stop <= l * 10 + 1:
                continue
            with Phase(k) as ph:
                cw = ph.sb([128, 6, 34], F32, dma=True, name="cw")
                ut = ph.ring(2, [128, 6, 544], F32, dma=True, name="ut")
                acc = ph.ring(2, [128, 6, 512], F32, name="acc")
                ysq = ph.ring(2, [128, 512], F32, name="ysq")
                mean = ph.sb([128, 512], F32, name="mean")
                msq = ph.sb([128, 512], F32, name="msq")
                rstd = ph.sb([128, 512], F32, name="rstd")
                zt = ph.ring(2, [128, 512], F32, name="zt")
                co = ph.ring(2, [128, 512], BF16, dma=True, name="co")
                sps = ph.pring(2, [128, 512], F32, name="sps")
                k.load('sp', cw, cw.t[:], cpar[l])
                k.acq_r('dve', cw); k.acq_r('act', cw)
                nblk = (oe - os_) // 512
                for b in range(nblk):
                    t0 = os_ + b * 512
                    ub = ut[b % 2]; ab = acc[b % 2]
                    lo = max(hs, t0 - 16); hi = min(he, t0 + 528)
                    fresh = True
                    if lo > t0 - 16 or hi < t0 + 528:
                        k.acq_w('pool', ub)
                        k.set_w(ub, k.sig('pool', nc.gpsimd.memset(ub.t[:], 0.0)))
                        fresh = False
                    for c in range(6):
                        k.load('sp', ub, ub.t[:, c, lo - (t0 - 16):hi - (t0 - 16)], seg.u[c * 128:(c + 1) * 128, lo:hi], fresh=(fresh and c == 0))
                    k.acq_r('dve', ub); k.acq_w('dve', ab)
                    k.acq_w('pe', sps[0]); k.acq_w('pe', sps[1])
                    for c in range(6):
                        nc.vector.tensor_scalar(out=ab.t[:, c, :], in0=ub.t[:, c, 1:513], scalar1=cw.t[:, c, 0:1], scalar2=cw.t[:, c, 31:32], op0=ALU.mult, op1=ALU.add)
                        for j in range(1, 31):
                            ins = nc.vector.scalar_tensor_tensor(out=ab.t[:, c, :], in0=ub.t[:, c, j + 1:j + 513], scalar=cw.t[:, c, j:j + 1], in1=ab.t[:, c, :], op0=ALU.mult, op1=ALU.add)
                        tok = k.sig('dve', ins)
                        k.set_w(ab, tok, fresh=(c == 0))
                        yb = ysq[c % 2]
                        k.wait('act', tok); k.acq_w('act', yb)
                        toka = k.sig('act', nc.scalar.activation(out=yb.t[:], in_=ab.t[:, c, :], func=AF.Square))
                        k.set_w(yb, toka)
                        k.wait('pe', tok)
                        nc.tensor.matmul(sps[0].t[:], lhsT=ones_f.t[:], rhs=ab.t[:, c, :], start=(c == 0), stop=(c == 5))
                        k.wait('pe', toka)
                        tokp = k.sig('pe', nc.tensor.matmul(sps[1].t[:], lhsT=ones_f.t[:], rhs=yb.t[:], start=(c == 0), stop=(c == 5)))
                        k.add_r(yb, tokp)
                    k.add_r(ub, tok)
                    k.set_w(sps[0], tokp); k.set_w(sps[1], tokp)
                    k.add_r(ab, tokp)
                    k.wait('act', tokp); k.acq_w('act', mean)
                    tokm = k.sig('act', nc.scalar.activation(out=mean.t[:], in_=sps[0].t[:], func=AF.Copy, scale=1.0 / D_CONV))
                    k.set_w(mean, tokm); k.add_r(sps[0], tokm)
                    k.wait('dve', tokm); k.wait('dve', tokp)
                    nc.vector.tensor_tensor(out=msq.t[:], in0=mean.t[:], in1=mean.t[:], op=ALU.mult)
                    tok = k.sig('dve', nc.vector.scalar_tensor_tensor(out=msq.t[:], in0=sps[1].t[:], scalar=1.0 / D_CONV, in1=msq.t[:], op0=ALU.mult, op1=ALU.subtract))
                    k.add_r(sps[1], tok)
                    k.wait('act', tok)
                    tok = k.sig('act', nc.scalar.activation(out=rstd.t[:], in_=msq.t[:], func=AF.Sqrt, bias=epsb.t[:, 0:1], scale=1.0))
                    k.wait('dve', tok)
                    nc.vector.reciprocal(out=rstd.t[:], in_=rstd.t[:])
                    for c in range(6):
                        zb = zt[c % 2]; cb = co[c % 2]
                        k.acq_w('dve', zb)
                        nc.vector.tensor_tensor(out=zb.t[:], in0=ab.t[:, c, :], in1=mean.t[:], op=ALU.subtract)
                        tok = k.sig('dve', nc.vector.tensor_tensor(out=zb.t[:], in0=zb.t[:], in1=rstd.t[:], op=ALU.mult))
                        k.set_w(zb, tok)
                        k.acq_r('act', zb); k.acq_w('act', cb)
                        toka = k.sig('act', nc.scalar.activation(out=cb.t[:], in_=zb.t[:], func=AF.Silu, bias=cw.t[:, c, 33:34], scale=cw.t[:, c, 32:33]))
                        k.add_r(zb, toka); k.set_w(cb, toka)
                        k.store('pool', cb, seg.mix[c * 128:(c + 1) * 128, t0:t0 + 512], cb.t[:])
                    k.add_r(ab, tok); k.add_r(mean, tok)
            if stop is not None and stop <= l * 10 + 2:
                continue
            with Phase(k) as ph:
                mk = ph.sb([128, MASK_J], BF16, dma=True, name="mk")
                qt = ph.ring(2, [128, 512], BF16, dma=True, name="qt")
                kt = ph.ring(2, [128, 2560], BF16, dma=True, name="kt")
                vxl = ph.ring(2, [128, 20, 256], BF16, dma=True, name="vxl")
                pe_t = ph.ring(3, [128, 512], BF16, name="pexp")
                pm_t = ph.ring(3, [128, 512], BF16, name="pmsk")
                rd = ph.ring(2, [128, 512], F32, name="rd")
                rd2 = ph.ring(2, [128, 512], F32, name="rd2")
                att = ph.ring(2, [128, 512], BF16, dma=True, name="att")
                sp_ = ph.pring(3, [128, 512], F32, name="sps")
                ap_ = ph.pring(2, [128, 512], F32, name="aps")
                k.load('pool', mk, mk.t[:], maskd[:, :])
                k.acq_r('dve', mk)
                it = 0; sc = 0; ac = 0
                nqb = (oe - os_) // 512
                for qb in range(nqb):
                    q0 = os_ + qb * 512
                    k0 = max(hs, q0 - 1024); k1 = min(he, q0 + 512 + 1024)
                    nkb = (k1 - k0) // 128
                    for hp in range(6):
                        qb_ = qt[it % 2]; kb_ = kt[it % 2]; vb_ = vxl[it % 2]; ab_ = att[it % 2]; it += 1
                        k.load('sp', qb_, qb_.t[:], seg.q[hp * 128:(hp + 1) * 128, q0:q0 + 512])
                        k.load('sp', kb_, kb_.t[:, 0:k1 - k0], seg.kk[hp * 128:(hp + 1) * 128, k0:k1])
                        k.load('sp', vb_, vb_.t[:, 0:nkb, :], seg.v[k0:k1, hp * 256:(hp + 1) * 256].rearrange("(kb p) c -> p kb c", p=128))
                        k.acq_r('pe', qb_); k.acq_r('pe', kb_); k.acq_r('pe', vb_)
                        for hh in range(2):
                            r0 = hh * 64
                            accb = ap_[ac % 2]; ac += 1
                            k.acq_w('pe', accb)
                            pend = []
                            for kbi in range(nkb):
                                d = (k0 + kbi * 128) - q0
                                j0 = MASK_D0 - d
                                sb_ = sp_[sc % 3]; peb = pe_t[sc % 3]; pmb = pm_t[sc % 3]; sc += 1
                                k.acq_w('pe', sb_)
                                tok = k.sig('pe', nc.tensor.matmul(sb_.t[:], lhsT=kb_.t[r0:r0 + 64, kbi * 128:(kbi + 1) * 128], rhs=qb_.t[r0:r0 + 64, :], start=True, stop=True))
                                k.set_w(sb_, tok)
                                k.acq_r('act', sb_); k.acq_w('act', peb)
                                tok = k.sig('act', nc.scalar.activation(out=peb.t[:], in_=sb_.t[:], func=AF.Exp, scale=0.125))
                                k.add_r(sb_, tok); k.set_w(peb, tok)
                                k.acq_r('dve', peb); k.acq_w('dve', pmb)
                                tok = k.sig('dve', nc.vector.tensor_tensor(out=pmb.t[:], in0=peb.t[:], in1=mk.t[:, j0:j0 + 512], op=ALU.mult))
                                k.add_r(peb, tok); k.set_w(pmb, tok)
                                pend.append((pmb, kbi))
                                if len(pend) > 1:
                                    pb2, kb2 = pend.pop(0)
                                    k.acq_r('pe', pb2)
                                    tok = k.sig('pe', nc.tensor.matmul(accb.t[:], lhsT=vb_.t[:, kb2, hh * 128:(hh + 1) * 128], rhs=pb2.t[:], start=(kb2 == 0), stop=False))
                                    k.add_r(pb2, tok)
                            pb2, kb2 = pend.pop(0)
                            k.acq_r('pe', pb2)
                            tok = k.sig('pe', nc.tensor.matmul(accb.t[:], lhsT=vb_.t[:, kb2, hh * 128:(hh + 1) * 128], rhs=pb2.t[:], start=(kb2 == 0), stop=True))
                            k.add_r(pb2, tok); k.set_w(accb, tok)
                            nr = r0; dr = 64 - r0
                            rdb = rd[hh]; rd2b = rd2[hh]
                            k.acq_r('dve', accb); k.acq_w('dve', rdb)
                            tok = k.sig('dve', nc.vector.reciprocal(out=rdb.t[dr:dr + 64, :], in_=accb.t[dr:dr + 64, :]))
                            k.set_w(rdb, tok)
                            k.acq_r('act', rdb); k.acq_w('act', rd2b)
                            tok = k.sig('act', nc.scalar.activation(out=rd2b.t[nr:nr + 64, :], in_=rdb.t[dr:dr + 64, :], func=AF.Copy))
                            k.add_r(rdb, tok); k.set_w(rd2b, tok)
                            k.acq_r('dve', rd2b)
                            if hh == 0:
                                k.acq_w('dve', ab_)
                            tok = k.sig('dve', nc.vector.tensor_tensor(out=ab_.t[nr:nr + 64, :], in0=accb.t[nr:nr + 64, :], in1=rd2b.t[nr:nr + 64, :], op=ALU.mult))
                            k.add_r(accb, tok); k.add_r(rd2b, tok); k.set_w(ab_, tok, fresh=(hh == 0))
                        k.add_r(qb_, tok); k.add_r(kb_, tok); k.add_r(vb_, tok)
                        k.store('pool', ab_, seg.mix[(6 + hp) * 128:(7 + hp) * 128, q0:q0 + 512], ab_.t[:])
            if stop is not None and stop <= l * 10 + 3:
                continue
            with Phase(k) as ph:
                mb = ph.sb([128, 2, 2048], BF16, dma=True, name="mb")
                memT = ph.sb([128, 16, 256], BF16, name="memT")
                wkv = ph.sb([128, 8, 2048], BF16, dma=True, name="wkv")
                kmT = ph.sb([128, 4, 256], BF16, name="kmT")
                vm = ph.sb([128, 2, 512], BF16, name="vm")
                qmt = ph.ring(2, [128, 4, 512], BF16, dma=True, name="qmt")
                pe_t = ph.ring(3, [128, 512], BF16, name="pexp")
                rd = ph.ring(2, [128, 512], F32, name="rd")
                mo = ph.ring(2, [128, 512], BF16, dma=True, name="mo")
                tpm = ph.pring(2, [128, 4, 256], BF16, name="tpm")
                sp_ = ph.pring(2, [128, 512], F32, name="sps")
                np_ = ph.pring(2, [128, 512], F32, name="nps")
                dp_ = ph.pring(2, [128, 512], F32, name="dps")
                k.load('pool', mb, mb.t[:], seg.mem.rearrange("(t p) d -> p t d", p=128))
                for m in range(8):
                    k.load('sp', wkv, wkv.t[:, m, :], wkv_b[l][m], fresh=(m == 0))
                k.acq_r('pe', mb); k.acq_r('pe', wkv)
                for kg in range(4):
                    tp = tpm[kg % 2]
                    k.acq_w('pe', tp)
                    for kk in range(4):
                        for t in range(2):
                            ins = nc.tensor.transpose(tp.t[:, kk, t * 128:(t + 1) * 128], mb.t[:, t, (kg * 4 + kk) * 128:(kg * 4 + kk + 1) * 128], ident_b.t[:])
                    k.set_w(tp, k.sig('pe', ins))
                    k.acq_r('act', tp)
                    tok = k.sig('act', nc.scalar.activation(out=memT.t[:, kg * 4:(kg + 1) * 4, :], in_=tp.t[:], func=AF.Copy))
                    k.add_r(tp, tok); k.set_w(memT, tok, fresh=(kg == 0))
                k.acq_r('pe', memT)
                for mh in range(4):
                    pb = sp_[mh % 2]
                    k.acq_w('pe', pb)
                    for kk in range(16):
                        ins = nc.tensor.matmul(pb.t[:, 0:256], lhsT=wkv.t[:, mh, kk * 128:(kk + 1) * 128], rhs=memT.t[:, kk, :], start=(kk == 0), stop=(kk == 15))
                    k.set_w(pb, k.sig('pe', ins))
                    k.acq_r('act', pb)
                    tok = k.sig('act', nc.scalar.activation(out=kmT.t[:, mh, :], in_=pb.t[:, 0:256], func=AF.Copy))
                    k.add_r(pb, tok); k.set_w(kmT, tok, fresh=(mh == 0))
                i = 0
                for t in range(2):
                    for mh in range(4):
                        pb = np_[i % 2]; i += 1
                        k.acq_w('pe', pb)
                        for kk in range(16):
                            ins = nc.tensor.matmul(pb.t[:, 0:128], lhsT=memT.t[:, kk, t * 128:(t + 1) * 128], rhs=wkv.t[:, 4 + mh, kk * 128:(kk + 1) * 128], start=(kk == 0), stop=(kk == 15))
                        k.set_w(pb, k.sig('pe', ins))
                        k.acq_r('dve', pb)
                        tok = k.sig('dve', nc.vector.tensor_copy(out=vm.t[:, t, mh * 128:(mh + 1) * 128], in_=pb.t[:, 0:128]))
                        k.add_r(pb, tok); k.set_w(vm, tok, fresh=(i == 1))
                k.acq_r('pe', kmT); k.acq_r('pe', vm)
                nqb = (oe - os_) // 512
                sc = 0; it = 0
                for qb in range(nqb):
                    q0 = os_ + qb * 512
                    qb_ = qmt[qb % 2]
                    k.load('sp', qb_, qb_.t[:], seg.qm[:, q0:q0 + 512].rearrange("(h p) t -> p h t", p=128))
                    k.acq_r('pe', qb_)
                    for mh in range(4):
                        nb_ = np_[it % 2]; db_ = dp_[it % 2]; rdb = rd[it % 2]; ob = mo[it % 2]; it += 1
                        k.acq_w('pe', nb_); k.acq_w('pe', db_)
                        for t in range(2):
                            sb_ = sp_[sc % 2]; peb = pe_t[sc % 3]; sc += 1
                            k.acq_w('pe', sb_)
                            tok = k.sig('pe', nc.tensor.matmul(sb_.t[:], lhsT=kmT.t[:, mh, t * 128:(t + 1) * 128], rhs=qb_.t[:, mh, :], start=True, stop=True))
                            k.set_w(sb_, tok)
                            k.acq_r('act', sb_); k.acq_w('act', peb)
                            tok = k.sig('act', nc.scalar.activation(out=peb.t[:], in_=sb_.t[:], func=AF.Exp, scale=float(128 ** -0.5)))
                            k.add_r(sb_, tok); k.set_w(peb, tok)
                            k.acq_r('pe', peb)
                            nc.tensor.matmul(nb_.t[:], lhsT=vm.t[:, t, mh * 128:(mh + 1) * 128], rhs=peb.t[:], start=(t == 0), stop=(t == 1))
                            tok = k.sig('pe', nc.tensor.matmul(db_.t[:], lhsT=ones_b.t[:], rhs=peb.t[:], start=(t == 0), stop=(t == 1)))
                            k.add_r(peb, tok)
                        k.set_w(nb_, tok); k.set_w(db_, tok)
                        k.acq_r('dve', db_); k.acq_w('dve', rdb)
                        k.wait('dve', k.sig('dve', nc.vector.reciprocal(out=rdb.t[:], in_=db_.t[:])), force=True)
                        k.acq_w('dve', ob)
                        tok = k.sig('dve', nc.vector.tensor_tensor(out=ob.t[:], in0=nb_.t[:], in1=rdb.t[:], op=ALU.mult))
                        k.add_r(nb_, tok); k.add_r(db_, tok); k.set_w(ob, tok)
                        k.store('pool', ob, seg.mix[(12 + mh) * 128:(13 + mh) * 128, q0:q0 + 512], ob.t[:])
                    k.add_r(qb_, tok)
            if stop is not None and stop <= l * 10 + 4:
                continue
            with Phase(k) as ph:
                wo = ph.sb([128, 16, 2048], BF16, dma=True, name="wo")
                gb = ph.sb([128, 2, 2048], F32, dma=True, name="gb")
                mt = ph.ring(2, [128, 16, 512], BF16, dma=True, name="mixT")
                xr = ph.ring(2, [128, 2048], F32, dma=True, name="xr")
                yt = ph.ring(2, [128, 2048], F32, dma=True, name="yt")
                st = ph.sb([128, 4, 6], F32); mv = ph.sb([128, 2], F32); rs = ph.sb([128, 1], F32); nb = ph.sb([128, 1], F32)
                ops = ph.pring(8, [128, 512], F32, name="ops")
                k.load('sp', wo, wo.t[:], wout_b[l].rearrange("(k p) n -> p k n", p=128))
                k.load('sp', gb, gb.t[:, 0, :], ln1_g[l].partition_broadcast(128))
                k.load('sp', gb, gb.t[:, 1, :], ln1_b[l].partition_broadcast(128), fresh=False)
                k.acq_r('pe', wo); k.acq_r('dve', gb)
                nblk = (oe - os_) // 512
                oc = 0; ti = 0
                for b in range(nblk):
                    t0 = os_ + b * 512
                    mb_ = mt[b % 2]
                    k.load('sp', mb_, mb_.t[:], seg.mix[:, t0:t0 + 512].rearrange("(k p) t -> p k t", p=128))
                    k.acq_r('pe', mb_)
                    for t in range(4):
                        xb_ = xr[ti % 2]; yb = yt[ti % 2]; ti += 1
                        r0 = t0 + t * 128
                        k.load('sp', xb_, xb_.t[:], xin[r0:r0 + 128, :])
                        pbs = []
                        for ch in range(4):
                            pb = ops[oc % 8]; oc += 1
                            k.acq_w('pe', pb)
                            for kk in range(16):
                                ins = nc.tensor.matmul(pb.t[:], lhsT=mb_.t[:, kk, t * 128:(t + 1) * 128], rhs=wo.t[:, kk, ch * 512:(ch + 1) * 512], start=(kk == 0), stop=(kk == 15))
                            tokp = k.sig('pe', ins)
                            k.set_w(pb, tokp)
                            pbs.append(pb)
                        k.acq_r('dve', xb_); k.acq_w('dve', yb)
                        for ch in range(4):
                            k.acq_r('dve', pbs[ch])
                            tok = k.sig('dve', nc.vector.scalar_tensor_tensor(out=yb.t[:, ch * 512:(ch + 1) * 512], in0=xb_.t[:, ch * 512:(ch + 1) * 512], scalar=float(ALPHA), in1=pbs[ch].t[:], op0=ALU.mult, op1=ALU.add))
                            k.add_r(pbs[ch], tok)
                        k.add_r(xb_, tok)
                        tok = ln_inplace(yb.t[:], gb, (st, mv, rs, nb))
                        k.set_w(yb, tok)
                        k.store('pool', yb, seg.x1[r0:r0 + 128, :], yb.t[:])
                    k.add_r(mb_, tokp)
            if stop is not None and stop <= l * 10 + 5:
                continue
            is_moe = (l % 2 == 1)
            if is_moe:
                with Phase(k) as ph:
                    rt = ph.sb([128, 16, NE], F32, dma=True, name="rt")
                    xr = ph.ring(2, [128, 2048], F32, dma=True, name="xr")
                    xT32 = ph.ring(2, [128, 16, 128], F32, name="xT32")
                    lg = ph.ring(2, [128, NE], F32, name="lg")
                    m8 = ph.sb([128, 8], F32); nv1 = ph.sb([128, 1], F32); ex = ph.sb([128, 8], F32); msk = ph.sb([128, 8], F32)
                    den = ph.sb([128, 1], F32); rden = ph.sb([128, 1], F32)
                    cb = ph.ring(2, [128, NE], F32, dma=True, name="cb")
                    tps = ph.pring(4, [128, 4, 128], F32, name="tps")
                    lps = ph.pring(2, [128, NE], F32, name="lps")
                    k.load('sp', rt, rt.t[:], router[0].rearrange("(k p) e -> p k e", p=128))
                    k.acq_r('pe', rt)
                    ntile = (oe - os_) // 128
                    tc_ = 0
                    for ti in range(ntile):
                        r0 = os_ + ti * 128
                        xb_ = xr[ti % 2]; xtb = xT32[ti % 2]; lgb = lg[ti % 2]; cbb = cb[ti % 2]; lp = lps[ti % 2]
                        k.load('sp', xb_, xb_.t[:], seg.x1[r0:r0 + 128, :])
                        k.acq_r('pe', xb_)
                        for kg in range(4):
                            tp = tps[tc_ % 4]; tc_ += 1
                            k.acq_w('pe', tp)
                            for kk in range(4):
                                ins = nc.tensor.transpose(tp.t[:, kk, :], xb_.t[:, (kg * 4 + kk) * 128:(kg * 4 + kk + 1) * 128], ident_f.t[:])
                            tokp = k.sig('pe', ins)
                            k.set_w(tp, tokp)
                            e = 'act' if kg % 2 == 0 else 'dve'
                            k.acq_r(e, tp); k.acq_w(e, xtb)
                            if e == 'act':
                                ins = nc.scalar.activation(out=xtb.t[:, kg * 4:(kg + 1) * 4, :], in_=tp.t[:], func=AF.Copy)
                            else:
                                ins = nc.vector.tensor_copy(out=xtb.t[:, kg * 4:(kg + 1) * 4, :], in_=tp.t[:])
                            tok = k.sig(e, ins)
                            k.add_r(tp, tok); k.set_w(xtb, tok, fresh=(kg == 0))
                        k.add_r(xb_, tokp)
                        k.acq_r('pe', xtb); k.acq_w('pe', lp)
                        for kk in range(16):
                            ins = nc.tensor.matmul(lp.t[:], lhsT=xtb.t[:, kk, :], rhs=rt.t[:, kk, :], start=(kk == 0), stop=(kk == 15))
                        tokp = k.sig('pe', ins)
                        k.set_w(lp, tokp); k.add_r(xtb, tokp)
                        k.acq_r('dve', lp); k.acq_w('dve', lgb); k.acq_w('dve', cbb)
                        k.wait('dve', k.sig('dve', nc.vector.tensor_copy(out=lgb.t[:], in_=lp.t[:])), force=True)
                        tok = k.sig('dve', nc.vector.max(out=m8.t[:], in_=lgb.t[:]))
                        k.wait('dve', tok, force=True)
                        nc.vector.tensor_scalar(out=msk.t[:], in0=lgb.t[:], scalar1=m8.t[:, 1:2], scalar2=None, op0=ALU.is_ge)
                        tok = k.sig('dve', nc.vector.tensor_scalar(out=nv1.t[:], in0=m8.t[:, 0:1], scalar1=-1.0, scalar2=None, op0=ALU.mult))
                        k.add_r(lp, tok)
                        k.wait('act', tok)
                        toka = k.sig('act', nc.scalar.activation(out=ex.t[:], in_=lgb.t[:], func=AF.Exp, bias=nv1.t[:, 0:1], scale=1.0))
                        k.wait('dve', toka)
                        k.wait('dve', k.sig('dve', nc.vector.tensor_tensor(out=ex.t[:], in0=ex.t[:], in1=msk.t[:], op=ALU.mult)), force=True)
                        k.wait('dve', k.sig('dve', nc.vector.tensor_reduce(out=den.t[:], in_=ex.t[:], axis=mybir.AxisListType.X, op=ALU.add)), force=True)
                        tok = k.sig('dve', nc.vector.reciprocal(out=rden.t[:], in_=den.t[:]))
                        k.wait('dve', tok, force=True)
                        tok = k.sig('dve', nc.vector.tensor_scalar(out=cbb.t[:], in0=ex.t[:], scalar1=rden.t[:, 0:1], scalar2=None, op0=ALU.mult))
                        k.set_w(cbb, tok); k.set_w(lgb, tok)
                        k.store('pool', cbb, seg.comb[r0:r0 + 128, :], cbb.t[:])
            last = (l == DEPTH - 1)
            with Phase(k) as ph:
                nexp = NE if is_moe else 1
                NF = (E_FF if is_moe else D_FF) // 128
                FG = 4
                xb = ph.sb([128, 4, 2048], BF16, dma=True, name="xb")
                xT = ph.sb([128, 16, 512], BF16, name="xT")
                gT = ph.sb([128, NF, 512], BF16, name="gT")
                accs = ph.sb([128, 4, 2048], F32, dma=True, name="acc")
                w13 = ph.ring(2, [128, 2, 2048], BF16, dma=True, name="w13")
                w2r = ph.ring(3, [128, FG, 512], BF16, dma=True, name="w2r")
                xr = ph.sb([128, 2048], F32, dma=True, name="xr")
                gb = ph.sb([128, 2, 2048], F32, dma=True, name="gb")
                stt = ph.ring(2, [128, 512], F32, name="silu")
                cbt = ph.sb([128, 4, NE], F32, dma=True, name="cbt")
                vt = ph.sb([128, W // 128], F32, dma=True, name="vt")
                st = ph.sb([128, 4, 6], F32); mv = ph.sb([128, 2], F32); rs = ph.sb([128, 1], F32); nb = ph.sb([128, 1], F32)
                tpr = ph.pring(1, [128, 4, 512], BF16, name="tp")
                hps = ph.pring(2, [128, 512], F32, name="hps")
                ops = ph.pring(4, [128, 512], F32, name="ops")
                k.load('sp', vt, vt.t[:], seg.valid[:, :])
                k.load('sp', gb, gb.t[:, 0, :], (ln2_g if True else ln1_g)[l].partition_broadcast(128))
                k.load('sp', gb, gb.t[:, 1, :], ln2_b[l].partition_broadcast(128), fresh=False)
                k.acq_r('dve', gb); k.acq_r('dve', vt)
                nblk = (oe - os_) // 512
                cnt = [0]
                wc = 0; w2c = 0; hc = 0
                for b in range(nblk):
                    t0 = os_ + b * 512
                    load_xT(ph, seg.x1[t0:t0 + 512, :], xb, xT, tpr, cnt)
                    if is_moe:
                        k.load('sp', cbt, cbt.t[:], seg.comb[t0:t0 + 512, :].rearrange("(t p) e -> p t e", p=128))
                        k.acq_r('dve', cbt)
                    for e in range(nexp):
                        w1s = (m1_b[e] if is_moe else f1_b); w3s = (m3_b[e] if is_moe else f3_b); w2s = (m2_b[e] if is_moe else f2_b)
                        for f in range(NF):
                            wb = w13[wc % 2]; wc += 1
                            k.load('sp', wb, wb.t[:, 0, :], w1s[f])
                            k.load('sp', wb, wb.t[:, 1, :], w3s[f], fresh=False)
                            k.acq_r('pe', wb); k.acq_r('pe', xT)
                            h1 = hps[0]; h3 = hps[1]
                            k.acq_w('pe', h1)
                            for kk in range(16):
                                ins = nc.tensor.matmul(h1.t[:], lhsT=wb.t[:, 0, kk * 128:(kk + 1) * 128], rhs=xT.t[:, kk, :], start=(kk == 0), stop=(kk == 15))
                            k.set_w(h1, k.sig('pe', ins))
                            k.acq_w('pe', h3)
                            for kk in range(16):
                                ins = nc.tensor.matmul(h3.t[:], lhsT=wb.t[:, 1, kk * 128:(kk + 1) * 128], rhs=xT.t[:, kk, :], start=(kk == 0), stop=(kk == 15))
                            tokp = k.sig('pe', ins)
                            k.set_w(h3, tokp); k.add_r(wb, tokp)
                            sb_ = stt[hc % 2]; hc += 1
                            k.acq_r('act', h1); k.acq_w('act', sb_)
                            tok = k.sig('act', nc.scalar.activation(out=sb_.t[:], in_=h1.t[:], func=AF.Silu))
                            k.add_r(h1, tok); k.set_w(sb_, tok)
                            k.acq_r('dve', sb_); k.acq_r('dve', h3)
                            if f == 0:
                                k.acq_w('dve', gT)
                            tok = k.sig('dve', nc.vector.tensor_tensor(out=gT.t[:, f, :], in0=sb_.t[:], in1=h3.t[:], op=ALU.mult))
                            k.add_r(sb_, tok); k.add_r(h3, tok); k.set_w(gT, tok, fresh=(f == 0))
                        if e == nexp - 1:
                            k.add_r(xT, tokp)
                        k.acq_r('pe', gT)
                        for dch in range(4):
                            for t in range(4):
                                k.acq_w('pe', ops[t])
                            for fg in range(NF // FG):
                                wb = w2r[w2c % 3]; w2c += 1
                                k.load('sp', wb, wb.t[:], w2s[fg * FG * 128:(fg + 1) * FG * 128, dch * 512:(dch + 1) * 512].rearrange("(f p) d -> p f d", p=128))
                                k.acq_r('pe', wb)
                                for fi in range(FG):
                                    f = fg * FG + fi
                                    for t in range(4):
                                        ins = nc.tensor.matmul(ops[t].t[:], lhsT=gT.t[:, f, t * 128:(t + 1) * 128], rhs=wb.t[:, fi, :], start=(f == 0), stop=(f == NF - 1))
                                tokp = k.sig('pe', ins)
                                k.add_r(wb, tokp)
                            for t in range(4):
                                k.set_w(ops[t], tokp)
                            if e == 0 and dch == 0:
                                k.acq_w('dve', accs)
                            for t in range(4):
                                k.acq_r('dve', ops[t])
                                dst = accs.t[:, t, dch * 512:(dch + 1) * 512]
                                if not is_moe:
                                    ins = nc.vector.tensor_copy(out=dst, in_=ops[t].t[:])
                                elif e == 0:
                                    ins = nc.vector.tensor_scalar(out=dst, in0=ops[t].t[:], scalar1=cbt.t[:, t, e:e + 1], scalar2=None, op0=ALU.mult)
                                else:
                                    ins = nc.vector.scalar_tensor_tensor(out=dst, in0=ops[t].t[:], scalar=cbt.t[:, t, e:e + 1], in1=dst, op0=ALU.mult, op1=ALU.add)
                                tok = k.sig('dve', ins)
                                k.add_r(ops[t], tok)
                            k.set_w(accs, tok, fresh=(e == 0 and dch == 0))
                        k.add_r(gT, tokp)
                    if is_moe:
                        k.add_r(cbt, tok)
                    for t in range(4):
                        r0 = t0 + t * 128
                        k.load('sp', xr, xr.t[:], seg.x1[r0:r0 + 128, :])
                        k.acq_r('dve', xr)
                        yt = accs.t[:, t, :]
                        tok = k.sig('dve', nc.vector.scalar_tensor_tensor(out=yt, in0=xr.t[:], scalar=float(ALPHA), in1=yt, op0=ALU.mult, op1=ALU.add))
                        k.add_r(xr, tok)
                        va = None if last else vt.t[:, r0 // 128:r0 // 128 + 1]
                        tok = ln_inplace(yt, gb, (st, mv, rs, nb), valid_ap=va)
                        k.set_w(accs, tok, fresh=False)
                        if last:
                            if os_ <= r0 < oe:
                                k.store('pool', accs, seg.yout[r0 - seg.yoff:r0 - seg.yoff + 128, :], yt)
                        else:
                            k.store('pool', accs, seg.x2[r0:r0 + 128, :], yt)
    return nc


def _host_prep(inputs):
    f32 = np.float32
    w_in = np.asarray(inputs["w_in"], f32)
    L = w_in.shape[0]
    idx = np.arange(64)
    perm = idx.copy()
    perm[0:8] = idx[8:16]
    perm[8:16] = idx[0:8]
    qcols = 1536 + (np.arange(12)[:, None] * 64 + perm[None, :]).reshape(-1)
    kcols = 2304 + (np.arange(12)[:, None] * 64 + perm[None, :]).reshape(-1)
    w_in_ext = np.concatenate([w_in, w_in[:, :, qcols], w_in[:, :, kcols]], axis=-1)
    cpar = np.zeros((L, 128, 6, 34), f32)
    for l in range(L):
        cpar[l, :, :, 0:31] = np.asarray(inputs["conv_w"], f32)[l].T.reshape(6, 128, 31).transpose(1, 0, 2)
        cpar[l, :, :, 31] = np.asarray(inputs["conv_b"], f32)[l].reshape(6, 128).T
        cpar[l, :, :, 32] = np.asarray(inputs["conv_ln_g"], f32)[l].reshape(6, 128).T
        cpar[l, :, :, 33] = np.asarray(inputs["conv_ln_b"], f32)[l].reshape(6, 128).T
    shared = {
        "w_in_ext": np.ascontiguousarray(w_in_ext), "cpar": cpar,
        "maskd": _mult_mask(), "identd": np.eye(128, dtype=f32),
        "cs_p": _rope_tables(np.arange(PW)), "valid_p": np.ones((128, PW // 128), f32),
    }
    for n in ("w_mem_kv", "w_out", "ln1_g", "ln1_b", "ln2_g", "ln2_b", "ffn_w1", "ffn_w3", "ffn_w2",
              "moe_router", "moe_w1", "moe_w3", "moe_w2"):
        shared[n] = np.ascontiguousarray(np.asarray(inputs[n], f32))
    xp = np.asarray(inputs["x_prompt"], f32)
    xs = np.asarray(inputs["x_sample"], f32)
    mp = np.asarray(inputs["mem_prompt"], f32)
    ms = np.asarray(inputs["mem_sample"], f32)
    in_maps = []
    for c in range(NCORE):
        sq, j = c // 4, c % 4
        a = j * 4096
        lo = a - 2048
        pos = np.arange(lo, lo + SW)
        ok = (pos >= 0) & (pos < 16384)
        xw = np.zeros((SW, D), f32)
        xw[ok] = xs[sq, pos[ok]]
        m = dict(shared)
        m["xp"] = np.ascontiguousarray(xp[c])
        m["xs"] = xw
        m["memp"] = np.ascontiguousarray(mp[c])
        m["mems"] = np.ascontiguousarray(ms[sq])
        m["valid_s"] = np.ascontiguousarray(ok.astype(f32).reshape(SW // 128, 128).T)
        m["cs_s"] = _rope_tables(np.where(ok, pos, 0))
        in_maps.append(m)
    return in_maps


def kernel(**inputs):
    in_maps = _host_prep(inputs)
    nc = build()
    res = run_bass_kernel_spmd(nc, in_maps, core_ids=list(range(NCORE)))
    yp = np.stack([res.results[c]["yp"] for c in range(NCORE)], axis=0)
    ysf = np.zeros((2, 16384, D), np.float32)
    for c in range(NCORE):
        sq, j = c // 4, c % 4
        ysf[sq, j * 4096:(j + 1) * 4096] = res.results[c]["ys"]
    return (yp.astype(np.float32), ysf)
```

```python
import numpy as np
from contextlib import ExitStack
import concourse.bass as bass
import concourse.mybir as mybir
from concourse.bass_utils import run_bass_kernel_spmd

F32 = mybir.dt.float32
BF16 = mybir.dt.bfloat16
AF = mybir.ActivationFunctionType
ALU = mybir.AluOpType

D = 2048
DEPTH = 2
NCORE = 8
D_CONV = 768
D_ATT = 768
D_MEM = 512
D_IN = 4352
D_EXT = D_IN + 2 * D_ATT
NMT = D_EXT // 128
D_FF = 5632
E_FF = 7168
NE = 8
ALPHA = (2 * DEPTH) ** 0.25
LN_EPS = 1e-5
ROPE_THETA = 500000.0
MASK_D0 = 1408
MASK_J = 2944
SW = 8192
PW = 2048


def _mult_mask():
    kk = np.arange(128)[:, None]
    j = np.arange(MASK_J)[None, :]
    o = kk - j + MASK_D0
    ao = np.abs(o)
    c = (ao <= 64).astype(np.float32) + ((o % 4 == 0) & (ao <= 256)) + ((o % 16 == 0) & (ao <= 1024))
    return c.astype(np.float32)


def _rope_tables(pos):
    half = 8
    inv = np.power(np.float32(ROPE_THETA), -np.arange(0, 16, 2, dtype=np.float32) / np.float32(16)).astype(np.float32)
    ang = pos.astype(np.float32)[None, :] * inv[:, None]
    cos = np.cos(ang).astype(np.float32)
    sin = np.sin(ang).astype(np.float32)
    W = pos.shape[0]
    C = np.ones((128, W), np.float32)
    S = np.zeros((128, W), np.float32)
    for hh in range(2):
        b = hh * 64
        C[b:b + 8] = cos
        C[b + 8:b + 16] = cos
        S[b:b + 8] = -sin
        S[b + 8:b + 16] = sin
    return np.stack([C, S], axis=0)


class Sem:
    def __init__(self, nc, name):
        self.h = nc.semaphore(name).__enter__()
        self.n = 0
        self.name = name


class Buf:
    def __init__(self, tile, sem=None):
        self.t = tile
        self.rd = []
        self.wr = []
        self.sem = sem


class KB:
    def __init__(self):
        self.nc = bass.Bass("TRN2", target_bir_lowering=False)
        nc = self.nc
        self.eng = {'pe': nc.tensor, 'act': nc.scalar, 'dve': nc.vector, 'pool': nc.gpsimd, 'sp': nc.sync}
        self.esem = {e: Sem(nc, "e_" + e) for e in ('pe', 'act', 'dve', 'pool')}
        self.dsems = [Sem(nc, f"d{i}") for i in range(72)]
        self.dfree = list(self.dsems)
        self.waited = {}
        self.pending = []
        self.uid = 0

    def sig(self, e, ins):
        s = self.esem[e]
        ins.then_inc(s.h, 1)
        s.n += 1
        return (s, s.n, e)

    def wait(self, e, tok, force=False):
        if tok is None or (tok[2] == e and not force):
            return
        key = (e, tok[0].name)
        if self.waited.get(key, 0) >= tok[1]:
            return
        self.waited[key] = tok[1]
        self.eng[e].wait_ge(tok[0].h, tok[1])

    def dma(self, q, out, in_, sem):
        ins = self.eng[q].dma_start(out=out, in_=in_)
        ins.then_inc(sem.h, 16)
        sem.n += 16
        return (sem, sem.n, None)

    def getsem(self):
        return self.dfree.pop()

    def acq_w(self, e, b):
        for t in b.rd + b.wr:
            self.wait(e, t)

    def set_w(self, b, tok, fresh=True):
        if fresh:
            b.rd = []
            b.wr = [tok]
        else:
            b.wr.append(tok)

    def acq_r(self, e, b):
        for t in b.wr:
            self.wait(e, t)

    def add_r(self, b, tok):
        b.rd.append(tok)

    def load(self, q, b, out, in_, fresh=True):
        self.acq_w(q, b)
        tok = self.dma(q, out, in_, b.sem)
        self.set_w(b, tok, fresh)
        return tok

    def iload(self, b, out, in_, idx_ap, elem_off=0, fresh=True, bounds=None):
        self.acq_w('pool', b)
        kw = {}
        if bounds is not None:
            kw = dict(bounds_check=bounds, oob_is_err=False)
        ins = self.nc.gpsimd.indirect_dma_start(out=out, out_offset=None, in_=in_,
                                                in_offset=bass.IndirectOffsetOnAxis(ap=idx_ap, axis=0),
                                                element_offset=elem_off, **kw)
        ins.then_inc(b.sem.h, 16)
        b.sem.n += 16
        tok = (b.sem, b.sem.n, None)
        self.set_w(b, tok, fresh)
        return tok

    def store(self, q, b, out, in_):
        self.acq_r(q, b)
        tok = self.dma(q, out, in_, b.sem)
        self.add_r(b, tok)
        self.pending.append((q, tok))
        return tok

    def phase_end(self):
        for q, tok in self.pending:
            self.wait(q, tok)
        self.pending = []
        self.nc.all_engine_barrier()


class Phase:
    def __init__(self, k):
        self.k = k
        self.es = ExitStack()
        self.sems = []

    def __enter__(self):
        self.es.__enter__()
        return self

    def __exit__(self, *a):
        self.k.phase_end()
        for s in self.sems:
            self.k.dfree.append(s)
        return self.es.__exit__(*a)

    def sb(self, shape, dt, dma=False, name=None):
        k = self.k
        k.uid += 1
        t = self.es.enter_context(k.nc.sbuf_tensor(f"{name or 't'}{k.uid}", list(shape), dt))
        s = None
        if dma:
            s = k.getsem()
            self.sems.append(s)
        return Buf(t, s)

    def ps(self, shape, dt, name=None):
        k = self.k
        k.uid += 1
        t = self.es.enter_context(k.nc.psum_tensor(f"{name or 'p'}{k.uid}", list(shape), dt))
        return Buf(t)

    def ring(self, n, shape, dt, dma=False, name=None):
        return [self.sb(shape, dt, dma, name) for _ in range(n)]

    def pring(self, n, shape, dt, name=None):
        return [self.ps(shape, dt, name) for _ in range(n)]


class Seg:
    pass


def build(dbg=False, stop=None, only_p=False):
    k = KB()
    nc = k.nc
    E = k.eng

    def din(name, shape, dt=F32):
        return nc.dram_tensor(name, list(shape), dt, kind="ExternalInput").ap()

    def dscr(name, shape, dt, out=False):
        return nc.dram_tensor(name, list(shape), dt, kind=("ExternalOutput" if out else "Internal")).ap()

    xp = din("xp", [PW, D])
    xs = din("xs", [SW, D])
    memp = din("memp", [256, D])
    mems = din("mems", [256, D])
    valid_s = din("valid_s", [128, SW // 128])
    valid_p = din("valid_p", [128, PW // 128])
    cs_p = din("cs_p", [2, 128, PW])
    cs_s = din("cs_s", [2, 128, SW])
    maskd = din("maskd", [128, MASK_J])
    identd = din("identd", [128, 128])
    triud = din("triud", [128, 128])
    iotad = din("iotad", [128, 1])
    w_in = din("w_in_ext", [DEPTH, D, D_EXT])
    cpar = din("cpar", [DEPTH, 128, 6, 34])
    w_memkv = din("w_mem_kv", [DEPTH, D, 1024])
    w_out = din("w_out", [DEPTH, D, D])
    ln1_g = din("ln1_g", [DEPTH, D]); ln1_b = din("ln1_b", [DEPTH, D])
    ln2_g = din("ln2_g", [DEPTH, D]); ln2_b = din("ln2_b", [DEPTH, D])
    ffn_w1 = din("ffn_w1", [1, D, D_FF]); ffn_w3 = din("ffn_w3", [1, D, D_FF]); ffn_w2 = din("ffn_w2", [1, D_FF, D])
    router = din("moe_router", [1, D, NE])
    moe_w1 = din("moe_w1", [1, NE, D, E_FF]); moe_w3 = din("moe_w3", [1, NE, D, E_FF]); moe_w2 = din("moe_w2", [1, NE, E_FF, D])
    yp = nc.dram_tensor("yp", [PW, D], F32, kind="ExternalOutput").ap()
    ys = nc.dram_tensor("ys", [4096, D], F32, kind="ExternalOutput").ap()

    win_b = [dscr(f"win_b{l}", [NMT, 128, 16 * 128], BF16) for l in range(DEPTH)]
    wkv_b = [dscr(f"wkv_b{l}", [8, 128, 16 * 128], BF16) for l in range(DEPTH)]
    wout_b = [dscr(f"wout_b{l}", [D, D], BF16) for l in range(DEPTH)]
    f1_b = dscr("f1_b", [D_FF // 128, 128, 2048], BF16)
    f3_b = dscr("f3_b", [D_FF // 128, 128, 2048], BF16)
    f2_b = dscr("f2_b", [D_FF, D], BF16)
    NFE = E_FF // 128
    FG = 4
    NFG = NFE // FG
    NFH = NFE // 2
    m13_h = [dscr(f"m13_b{h}", [NE * NFH * 128, 2 * 2048], BF16) for h in range(2)]
    m2_b = dscr("m2_b", [NE * 4 * NFG * 128, FG * 512], BF16)

    segs = []
    for nm, W, xin, mem, valid, cs, hr, orr, yout, yoff in (
            ("p", PW, xp, memp, valid_p, cs_p, [(0, PW), (0, PW)], [(0, PW), (0, PW)], yp, 0),
            ("s", SW, xs, mems, valid_s, cs_s, [(0, SW), (1024, 7168)], [(1024, 7168), (2048, 6144)], ys, 2048)):
        s = Seg()
        s.nm, s.W, s.x0, s.mem, s.valid, s.cs, s.hr, s.orr, s.yout, s.yoff = nm, W, xin, mem, valid, cs, hr, orr, yout, yoff
        s.u = dscr(f"u_{nm}", [D_CONV, W], F32, out=dbg)
        s.q = dscr(f"q_{nm}", [D_ATT, W], BF16, out=dbg)
        s.kk = dscr(f"k_{nm}", [D_ATT, W], BF16, out=dbg)
        s.qm = dscr(f"qm_{nm}", [D_MEM, W], BF16, out=dbg)
        s.v = dscr(f"v_{nm}", [W, 12 * 128], BF16, out=dbg)
        s.mix = dscr(f"mix_{nm}", [D, W], BF16, out=dbg)
        s.x1 = dscr(f"x1_{nm}", [W, D], F32, out=dbg)
        s.x2 = dscr(f"x2_{nm}", [W, D], F32, out=dbg)
        s.comb = dscr(f"comb_{nm}", [W, NE], F32, out=dbg)
        segs.append(s)
    if only_p:
        segs = segs[:1]

    wsem = k.getsem()

    def conv_tiled(src, dst, ncols):
        for m in range(ncols // 128):
            s_ap = src[:, m * 128:(m + 1) * 128].rearrange("(k p) c -> p k c", p=128)
            d_ap = dst[m].rearrange("p (k c) -> p k c", c=128)
            k.pending.append(('pool', k.dma('pool', d_ap, s_ap, wsem)))

    def conv_plain(src, dst, nrows):
        for r in range(0, nrows, 128):
            k.pending.append(('pool', k.dma('pool', dst[r:r + 128, :], src[r:r + 128, :], wsem)))

    for l in range(DEPTH):
        conv_tiled(w_in[l], win_b[l], D_EXT)
        conv_tiled(w_memkv[l], wkv_b[l], 1024)
        conv_plain(w_out[l], wout_b[l], D)
    conv_tiled(ffn_w1[0], f1_b, D_FF)
    conv_tiled(ffn_w3[0], f3_b, D_FF)
    conv_plain(ffn_w2[0], f2_b, D_FF)
    if stop is None or stop > 10:
        for e in range(NE):
            for m in range(NFE):
                r0 = (e * NFH + (m % NFH)) * 128
                for wi, src in enumerate((moe_w1[0, e], moe_w3[0, e])):
                    s_ap = src[:, m * 128:(m + 1) * 128].rearrange("(k p) c -> p k c", p=128)
                    d_ap = m13_h[m // NFH][r0:r0 + 128, wi * 2048:(wi + 1) * 2048].rearrange("p (k c) -> p k c", c=128)
                    k.pending.append(('pool', k.dma('pool', d_ap, s_ap, wsem)))
            for fg in range(NFG):
                for dch in range(4):
                    r0 = ((e * 4 + dch) * NFG + fg) * 128
                    s_ap = moe_w2[0, e][fg * 512:(fg + 1) * 512, dch * 512:(dch + 1) * 512].rearrange("(fi p) c -> p fi c", p=128)
                    d_ap = m2_b[r0:r0 + 128, :].rearrange("p (fi c) -> p fi c", c=512)
                    k.pending.append(('pool', k.dma('pool', d_ap, s_ap, wsem)))
    k.pending = [k.pending[-1]]
    k.phase_end()

    cst = ExitStack()
    ident_b = Buf(cst.enter_context(nc.sbuf_tensor("ident_b", [128, 128], BF16)), k.getsem())
    ident_f = Buf(cst.enter_context(nc.sbuf_tensor("ident_f", [128, 128], F32)), k.getsem())
    ones_b = Buf(cst.enter_context(nc.sbuf_tensor("ones_b", [128, 128], BF16)))
    ones_f = Buf(cst.enter_context(nc.sbuf_tensor("ones_f", [128, 128], F32)))
    epsb = Buf(cst.enter_context(nc.sbuf_tensor("epsb", [128, 1], F32)))
    k.load('pool', ident_b, ident_b.t[:], identd[:, :])
    k.load('sp', ident_f, ident_f.t[:], identd[:, :])
    k.set_w(ones_b, k.sig('dve', nc.vector.memset(ones_b.t[:], 1.0)))
    k.set_w(ones_f, k.sig('dve', nc.vector.memset(ones_f.t[:], 1.0)))
    k.set_w(epsb, k.sig('dve', nc.vector.memset(epsb.t[:], LN_EPS)))
    for e in ('pe', 'act', 'dve', 'pool'):
        k.acq_r(e, ident_b); k.acq_r(e, ident_f); k.acq_r(e, ones_b); k.acq_r(e, ones_f); k.acq_r(e, epsb)

    def load_xT(ph, src_rows, xb, xT, tp_ring, cnt):
        k.load('pool', xb, xb.t[:], src_rows.rearrange("(t p) d -> p t d", p=128))
        k.acq_r('pe', xb)
        k.acq_w('act', xT); k.acq_w('dve', xT)
        first = True
        tokp_last = [None]
        for kg in range(4):
            tp = tp_ring[cnt[0] % len(tp_ring)]; cnt[0] += 1
            k.acq_w('pe', tp)
            for kk in range(4):
                for t in range(4):
                    ins = nc.tensor.transpose(tp.t[:, kk, t * 128:(t + 1) * 128], xb.t[:, t, (kg * 4 + kk) * 128:(kg * 4 + kk + 1) * 128], ident_b.t[:])
            tokp_last[0] = k.sig('pe', ins)
            k.set_w(tp, tokp_last[0])
            e = 'act' if kg % 2 == 0 else 'dve'
            k.acq_r(e, tp)
            if e == 'act':
                ins = nc.scalar.activation(out=xT.t[:, kg * 4:(kg + 1) * 4, :], in_=tp.t[:], func=AF.Copy)
            else:
                ins = nc.vector.tensor_copy(out=xT.t[:, kg * 4:(kg + 1) * 4, :], in_=tp.t[:])
            tok = k.sig(e, ins)
            k.add_r(tp, tok)
            k.set_w(xT, tok, fresh=first)
            first = False
        k.add_r(xb, tokp_last[0])

    def ln_inplace(yt, gb, rs_pool, valid_ap=None):
        st, mv, rs, nb = rs_pool
        for ch in range(4):
            ins = nc.vector.bn_stats(out=st.t[:, ch, :], in_=yt[:, ch * 512:(ch + 1) * 512])
        k.wait('dve', k.sig('dve', ins), force=True)
        tok = k.sig('dve', nc.vector.bn_aggr(out=mv.t[:], in_=st.t[:].rearrange("p a b -> p (a b)")))
        k.wait('act', tok)
        tok = k.sig('act', nc.scalar.activation(out=rs.t[:], in_=mv.t[:, 1:2], func=AF.Sqrt, bias=epsb.t[:, 0:1], scale=1.0))
        k.wait('dve', tok)
        tok = k.sig('dve', nc.vector.reciprocal(out=rs.t[:], in_=rs.t[:]))
        k.wait('dve', tok, force=True)
        tok = k.sig('dve', nc.vector.tensor_scalar(out=nb.t[:], in0=mv.t[:, 0:1], scalar1=rs.t[:, 0:1], scalar2=-1.0, op0=ALU.mult, op1=ALU.mult))
        k.wait('act', tok)
        tok = k.sig('act', nc.scalar.activation(out=yt, in_=yt, func=AF.Identity, bias=nb.t[:, 0:1], scale=rs.t[:, 0:1]))
        k.wait('dve', tok)
        nc.vector.tensor_tensor(out=yt, in0=yt, in1=gb.t[:, 0, :], op=ALU.mult)
        ins = nc.vector.tensor_tensor(out=yt, in0=yt, in1=gb.t[:, 1, :], op=ALU.add)
        if valid_ap is not None:
            ins = nc.vector.tensor_scalar(out=yt, in0=yt, scalar1=valid_ap, scalar2=None, op0=ALU.mult)
        return k.sig('dve', ins)

    for l in range(DEPTH):
        if stop is not None and stop <= l * 10:
            break
        for seg in segs:
            xin = seg.x0 if l == 0 else seg.x2
            hs, he = seg.hr[l]
            os_, oe = seg.orr[l]
            W = seg.W
            with Phase(k) as ph:
                xb = ph.sb([128, 4, 2048], BF16, dma=True, name="xb")
                xT = ph.sb([128, 16, 512], BF16, name="xT")
                wv = ph.sb([128, 6, 2048], BF16, dma=True, name="wv")
                wt = ph.ring(6, [128, 2048], BF16, dma=True, name="wt")
                cst_ = ph.ring(2, [128, 2, 512], F32, dma=True, name="cs")
                vt = ph.sb([128, W // 128], F32, dma=True, name="vt")
                onesv = ph.sb([128, 6, 64], BF16, name="onesv")
                sg = ph.ring(2, [128, 512], F32, name="sg")
                uo = ph.ring(2, [128, 512], F32, dma=True, name="uo")
                t1 = ph.ring(2, [128, 512], F32, name="t1")
                t2 = ph.ring(2, [128, 512], F32, name="t2")
                qo = ph.ring(2, [128, 512], BF16, dma=True, name="qo")
                qmo = ph.ring(2, [128, 512], BF16, dma=True, name="qmo")
                vx = ph.ring(2, [128, 12, 128], BF16, dma=True, name="vx")
                tpr = ph.pring(2, [128, 4, 512], BF16, name="tp")
                mm = ph.pring(4, [128, 512], F32, name="mm")
                cnt = [0]
                mmc = [0]
                wtc = [0]
                k.set_w(onesv, k.sig('pool', nc.gpsimd.memset(onesv.t[:], 1.0)))
                k.load('sp', vt, vt.t[:], seg.valid[:, :])
                for m in range(6):
                    k.load('sp', wv, wv.t[:, m, :], win_b[l][24 + m], fresh=(m == 0))
                k.acq_r('pe', wv)
                k.acq_r('pool', vt)

                def mtile(m, xTb):
                    wb = wt[wtc[0] % 6]; wtc[0] += 1
                    k.load('sp', wb, wb.t[:], win_b[l][m])
                    pb = mm[mmc[0] % 4]; mmc[0] += 1
                    k.acq_r('pe', wb); k.acq_w('pe', pb); k.acq_r('pe', xTb)
                    for kk in range(16):
                        ins = nc.tensor.matmul(pb.t[:], lhsT=wb.t[:, kk * 128:(kk + 1) * 128], rhs=xTb.t[:, kk, :], start=(kk == 0), stop=(kk == 15))
                    tok = k.sig('pe', ins)
                    k.set_w(pb, tok); k.add_r(wb, tok)
                    return pb, tok

                nblk = (he - hs) // 512
                for b in range(nblk):
                    t0 = hs + b * 512
                    load_xT(ph, xin[t0:t0 + 512, :], xb, xT, tpr, cnt)
                    csb = cst_[b % 2]
                    k.load('sp', csb, csb.t[:], seg.cs[:, :, t0:t0 + 512].rearrange("a p t -> p a t"))
                    lasttok = None
                    for c in range(6):
                        pa, _ = mtile(c, xT)
                        pg, _ = mtile(6 + c, xT)
                        sgb = sg[c % 2]; uob = uo[c % 2]
                        k.acq_r('act', pg); k.acq_w('act', sgb)
                        tok = k.sig('act', nc.scalar.activation(out=sgb.t[:], in_=pg.t[:], func=AF.Sigmoid))
                        k.add_r(pg, tok); k.set_w(sgb, tok)
                        k.acq_r('dve', pa); k.acq_r('dve', sgb); k.acq_w('dve', uob)
                        tok = k.sig('dve', nc.vector.tensor_tensor(out=uob.t[:], in0=pa.t[:], in1=sgb.t[:], op=ALU.mult))
                        k.add_r(pa, tok); k.add_r(sgb, tok); k.set_w(uob, tok)
                        k.store('pool', uob, seg.u[c * 128:(c + 1) * 128, t0:t0 + 512], uob.t[:])
                    k.acq_r('dve', csb)
                    for which, base, pbase, dst in (("q", 12, 34, seg.q), ("k", 18, 40, seg.kk)):
                        for hp in range(6):
                            pq, _ = mtile(base + hp, xT)
                            pp, _ = mtile(pbase + hp, xT)
                            i2 = hp % 2
                            k.acq_r('dve', pq); k.acq_w('dve', t1[i2])
                            tok = k.sig('dve', nc.vector.tensor_tensor(out=t1[i2].t[:], in0=pq.t[:], in1=csb.t[:, 0, :], op=ALU.mult))
                            k.add_r(pq, tok); k.set_w(t1[i2], tok)
                            k.acq_r('dve', pp); k.acq_w('dve', t2[i2])
                            tok = k.sig('dve', nc.vector.tensor_tensor(out=t2[i2].t[:], in0=pp.t[:], in1=csb.t[:, 1, :], op=ALU.mult))
                            k.add_r(pp, tok); k.set_w(t2[i2], tok); k.add_r(csb, tok)
                            k.acq_r('pool', t1[i2]); k.acq_r('pool', t2[i2]); k.acq_w('pool', qo[i2])
                            tok = k.sig('pool', nc.gpsimd.tensor_tensor(out=qo[i2].t[:], in0=t1[i2].t[:], in1=t2[i2].t[:], op=ALU.add))
                            k.add_r(t1[i2], tok); k.add_r(t2[i2], tok); k.set_w(qo[i2], tok)
                            k.store('pool', qo[i2], dst[hp * 128:(hp + 1) * 128, t0:t0 + 512], qo[i2].t[:])
                    for mh in range(4):
                        pq, _ = mtile(30 + mh, xT)
                        ob = qmo[mh % 2]
                        k.acq_r('act', pq); k.acq_w('act', ob)
                        tok = k.sig('act', nc.scalar.activation(out=ob.t[:], in_=pq.t[:], func=AF.Copy))
                        k.add_r(pq, tok); k.set_w(ob, tok)
                        k.store('pool', ob, seg.qm[mh * 128:(mh + 1) * 128, t0:t0 + 512], ob.t[:])
                    for t in range(4):
                        vb = vx[t % 2]
                        tile_idx = (t0 // 128) + t
                        k.acq_w('pool', vb)
                        v5 = vb.t[:].rearrange("p (a two) d -> p a two d", two=2)
                        nc.gpsimd.tensor_scalar(out=v5[:, :, 0, 64:128], in0=onesv.t[:], scalar1=vt.t[:, tile_idx:tile_idx + 1], scalar2=None, op0=ALU.mult)
                        tok = k.sig('pool', nc.gpsimd.tensor_scalar(out=v5[:, :, 1, 0:64], in0=onesv.t[:], scalar1=vt.t[:, tile_idx:tile_idx + 1], scalar2=None, op0=ALU.mult))
                        k.set_w(vb, tok)
                        for (c0, ncol, h0, nh) in ((0, 512, 0, 8), (512, 256, 8, 4)):
                            pb = mm[mmc[0] % 4]; mmc[0] += 1
                            k.acq_w('pe', pb); k.acq_r('pe', xT)
                            for kk in range(16):
                                ins = nc.tensor.matmul(pb.t[:, 0:ncol], lhsT=xT.t[:, kk, t * 128:(t + 1) * 128],
                                                       rhs=wv.t[:, c0 // 128:(c0 + ncol) // 128, kk * 128:(kk + 1) * 128],
                                                       start=(kk == 0), stop=(kk == 15))
                            tokp = k.sig('pe', ins)
                            lasttok = tokp
                            k.set_w(pb, tokp)
                            p4 = pb.t[:, 0:ncol].rearrange("p (a two d) -> p a two d", two=2, d=64)
                            d4 = vb.t[:, h0:h0 + nh, :].rearrange("p (a two) d -> p a two d", two=2)
                            k.acq_r('act', pb); k.acq_w('act', vb)
                            tok = k.sig('act', nc.scalar.activation(out=d4[:, :, 0, 0:64], in_=p4[:, :, 0, :], func=AF.Copy))
                            k.add_r(pb, tok); k.set_w(vb, tok, fresh=False)
                            k.acq_r('dve', pb); k.acq_w('dve', vb)
                            tok = k.sig('dve', nc.vector.tensor_copy(out=d4[:, :, 1, 64:128], in_=p4[:, :, 1, :]))
                            k.add_r(pb, tok); k.set_w(vb, tok, fresh=False)
                        k.store('pool', vb, seg.v[t0 + t * 128:t0 + (t + 1) * 128, :], vb.t[:].rearrange("p h d -> p (h d)"))
                    k.add_r(xT, lasttok)
            if stop is not None and stop <= l * 10 + 1:
                continue
            with Phase(k) as ph:
                cw = ph.sb([128, 6, 34], F32, dma=True, name="cw")
                ut = ph.ring(2, [128, 6, 544], F32, dma=True, name="ut")
                acc = ph.ring(2, [128, 6, 512], F32, name="acc")
                ysq = ph.ring(2, [128, 512], F32, name="ysq")
                mean = ph.sb([128, 512], F32, name="mean")
                msq = ph.sb([128, 512], F32, name="msq")
                rstd = ph.sb([128, 512], F32, name="rstd")
                zt = ph.ring(2, [128, 512], F32, name="zt")
                co = ph.ring(2, [128, 512], BF16, dma=True, name="co")
                sps = ph.pring(2, [128, 512], F32, name="sps")
                k.load('sp', cw, cw.t[:], cpar[l])
                k.acq_r('dve', cw); k.acq_r('act', cw)
                nblk = (oe - os_) // 512
                for b in range(nblk):
                    t0 = os_ + b * 512
                    ub = ut[b % 2]; ab = acc[b % 2]
                    lo = max(hs, t0 - 16); hi = min(he, t0 + 528)
                    fresh = True
                    if lo > t0 - 16 or hi < t0 + 528:
                        k.acq_w('pool', ub)
                        k.set_w(ub, k.sig('pool', nc.gpsimd.memset(ub.t[:], 0.0)))
                        fresh = False
                    for c in range(6):
                        k.load('sp', ub, ub.t[:, c, lo - (t0 - 16):hi - (t0 - 16)], seg.u[c * 128:(c + 1) * 128, lo:hi], fresh=(fresh and c == 0))
                    k.acq_r('dve', ub); k.acq_w('dve', ab)
                    k.acq_w('pe', sps[0]); k.acq_w('pe', sps[1])
                    for c in range(6):
                        nc.vector.tensor_scalar(out=ab.t[:, c, :], in0=ub.t[:, c, 1:513], scalar1=cw.t[:, c, 0:1], scalar2=cw.t[:, c, 31:32], op0=ALU.mult, op1=ALU.add)
                        for j in range(1, 31):
                            ins = nc.vector.scalar_tensor_tensor(out=ab.t[:, c, :], in0=ub.t[:, c, j + 1:j + 513], scalar=cw.t[:, c, j:j + 1], in1=ab.t[:, c, :], op0=ALU.mult, op1=ALU.add)
                        tok = k.sig('dve', ins)
                        k.set_w(ab, tok, fresh=(c == 0))
                        yb = ysq[c % 2]
                        k.wait('act', tok); k.acq_w('act', yb)
                        toka = k.sig('act', nc.scalar.activation(out=yb.t[:], in_=ab.t[:, c, :], func=AF.Square))
                        k.set_w(yb, toka)
                        k.wait('pe', tok)
                        nc.tensor.matmul(sps[0].t[:], lhsT=ones_f.t[:], rhs=ab.t[:, c, :], start=(c == 0), stop=(c == 5))
                        k.wait('pe', toka)
                        tokp = k.sig('pe', nc.tensor.matmul(sps[1].t[:], lhsT=ones_f.t[:], rhs=yb.t[:], start=(c == 0), stop=(c == 5)))
                        k.add_r(yb, tokp)
                    k.add_r(ub, tok)
                    k.set_w(sps[0], tokp); k.set_w(sps[1], tokp)
                    k.add_r(ab, tokp)
                    k.wait('act', tokp); k.acq_w('act', mean)
                    tokm = k.sig('act', nc.scalar.activation(out=mean.t[:], in_=sps[0].t[:], func=AF.Copy, scale=1.0 / D_CONV))
                    k.set_w(mean, tokm); k.add_r(sps[0], tokm)
                    k.wait('dve', tokm); k.wait('dve', tokp)
                    nc.vector.tensor_tensor(out=msq.t[:], in0=mean.t[:], in1=mean.t[:], op=ALU.mult)
                    tok = k.sig('dve', nc.vector.scalar_tensor_tensor(out=msq.t[:], in0=sps[1].t[:], scalar=1.0 / D_CONV, in1=msq.t[:], op0=ALU.mult, op1=ALU.subtract))
                    k.add_r(sps[1], tok)
                    k.wait('act', tok)
                    tok = k.sig('act', nc.scalar.activation(out=rstd.t[:], in_=msq.t[:], func=AF.Sqrt, bias=epsb.t[:, 0:1], scale=1.0))
                    k.wait('dve', tok)
                    nc.vector.reciprocal(out=rstd.t[:], in_=rstd.t[:])
                    for c in range(6):
                        zb = zt[c % 2]; cb = co[c % 2]
                        k.acq_w('dve', zb)
                        nc.vector.tensor_tensor(out=zb.t[:], in0=ab.t[:, c, :], in1=mean.t[:], op=ALU.subtract)
                        tok = k.sig('dve', nc.vector.tensor_tensor(out=zb.t[:], in0=zb.t[:], in1=rstd.t[:], op=ALU.mult))
                        k.set_w(zb, tok)
                        k.acq_r('act', zb); k.acq_w('act', cb)
                        toka = k.sig('act', nc.scalar.activation(out=cb.t[:], in_=zb.t[:], func=AF.Silu, bias=cw.t[:, c, 33:34], scale=cw.t[:, c, 32:33]))
                        k.add_r(zb, toka); k.set_w(cb, toka)
                        k.store('pool', cb, seg.mix[c * 128:(c + 1) * 128, t0:t0 + 512], cb.t[:])
                    k.add_r(ab, tok); k.add_r(mean, tok)
            if stop is not None and stop <= l * 10 + 2:
                continue
            with Phase(k) as ph:
                mk = ph.sb([128, MASK_J], BF16, dma=True, name="mk")
                qt = ph.ring(2, [128, 512], BF16, dma=True, name="qt")
                kt = ph.ring(2, [128, 2560], BF16, dma=True, name="kt")
                vxl = ph.ring(2, [128, 20, 256], BF16, dma=True, name="vxl")
                pe_t = ph.ring(3, [128, 512], BF16, name="pexp")
                pm_t = ph.ring(3, [128, 512], BF16, name="pmsk")
                rd = ph.ring(2, [128, 512], F32, name="rd")
                rd2 = ph.ring(2, [128, 512], F32, name="rd2")
                att = ph.ring(2, [128, 512], BF16, dma=True, name="att")
                sp_ = ph.pring(3, [128, 512], F32, name="sps")
                ap_ = ph.pring(2, [128, 512], F32, name="aps")
                k.load('pool', mk, mk.t[:], maskd[:, :])
                k.acq_r('dve', mk)
                it = 0; sc = 0; ac = 0
                nqb = (oe - os_) // 512
                for qb in range(nqb):
                    q0 = os_ + qb * 512
                    k0 = max(hs, q0 - 1024); k1 = min(he, q0 + 512 + 1024)
                    nkb = (k1 - k0) // 128
                    for hp in range(6):
                        qb_ = qt[it % 2]; kb_ = kt[it % 2]; vb_ = vxl[it % 2]; ab_ = att[it % 2]; it += 1
                        k.load('sp', qb_, qb_.t[:], seg.q[hp * 128:(hp + 1) * 128, q0:q0 + 512])
                        k.load('sp', kb_, kb_.t[:, 0:k1 - k0], seg.kk[hp * 128:(hp + 1) * 128, k0:k1])
                        k.load('sp', vb_, vb_.t[:, 0:nkb, :], seg.v[k0:k1, hp * 256:(hp + 1) * 256].rearrange("(kb p) c -> p kb c", p=128))
                        k.acq_r('pe', qb_); k.acq_r('pe', kb_); k.acq_r('pe', vb_)
                        for hh in range(2):
                            r0 = hh * 64
                            accb = ap_[ac % 2]; ac += 1
                            k.acq_w('pe', accb)
                            pend = []
                            for kbi in range(nkb):
                                d = (k0 + kbi * 128) - q0
                                j0 = MASK_D0 - d
                                sb_ = sp_[sc % 3]; peb = pe_t[sc % 3]; pmb = pm_t[sc % 3]; sc += 1
                                k.acq_w('pe', sb_)
                                tok = k.sig('pe', nc.tensor.matmul(sb_.t[:], lhsT=kb_.t[r0:r0 + 64, kbi * 128:(kbi + 1) * 128], rhs=qb_.t[r0:r0 + 64, :], start=True, stop=True))
                                k.set_w(sb_, tok)
                                k.acq_r('act', sb_); k.acq_w('act', peb)
                                tok = k.sig('act', nc.scalar.activation(out=peb.t[:], in_=sb_.t[:], func=AF.Exp, scale=0.125))
                                k.add_r(sb_, tok); k.set_w(peb, tok)
                                k.acq_r('dve', peb); k.acq_w('dve', pmb)
                                tok = k.sig('dve', nc.vector.tensor_tensor(out=pmb.t[:], in0=peb.t[:], in1=mk.t[:, j0:j0 + 512], op=ALU.mult))
                                k.add_r(peb, tok); k.set_w(pmb, tok)
                                pend.append((pmb, kbi))
                                if len(pend) > 1:
                                    pb2, kb2 = pend.pop(0)
                                    k.acq_r('pe', pb2)
                                    tok = k.sig('pe', nc.tensor.matmul(accb.t[:], lhsT=vb_.t[:, kb2, hh * 128:(hh + 1) * 128], rhs=pb2.t[:], start=(kb2 == 0), stop=False))
                                    k.add_r(pb2, tok)
                            pb2, kb2 = pend.pop(0)
                            k.acq_r('pe', pb2)
                            tok = k.sig('pe', nc.tensor.matmul(accb.t[:], lhsT=vb_.t[:, kb2, hh * 128:(hh + 1) * 128], rhs=pb2.t[:], start=(kb2 == 0), stop=True))
                            k.add_r(pb2, tok); k.set_w(accb, tok)
                            nr = r0; dr = 64 - r0
                            rdb = rd[hh]; rd2b = rd2[hh]
                            k.acq_r('dve', accb); k.acq_w('dve', rdb)
                            tok = k.sig('dve', nc.vector.reciprocal(out=rdb.t[dr:dr + 64, :], in_=accb.t[dr:dr + 64, :]))
                            k.set_w(rdb, tok)
                            k.acq_r('act', rdb); k.acq_w('act', rd2b)
                            tok = k.sig('act', nc.scalar.activation(out=rd2b.t[nr:nr + 64, :], in_=rdb.t[dr:dr + 64, :], func=AF.Copy))
                            k.add_r(rdb, tok); k.set_w(rd2b, tok)
                            k.acq_r('dve', rd2b)
                            if hh == 0:
                                k.acq_w('dve', ab_)
                            tok = k.sig('dve', nc.vector.tensor_tensor(out=ab_.t[nr:nr + 64, :], in0=accb.t[nr:nr + 64, :], in1=rd2b.t[nr:nr + 64, :], op=ALU.mult))
                            k.add_r(accb, tok); k.add_r(rd2b, tok); k.set_w(ab_, tok, fresh=(hh == 0))
                        k.add_r(qb_, tok); k.add_r(kb_, tok); k.add_r(vb_, tok)
                        k.store('pool', ab_, seg.mix[(6 + hp) * 128:(7 + hp) * 128, q0:q0 + 512], ab_.t[:])
            if stop is not None and stop <= l * 10 + 3:
                continue
            with Phase(k) as ph:
                mb = ph.sb([128, 2, 2048], BF16, dma=True, name="mb")
                memT = ph.sb([128, 16, 256], BF16, name="memT")
                wkv = ph.sb([128, 8, 2048], BF16, dma=True, name="wkv")
                kmT = ph.sb([128, 4, 256], BF16, name="kmT")
                vm = ph.sb([128, 2, 512], BF16, name="vm")
                qmt = ph.ring(2, [128, 4, 512], BF16, dma=True, name="qmt")
                pe_t = ph.ring(3, [128, 512], BF16, name="pexp")
                rd = ph.ring(2, [128, 512], F32, name="rd")
                mo = ph.ring(2, [128, 512], BF16, dma=True, name="mo")
                tpm = ph.pring(2, [128, 4, 256], BF16, name="tpm")
                sp_ = ph.pring(2, [128, 512], F32, name="sps")
                np_ = ph.pring(2, [128, 512], F32, name="nps")
                dp_ = ph.pring(2, [128, 512], F32, name="dps")
                k.load('pool', mb, mb.t[:], seg.mem.rearrange("(t p) d -> p t d", p=128))
                for m in range(8):
                    k.load('sp', wkv, wkv.t[:, m, :], wkv_b[l][m], fresh=(m == 0))
                k.acq_r('pe', mb); k.acq_r('pe', wkv)
                for kg in range(4):
                    tp = tpm[kg % 2]
                    k.acq_w('pe', tp)
                    for kk in range(4):
                        for t in range(2):
                            ins = nc.tensor.transpose(tp.t[:, kk, t * 128:(t + 1) * 128], mb.t[:, t, (kg * 4 + kk) * 128:(kg * 4 + kk + 1) * 128], ident_b.t[:])
                    k.set_w(tp, k.sig('pe', ins))
                    k.acq_r('act', tp)
                    tok = k.sig('act', nc.scalar.activation(out=memT.t[:, kg * 4:(kg + 1) * 4, :], in_=tp.t[:], func=AF.Copy))
                    k.add_r(tp, tok); k.set_w(memT, tok, fresh=(kg == 0))
                k.acq_r('pe', memT)
                for mh in range(4):
                    pb = sp_[mh % 2]
                    k.acq_w('pe', pb)
                    for kk in range(16):
                        ins = nc.tensor.matmul(pb.t[:, 0:256], lhsT=wkv.t[:, mh, kk * 128:(kk + 1) * 128], rhs=memT.t[:, kk, :], start=(kk == 0), stop=(kk == 15))
                    k.set_w(pb, k.sig('pe', ins))
                    k.acq_r('act', pb)
                    tok = k.sig('act', nc.scalar.activation(out=kmT.t[:, mh, :], in_=pb.t[:, 0:256], func=AF.Copy))
                    k.add_r(pb, tok); k.set_w(kmT, tok, fresh=(mh == 0))
                i = 0
                for t in range(2):
                    for mh in range(4):
                        pb = np_[i % 2]; i += 1
                        k.acq_w('pe', pb)
                        for kk in range(16):
                            ins = nc.tensor.matmul(pb.t[:, 0:128], lhsT=memT.t[:, kk, t * 128:(t + 1) * 128], rhs=wkv.t[:, 4 + mh, kk * 128:(kk + 1) * 128], start=(kk == 0), stop=(kk == 15))
                        k.set_w(pb, k.sig('pe', ins))
                        k.acq_r('dve', pb)
                        tok = k.sig('dve', nc.vector.tensor_copy(out=vm.t[:, t, mh * 128:(mh + 1) * 128], in_=pb.t[:, 0:128]))
                        k.add_r(pb, tok); k.set_w(vm, tok, fresh=(i == 1))
                k.acq_r('pe', kmT); k.acq_r('pe', vm)
                nqb = (oe - os_) // 512
                sc = 0; it = 0
                for qb in range(nqb):
                    q0 = os_ + qb * 512
                    qb_ = qmt[qb % 2]
                    k.load('sp', qb_, qb_.t[:], seg.qm[:, q0:q0 + 512].rearrange("(h p) t -> p h t", p=128))
                    k.acq_r('pe', qb_)
                    for mh in range(4):
                        nb_ = np_[it % 2]; db_ = dp_[it % 2]; rdb = rd[it % 2]; ob = mo[it % 2]; it += 1
                        k.acq_w('pe', nb_); k.acq_w('pe', db_)
                        for t in range(2):
                            sb_ = sp_[sc % 2]; peb = pe_t[sc % 3]; sc += 1
                            k.acq_w('pe', sb_)
                            tok = k.sig('pe', nc.tensor.matmul(sb_.t[:], lhsT=kmT.t[:, mh, t * 128:(t + 1) * 128], rhs=qb_.t[:, mh, :], start=True, stop=True))
                            k.set_w(sb_, tok)
                            k.acq_r('act', sb_); k.acq_w('act', peb)
                            tok = k.sig('act', nc.scalar.activation(out=peb.t[:], in_=sb_.t[:], func=AF.Exp, scale=float(128 ** -0.5)))
                            k.add_r(sb_, tok); k.set_w(peb, tok)
                            k.acq_r('pe', peb)
                            nc.tensor.matmul(nb_.t[:], lhsT=vm.t[:, t, mh * 128:(mh + 1) * 128], rhs=peb.t[:], start=(t == 0), stop=(t == 1))
                            tok = k.sig('pe', nc.tensor.matmul(db_.t[:], lhsT=ones_b.t[:], rhs=peb.t[:], start=(t == 0), stop=(t == 1)))
                            k.add_r(peb, tok)
                        k.set_w(nb_, tok); k.set_w(db_, tok)
                        k.acq_r('dve', db_); k.acq_w('dve', rdb)
                        k.wait('dve', k.sig('dve', nc.vector.reciprocal(out=rdb.t[:], in_=db_.t[:])), force=True)
                        k.acq_w('dve', ob)
                        tok = k.sig('dve', nc.vector.tensor_tensor(out=ob.t[:], in0=nb_.t[:], in1=rdb.t[:], op=ALU.mult))
                        k.add_r(nb_, tok); k.add_r(db_, tok); k.set_w(ob, tok)
                        k.store('pool', ob, seg.mix[(12 + mh) * 128:(13 + mh) * 128, q0:q0 + 512], ob.t[:])
                    k.add_r(qb_, tok)
            if stop is not None and stop <= l * 10 + 4:
                continue
            with Phase(k) as ph:
                wo = ph.sb([128, 16, 2048], BF16, dma=True, name="wo")
                gb = ph.sb([128, 2, 2048], F32, dma=True, name="gb")
                mt = ph.ring(2, [128, 16, 512], BF16, dma=True, name="mixT")
                xr = ph.ring(2, [128, 2048], F32, dma=True, name="xr")
                yt = ph.ring(2, [128, 2048], F32, dma=True, name="yt")
                st = ph.sb([128, 4, 6], F32); mv = ph.sb([128, 2], F32); rs = ph.sb([128, 1], F32); nb = ph.sb([128, 1], F32)
                ops = ph.pring(8, [128, 512], F32, name="ops")
                k.load('sp', wo, wo.t[:], wout_b[l].rearrange("(k p) n -> p k n", p=128))
                k.load('sp', gb, gb.t[:, 0, :], ln1_g[l].partition_broadcast(128))
                k.load('sp', gb, gb.t[:, 1, :], ln1_b[l].partition_broadcast(128), fresh=False)
                k.acq_r('pe', wo); k.acq_r('dve', gb)
                nblk = (oe - os_) // 512
                oc = 0; ti = 0
                for b in range(nblk):
                    t0 = os_ + b * 512
                    mb_ = mt[b % 2]
                    k.load('sp', mb_, mb_.t[:], seg.mix[:, t0:t0 + 512].rearrange("(k p) t -> p k t", p=128))
                    k.acq_r('pe', mb_)
                    for t in range(4):
                        xb_ = xr[ti % 2]; yb = yt[ti % 2]; ti += 1
                        r0 = t0 + t * 128
                        k.load('sp', xb_, xb_.t[:], xin[r0:r0 + 128, :])
                        pbs = []
                        for ch in range(4):
                            pb = ops[oc % 8]; oc += 1
                            k.acq_w('pe', pb)
                            for kk in range(16):
                                ins = nc.tensor.matmul(pb.t[:], lhsT=mb_.t[:, kk, t * 128:(t + 1) * 128], rhs=wo.t[:, kk, ch * 512:(ch + 1) * 512], start=(kk == 0), stop=(kk == 15))
                            tokp = k.sig('pe', ins)
                            k.set_w(pb, tokp)
                            pbs.append(pb)
                        k.acq_r('dve', xb_); k.acq_w('dve', yb)
                        for ch in range(4):
                            k.acq_r('dve', pbs[ch])
                            tok = k.sig('dve', nc.vector.scalar_tensor_tensor(out=yb.t[:, ch * 512:(ch + 1) * 512], in0=xb_.t[:, ch * 512:(ch + 1) * 512], scalar=float(ALPHA), in1=pbs[ch].t[:], op0=ALU.mult, op1=ALU.add))
                            k.add_r(pbs[ch], tok)
                        k.add_r(xb_, tok)
                        tok = ln_inplace(yb.t[:], gb, (st, mv, rs, nb))
                        k.set_w(yb, tok)
                        k.store('pool', yb, seg.x1[r0:r0 + 128, :], yb.t[:])
                    k.add_r(mb_, tokp)
            if stop is not None and stop <= l * 10 + 5:
                continue
            is_moe = (l % 2 == 1)
            if is_moe:
                with Phase(k) as ph:
                    rt = ph.sb([128, 16, NE], F32, dma=True, name="rt")
                    xr = ph.ring(2, [128, 2048], F32, dma=True, name="xr")
                    xT32 = ph.ring(2, [128, 16, 128], F32, name="xT32")
                    lg = ph.ring(2, [128, NE], F32, name="lg")
                    m8 = ph.sb([128, 8], F32); nv1 = ph.sb([128, 1], F32); ex = ph.sb([128, 8], F32); msk = ph.sb([128, 8], F32)
                    den = ph.sb([128, 1], F32); rden = ph.sb([128, 1], F32)
                    cb = ph.ring(2, [128, NE], F32, dma=True, name="cb")
                    tps = ph.pring(4, [128, 4, 128], F32, name="tps")
                    lps = ph.pring(2, [128, NE], F32, name="lps")
                    k.load('sp', rt, rt.t[:], router[0].rearrange("(k p) e -> p k e", p=128))
                    k.acq_r('pe', rt)
                    ntile = (oe - os_) // 128
                    tc_ = 0
                    for ti in range(ntile):
                        r0 = os_ + ti * 128
                        xb_ = xr[ti % 2]; xtb = xT32[ti % 2]; lgb = lg[ti % 2]; cbb = cb[ti % 2]; lp = lps[ti % 2]
                        k.load('sp', xb_, xb_.t[:], seg.x1[r0:r0 + 128, :])
                        k.acq_r('pe', xb_)
                        for kg in range(4):
                            tp = tps[tc_ % 4]; tc_ += 1
                            k.acq_w('pe', tp)
                            for kk in range(4):
                                ins = nc.tensor.transpose(tp.t[:, kk, :], xb_.t[:, (kg * 4 + kk) * 128:(kg * 4 + kk + 1) * 128], ident_f.t[:])
                            tokp = k.sig('pe', ins)
                            k.set_w(tp, tokp)
                            e = 'act' if kg % 2 == 0 else 'dve'
                            k.acq_r(e, tp); k.acq_w(e, xtb)
                            if e == 'act':
                                ins = nc.scalar.activation(out=xtb.t[:, kg * 4:(kg + 1) * 4, :], in_=tp.t[:], func=AF.Copy)
                            else:
                                ins = nc.vector.tensor_copy(out=xtb.t[:, kg * 4:(kg + 1) * 4, :], in_=tp.t[:])
                            tok = k.sig(e, ins)
                            k.add_r(tp, tok); k.set_w(xtb, tok, fresh=(kg == 0))
                        k.add_r(xb_, tokp)
                        k.acq_r('pe', xtb); k.acq_w('pe', lp)
                        for kk in range(16):
                            ins = nc.tensor.matmul(lp.t[:], lhsT=xtb.t[:, kk, :], rhs=rt.t[:, kk, :], start=(kk == 0), stop=(kk == 15))
                        tokp = k.sig('pe', ins)
                        k.set_w(lp, tokp); k.add_r(xtb, tokp)
                        k.acq_r('dve', lp); k.acq_w('dve', lgb); k.acq_w('dve', cbb)
                        k.wait('dve', k.sig('dve', nc.vector.tensor_copy(out=lgb.t[:], in_=lp.t[:])), force=True)
                        tok = k.sig('dve', nc.vector.max(out=m8.t[:], in_=lgb.t[:]))
                        k.wait('dve', tok, force=True)
                        nc.vector.tensor_scalar(out=msk.t[:], in0=lgb.t[:], scalar1=m8.t[:, 1:2], scalar2=None, op0=ALU.is_ge)
                        tok = k.sig('dve', nc.vector.tensor_scalar(out=nv1.t[:], in0=m8.t[:, 0:1], scalar1=-1.0, scalar2=None, op0=ALU.mult))
                        k.add_r(lp, tok)
                        k.wait('act', tok)
                        toka = k.sig('act', nc.scalar.activation(out=ex.t[:], in_=lgb.t[:], func=AF.Exp, bias=nv1.t[:, 0:1], scale=1.0))
                        k.wait('dve', toka)
                        k.wait('dve', k.sig('dve', nc.vector.tensor_tensor(out=ex.t[:], in0=ex.t[:], in1=msk.t[:], op=ALU.mult)), force=True)
                        k.wait('dve', k.sig('dve', nc.vector.tensor_reduce(out=den.t[:], in_=ex.t[:], axis=mybir.AxisListType.X, op=ALU.add)), force=True)
                        tok = k.sig('dve', nc.vector.reciprocal(out=rden.t[:], in_=den.t[:]))
                        k.wait('dve', tok, force=True)
                        tok = k.sig('dve', nc.vector.tensor_scalar(out=cbb.t[:], in0=ex.t[:], scalar1=rden.t[:, 0:1], scalar2=None, op0=ALU.mult))
                        k.set_w(cbb, tok); k.set_w(lgb, tok)
                        k.store('pool', cbb, seg.comb[r0:r0 + 128, :], cbb.t[:])
            last = (l == DEPTH - 1)
            if is_moe:
                continue
            with Phase(k) as ph:
                nexp = NE if is_moe else 1
                NF = (E_FF if is_moe else D_FF) // 128
                FG = 4
                xb = ph.sb([128, 4, 2048], BF16, dma=True, name="xb")
                xT = ph.sb([128, 16, 512], BF16, name="xT")
                gT = ph.sb([128, NF, 512], BF16, name="gT")
                accs = ph.sb([128, 4, 2048], F32, dma=True, name="acc")
                w13 = ph.ring(2, [128, 2, 2048], BF16, dma=True, name="w13")
                w2r = ph.ring(3, [128, FG, 512], BF16, dma=True, name="w2r")
                xr = ph.sb([128, 2048], F32, dma=True, name="xr")
                gb = ph.sb([128, 2, 2048], F32, dma=True, name="gb")
                stt = ph.ring(2, [128, 512], F32, name="silu")
                cbt = ph.sb([128, 4, NE], F32, dma=True, name="cbt")
                vt = ph.sb([128, W // 128], F32, dma=True, name="vt")
                st = ph.sb([128, 4, 6], F32); mv = ph.sb([128, 2], F32); rs = ph.sb([128, 1], F32); nb = ph.sb([128, 1], F32)
                tpr = ph.pring(1, [128, 4, 512], BF16, name="tp")
                hps = ph.pring(2, [128, 512], F32, name="hps")
                ops = ph.pring(4, [128, 512], F32, name="ops")
                k.load('sp', vt, vt.t[:], seg.valid[:, :])
                k.load('sp', gb, gb.t[:, 0, :], (ln2_g if True else ln1_g)[l].partition_broadcast(128))
                k.load('sp', gb, gb.t[:, 1, :], ln2_b[l].partition_broadcast(128), fresh=False)
                k.acq_r('dve', gb); k.acq_r('dve', vt)
                nblk = (oe - os_) // 512
                cnt = [0]
                wc = 0; w2c = 0; hc = 0
                for b in range(nblk):
                    t0 = os_ + b * 512
                    load_xT(ph, seg.x1[t0:t0 + 512, :], xb, xT, tpr, cnt)
                    if is_moe:
                        k.load('sp', cbt, cbt.t[:], seg.comb[t0:t0 + 512, :].rearrange("(t p) e -> p t e", p=128))
                        k.acq_r('dve', cbt)
                    for e in range(nexp):
                        w1s = f1_b; w3s = f3_b; w2s = f2_b
                        for f in range(NF):
                            wb = w13[wc % 2]; wc += 1
                            k.load('sp', wb, wb.t[:, 0, :], w1s[f])
                            k.load('sp', wb, wb.t[:, 1, :], w3s[f], fresh=False)
                            k.acq_r('pe', wb); k.acq_r('pe', xT)
                            h1 = hps[0]; h3 = hps[1]
                            k.acq_w('pe', h1)
                            for kk in range(16):
                                ins = nc.tensor.matmul(h1.t[:], lhsT=wb.t[:, 0, kk * 128:(kk + 1) * 128], rhs=xT.t[:, kk, :], start=(kk == 0), stop=(kk == 15))
                            k.set_w(h1, k.sig('pe', ins))
                            k.acq_w('pe', h3)
                            for kk in range(16):
                                ins = nc.tensor.matmul(h3.t[:], lhsT=wb.t[:, 1, kk * 128:(kk + 1) * 128], rhs=xT.t[:, kk, :], start=(kk == 0), stop=(kk == 15))
                            tokp = k.sig('pe', ins)
                            k.set_w(h3, tokp); k.add_r(wb, tokp)
                            sb_ = stt[hc % 2]; hc += 1
                            k.acq_r('act', h1); k.acq_w('act', sb_)
                            tok = k.sig('act', nc.scalar.activation(out=sb_.t[:], in_=h1.t[:], func=AF.Silu))
                            k.add_r(h1, tok); k.set_w(sb_, tok)
                            k.acq_r('dve', sb_); k.acq_r('dve', h3)
                            if f == 0:
                                k.acq_w('dve', gT)
                            tok = k.sig('dve', nc.vector.tensor_tensor(out=gT.t[:, f, :], in0=sb_.t[:], in1=h3.t[:], op=ALU.mult))
                            k.add_r(sb_, tok); k.add_r(h3, tok); k.set_w(gT, tok, fresh=(f == 0))
                        if e == nexp - 1:
                            k.add_r(xT, tokp)
                        k.acq_r('pe', gT)
                        for dch in range(4):
                            for t in range(4):
                                k.acq_w('pe', ops[t])
                            for fg in range(NF // FG):
                                wb = w2r[w2c % 3]; w2c += 1
                                k.load('sp', wb, wb.t[:], w2s[fg * FG * 128:(fg + 1) * FG * 128, dch * 512:(dch + 1) * 512].rearrange("(f p) d -> p f d", p=128))
                                k.acq_r('pe', wb)
                                for fi in range(FG):
                                    f = fg * FG + fi
                                    for t in range(4):
                                        ins = nc.tensor.matmul(ops[t].t[:], lhsT=gT.t[:, f, t * 128:(t + 1) * 128], rhs=wb.t[:, fi, :], start=(f == 0), stop=(f == NF - 1))
                                tokp = k.sig('pe', ins)
                                k.add_r(wb, tokp)
                            for t in range(4):
                                k.set_w(ops[t], tokp)
                            if e == 0 and dch == 0:
                                k.acq_w('dve', accs)
                            for t in range(4):
                                k.acq_r('dve', ops[t])
                                dst = accs.t[:, t, dch * 512:(dch + 1) * 512]
                                if not is_moe:
                                    ins = nc.vector.tensor_copy(out=dst, in_=ops[t].t[:])
                                elif e == 0:
                                    ins = nc.vector.tensor_scalar(out=dst, in0=ops[t].t[:], scalar1=cbt.t[:, t, e:e + 1], scalar2=None, op0=ALU.mult)
                                else:
                                    ins = nc.vector.scalar_tensor_tensor(out=dst, in0=ops[t].t[:], scalar=cbt.t[:, t, e:e + 1], in1=dst, op0=ALU.mult, op1=ALU.add)
                                tok = k.sig('dve', ins)
                                k.add_r(ops[t], tok)
                            k.set_w(accs, tok, fresh=(e == 0 and dch == 0))
                        k.add_r(gT, tokp)
                    if is_moe:
                        k.add_r(cbt, tok)
                    for t in range(4):
                        r0 = t0 + t * 128
                        k.load('sp', xr, xr.t[:], seg.x1[r0:r0 + 128, :])
                        k.acq_r('dve', xr)
                        yt = accs.t[:, t, :]
                        tok = k.sig('dve', nc.vector.scalar_tensor_tensor(out=yt, in0=xr.t[:], scalar=float(ALPHA), in1=yt, op0=ALU.mult, op1=ALU.add))
                        k.add_r(xr, tok)
                        va = None if last else vt.t[:, r0 // 128:r0 // 128 + 1]
                        tok = ln_inplace(yt, gb, (st, mv, rs, nb), valid_ap=va)
                        k.set_w(accs, tok, fresh=False)
                        if last:
                            if os_ <= r0 < oe:
                                k.store('pool', accs, seg.yout[r0 - seg.yoff:r0 - seg.yoff + 128, :], yt)
                        else:
                            k.store('pool', accs, seg.x2[r0:r0 + 128, :], yt)

        if l % 2 == 1 and (stop is None or stop > l * 10 + 5):
            I32 = mybir.dt.int32
            AX = mybir.AxisListType.X
            tiles = []
            for seg in segs:
                o0, o1 = seg.orr[l]
                for r0 in range(o0, o1, 128):
                    tiles.append((seg, r0))
            NT = len(tiles)
            NB = NT // 2 + NE
            NROW = NB * 512
            xsort = dscr(f"xsort{l}", [NROW + 128, D], BF16)
            ysort = dscr(f"ysort{l}", [NROW + 128, D], F32)
            outer = ExitStack()

            def osb(shape, dt, name):
                k.uid += 1
                return Buf(outer.enter_context(nc.sbuf_tensor(f"{name}{k.uid}", list(shape), dt)))

            IDX = osb([128, NT * 2], I32, "IDX")
            G12 = osb([128, NT, 2], F32, "G12")
            WI13 = osb([128, NB], I32, "WI13")
            WI2 = osb([128, NB * 4], I32, "WI2")

            def fw(ins):
                tok = k.sig('dve', ins)
                k.wait('dve', tok, force=True)
                return tok

            with Phase(k) as ph:
                U = ph.sb([128, 128], F32, dma=True, name="U")
                iot = ph.sb([128, 1], F32, dma=True, name="iot")
                CB = ph.sb([128, NT, 8], F32, dma=True, name="CB")
                Mt = ph.sb([128, NT, 8], F32, name="Mt")
                RK = ph.sb([128, NT, 8], F32, name="RK")
                carry = ph.sb([128, 8], F32); cmpn = ph.sb([128, 8, 12], F32); pe_ = ph.sb([128, 8], F32)
                o1_ = ph.sb([128, 8], F32); end_ = ph.sb([128, 8], F32)
                cmpE = ph.sb([128, NB, 8], F32); EJ = ph.sb([128, NB], F32); wf = ph.sb([128, NB], F32)
                key = ph.sb([128, 8], F32); m8 = ph.sb([128, 8], F32); eq = ph.sb([128, 8], F32)
                rps = ph.pring(2, [128, 8], F32, name="rps"); cps = ph.pring(2, [128, 8], F32, name="cps")
                k.load('sp', U, U.t[:], triud[:, :])
                k.load('sp', iot, iot.t[:], iotad[:, :])
                i = 0
                for seg in segs:
                    o0, o1 = seg.orr[l]
                    n = (o1 - o0) // 128
                    k.load('sp', CB, CB.t[:, i:i + n, :], seg.comb[o0:o1, :].rearrange("(t p) e -> p t e", p=128), fresh=(i == 0))
                    i += n
                k.acq_r('dve', CB); k.acq_r('dve', iot); k.acq_r('pe', U)
                tokM = fw(nc.vector.tensor_scalar(out=Mt.t[:], in0=CB.t[:], scalar1=0.0, scalar2=None, op0=ALU.is_gt))
                k.wait('pe', tokM)
                fw(nc.vector.memset(carry.t[:], 0.0))
                for i in range(NT):
                    rp = rps[i % 2]; cp = cps[i % 2]
                    k.acq_w('pe', rp); k.acq_w('pe', cp)
                    nc.tensor.matmul(rp.t[:], lhsT=U.t[:], rhs=Mt.t[:, i, :], start=True, stop=True)
                    tok = k.sig('pe', nc.tensor.matmul(cp.t[:], lhsT=ones_f.t[:], rhs=Mt.t[:, i, :], start=True, stop=True))
                    k.set_w(rp, tok); k.set_w(cp, tok)
                    k.acq_r('dve', rp)
                    nc.vector.tensor_tensor(out=RK.t[:, i, :], in0=rp.t[:], in1=carry.t[:], op=ALU.add)
                    tok = fw(nc.vector.tensor_tensor(out=carry.t[:], in0=cp.t[:], in1=carry.t[:], op=ALU.add))
                    k.add_r(rp, tok); k.add_r(cp, tok)
                for j in range(12):
                    ins = nc.vector.tensor_scalar(out=cmpn.t[:, :, j], in0=carry.t[:], scalar1=float(512 * j), scalar2=None, op0=ALU.is_gt)
                fw(ins)
                fw(nc.vector.tensor_reduce(out=pe_.t[:], in_=cmpn.t[:], axis=AX, op=ALU.add))
                fw(nc.vector.tensor_scalar(out=pe_.t[:], in0=pe_.t[:], scalar1=512.0, scalar2=None, op0=ALU.mult))
                fw(nc.vector.tensor_copy(out=end_.t[:, 0:1], in_=pe_.t[:, 0:1]))
                for e in range(1, NE):
                    fw(nc.vector.tensor_tensor(out=end_.t[:, e:e + 1], in0=end_.t[:, e - 1:e], in1=pe_.t[:, e:e + 1], op=ALU.add))
                fw(nc.vector.tensor_tensor(out=o1_.t[:], in0=end_.t[:], in1=pe_.t[:], op=ALU.subtract))
                fw(nc.vector.tensor_scalar(out=o1_.t[:], in0=o1_.t[:], scalar1=1.0, scalar2=None, op0=ALU.add))
                for j in range(NB):
                    ins = nc.vector.tensor_scalar(out=cmpE.t[:, j, :], in0=end_.t[:], scalar1=float(512 * j), scalar2=None, op0=ALU.is_le)
                fw(ins)
                fw(nc.vector.tensor_reduce(out=EJ.t[:], in_=cmpE.t[:], axis=AX, op=ALU.add))
                fw(nc.vector.tensor_scalar(out=EJ.t[:], in0=EJ.t[:], scalar1=float(NE - 1), scalar2=None, op0=ALU.min))
                fw(nc.vector.tensor_scalar(out=wf.t[:], in0=EJ.t[:], scalar1=float(NFH * 128), scalar2=iot.t[:, 0:1], op0=ALU.mult, op1=ALU.add))
                fw(nc.vector.tensor_copy(out=WI13.t[:], in_=wf.t[:]))
                for dch in range(4):
                    fw(nc.vector.tensor_scalar(out=wf.t[:], in0=EJ.t[:], scalar1=float(4 * NFG * 128), scalar2=float(dch * NFG * 128), op0=ALU.mult, op1=ALU.add))
                    fw(nc.vector.tensor_scalar(out=wf.t[:], in0=wf.t[:], scalar1=iot.t[:, 0:1], scalar2=None, op0=ALU.add))
                    fw(nc.vector.tensor_copy(out=WI2.t[:].rearrange("p (b d) -> p b d", d=4)[:, :, dch], in_=wf.t[:]))
                for i in range(NT):
                    fw(nc.vector.tensor_tensor(out=key.t[:], in0=RK.t[:, i, :], in1=o1_.t[:], op=ALU.add))
                    fw(nc.vector.tensor_tensor(out=key.t[:], in0=key.t[:], in1=Mt.t[:, i, :], op=ALU.mult))
                    fw(nc.vector.tensor_scalar(out=key.t[:], in0=key.t[:], scalar1=-1.0, scalar2=None, op0=ALU.add))
                    fw(nc.vector.max(out=m8.t[:], in_=key.t[:]))
                    fw(nc.vector.tensor_scalar(out=eq.t[:, 0:2], in0=m8.t[:, 0:2], scalar1=0.0, scalar2=float(NROW + 1), op0=ALU.is_lt, op1=ALU.mult))
                    fw(nc.vector.tensor_tensor(out=eq.t[:, 0:2], in0=eq.t[:, 0:2], in1=m8.t[:, 0:2], op=ALU.add))
                    fw(nc.vector.tensor_copy(out=IDX.t[:, 2 * i:2 * i + 2], in_=eq.t[:, 0:2]))
                    for c in range(2):
                        fw(nc.vector.tensor_scalar(out=eq.t[:], in0=key.t[:], scalar1=m8.t[:, c:c + 1], scalar2=None, op0=ALU.is_equal))
                        fw(nc.vector.tensor_tensor(out=eq.t[:], in0=eq.t[:], in1=CB.t[:, i, :], op=ALU.mult))
                        fw(nc.vector.tensor_reduce(out=G12.t[:, i, c:c + 1], in_=eq.t[:], axis=AX, op=ALU.add))
            with Phase(k) as ph:
                xr = ph.ring(3, [128, 2048], BF16, dma=True, name="xsc")
                for i, (seg, r0) in enumerate(tiles):
                    b = xr[i % 3]
                    k.load('pool', b, b.t[:], seg.x1[r0:r0 + 128, :])
                    k.acq_r('pool', b)
                    for c in range(2):
                        ins = nc.gpsimd.indirect_dma_start(out=xsort[:, :], out_offset=bass.IndirectOffsetOnAxis(ap=IDX.t[:, 2 * i + c:2 * i + c + 1], axis=0),
                                                           in_=b.t[:], in_offset=None)
                        ins.then_inc(b.sem.h, 16)
                        b.sem.n += 16
                        tok = (b.sem, b.sem.n, None)
                        k.add_r(b, tok)
                        k.pending.append(('pool', tok))
            with Phase(k) as ph:
                xb = ph.sb([128, 4, 2048], BF16, dma=True, name="xb")
                xT = ph.sb([128, 16, 512], BF16, name="xT")
                gT = ph.sb([128, NFE, 512], BF16, name="gT")
                w13 = ph.ring(3, [128, 4096], BF16, dma=True, name="w13")
                w2r = ph.ring(4, [128, FG, 512], BF16, dma=True, name="w2r")
                yst = ph.ring(2, [128, 4, 512], F32, dma=True, name="yst")
                stt = ph.ring(2, [128, 512], F32, name="silu")
                tpr = ph.pring(1, [128, 4, 512], BF16, name="tp")
                hps = ph.pring(2, [128, 512], F32, name="hps")
                ops = ph.pring(4, [128, 512], F32, name="ops")
                cnt = [0]; wc = 0; w2c = 0; hc = 0; yc = 0
                for j in range(NB):
                    load_xT(ph, xsort[j * 512:(j + 1) * 512, :], xb, xT, tpr, cnt)
                    for f in range(NFE):
                        wb = w13[wc % 3]; wc += 1
                        k.iload(wb, wb.t[:], m13_h[f // NFH][:, :], WI13.t[:, j:j + 1], elem_off=(f % NFH) * 128 * 4096)
                        k.acq_r('pe', wb); k.acq_r('pe', xT)
                        h1 = hps[0]; h3 = hps[1]
                        k.acq_w('pe', h1)
                        for kk in range(16):
                            ins = nc.tensor.matmul(h1.t[:], lhsT=wb.t[:, kk * 128:(kk + 1) * 128], rhs=xT.t[:, kk, :], start=(kk == 0), stop=(kk == 15))
                        k.set_w(h1, k.sig('pe', ins))
                        k.acq_w('pe', h3)
                        for kk in range(16):
                            ins = nc.tensor.matmul(h3.t[:], lhsT=wb.t[:, 2048 + kk * 128:2048 + (kk + 1) * 128], rhs=xT.t[:, kk, :], start=(kk == 0), stop=(kk == 15))
                        tokp = k.sig('pe', ins)
                        k.set_w(h3, tokp); k.add_r(wb, tokp)
                        sb_ = stt[hc % 2]; hc += 1
                        k.acq_r('act', h1); k.acq_w('act', sb_)
                        tok = k.sig('act', nc.scalar.activation(out=sb_.t[:], in_=h1.t[:], func=AF.Silu))
                        k.add_r(h1, tok); k.set_w(sb_, tok)
                        k.acq_r('dve', sb_); k.acq_r('dve', h3)
                        if f == 0:
                            k.acq_w('dve', gT)
                        tok = k.sig('dve', nc.vector.tensor_tensor(out=gT.t[:, f, :], in0=sb_.t[:], in1=h3.t[:], op=ALU.mult))
                        k.add_r(sb_, tok); k.add_r(h3, tok); k.set_w(gT, tok, fresh=(f == 0))
                    k.add_r(xT, tokp)
                    k.acq_r('pe', gT)
                    for dch in range(4):
                        for t in range(4):
                            k.acq_w('pe', ops[t])
                        for fg in range(NFG):
                            wb = w2r[w2c % 4]; w2c += 1
                            k.iload(wb, wb.t[:].rearrange("p f c -> p (f c)"), m2_b[:, :], WI2.t[:, 4 * j + dch:4 * j + dch + 1], elem_off=fg * 128 * 2048)
                            k.acq_r('pe', wb)
                            for fi in range(FG):
                                f = fg * FG + fi
                                for t in range(4):
                                    ins = nc.tensor.matmul(ops[t].t[:], lhsT=gT.t[:, f, t * 128:(t + 1) * 128], rhs=wb.t[:, fi, :], start=(f == 0), stop=(f == NFE - 1))
                            tokp = k.sig('pe', ins)
                            k.add_r(wb, tokp)
                        ys = yst[yc % 2]; yc += 1
                        for t in range(4):
                            k.set_w(ops[t], tokp)
                        for t in range(4):
                            e_ = 'act' if t % 2 == 0 else 'dve'
                            k.acq_r(e_, ops[t]); k.acq_w(e_, ys)
                            if e_ == 'act':
                                ins = nc.scalar.activation(out=ys.t[:, t, :], in_=ops[t].t[:], func=AF.Copy)
                            else:
                                ins = nc.vector.tensor_copy(out=ys.t[:, t, :], in_=ops[t].t[:])
                            tok = k.sig(e_, ins)
                            k.add_r(ops[t], tok); k.set_w(ys, tok, fresh=(t == 0))
                        k.store('sp', ys, ysort[j * 512:(j + 1) * 512, dch * 512:(dch + 1) * 512].rearrange("(t p) c -> p t c", p=128), ys.t[:])
                    k.add_r(gT, tokp)
            with Phase(k) as ph:
                gb = ph.sb([128, 2, 2048], F32, dma=True, name="gb")
                Y1 = ph.ring(2, [128, 2048], F32, dma=True, name="Y1")
                Y2 = ph.ring(2, [128, 2048], F32, dma=True, name="Y2")
                xr = ph.ring(2, [128, 2048], F32, dma=True, name="xr")
                st = ph.sb([128, 4, 6], F32); mv = ph.sb([128, 2], F32); rs = ph.sb([128, 1], F32); nb = ph.sb([128, 1], F32)
                k.load('sp', gb, gb.t[:, 0, :], ln2_g[l].partition_broadcast(128))
                k.load('sp', gb, gb.t[:, 1, :], ln2_b[l].partition_broadcast(128), fresh=False)
                k.acq_r('dve', gb)
                zt_ = Y1[0]
                k.set_w(zt_, k.sig('dve', nc.vector.memset(zt_.t[:], 0.0)))
                ztok = k.store('sp', zt_, ysort[NROW:NROW + 128, :], zt_.t[:])
                k.wait('pool', ztok)
                for i, (seg, r0) in enumerate(tiles):
                    y1 = Y1[i % 2]; y2 = Y2[i % 2]; xb_ = xr[i % 2]
                    for yb, c in ((y1, 0), (y2, 1)):
                        k.iload(yb, yb.t[:], ysort[:, :], IDX.t[:, 2 * i + c:2 * i + c + 1])
                    k.load('sp', xb_, xb_.t[:], seg.x1[r0:r0 + 128, :])
                    k.acq_r('dve', y1); k.acq_r('dve', y2); k.acq_r('dve', xb_)
                    nc.vector.tensor_scalar(out=y1.t[:], in0=y1.t[:], scalar1=G12.t[:, i, 0:1], scalar2=None, op0=ALU.mult)
                    nc.vector.scalar_tensor_tensor(out=y1.t[:], in0=y2.t[:], scalar=G12.t[:, i, 1:2], in1=y1.t[:], op0=ALU.mult, op1=ALU.add)
                    tok = k.sig('dve', nc.vector.scalar_tensor_tensor(out=y1.t[:], in0=xb_.t[:], scalar=float(ALPHA), in1=y1.t[:], op0=ALU.mult, op1=ALU.add))
                    k.add_r(y2, tok); k.add_r(xb_, tok)
                    tok = ln_inplace(y1.t[:], gb, (st, mv, rs, nb))
                    k.set_w(y1, tok, fresh=False)
                    k.store('sp', y1, seg.yout[r0 - seg.yoff:r0 - seg.yoff + 128, :], y1.t[:])
            outer.close()
    return nc


def _host_prep(inputs):
    f32 = np.float32
    w_in = np.asarray(inputs["w_in"], f32)
    L = w_in.shape[0]
    idx = np.arange(64)
    perm = idx.copy()
    perm[0:8] = idx[8:16]
    perm[8:16] = idx[0:8]
    qcols = 1536 + (np.arange(12)[:, None] * 64 + perm[None, :]).reshape(-1)
    kcols = 2304 + (np.arange(12)[:, None] * 64 + perm[None, :]).reshape(-1)
    w_in_ext = np.concatenate([w_in, w_in[:, :, qcols], w_in[:, :, kcols]], axis=-1)
    cpar = np.zeros((L, 128, 6, 34), f32)
    for l in range(L):
        cpar[l, :, :, 0:31] = np.asarray(inputs["conv_w"], f32)[l].T.reshape(6, 128, 31).transpose(1, 0, 2)
        cpar[l, :, :, 31] = np.asarray(inputs["conv_b"], f32)[l].reshape(6, 128).T
        cpar[l, :, :, 32] = np.asarray(inputs["conv_ln_g"], f32)[l].reshape(6, 128).T
        cpar[l, :, :, 33] = np.asarray(inputs["conv_ln_b"], f32)[l].reshape(6, 128).T
    shared = {
        "w_in_ext": np.ascontiguousarray(w_in_ext), "cpar": cpar,
        "maskd": _mult_mask(), "identd": np.eye(128, dtype=f32),
        "triud": np.triu(np.ones((128, 128), f32), 1), "iotad": np.arange(128, dtype=f32).reshape(128, 1),
        "cs_p": _rope_tables(np.arange(PW)), "valid_p": np.ones((128, PW // 128), f32),
    }
    for n in ("w_mem_kv", "w_out", "ln1_g", "ln1_b", "ln2_g", "ln2_b", "ffn_w1", "ffn_w3", "ffn_w2",
              "moe_router", "moe_w1", "moe_w3", "moe_w2"):
        shared[n] = np.ascontiguousarray(np.asarray(inputs[n], f32))
    xp = np.asarray(inputs["x_prompt"], f32)
    xs = np.asarray(inputs["x_sample"], f32)
    mp = np.asarray(inputs["mem_prompt"], f32)
    ms = np.asarray(inputs["mem_sample"], f32)
    in_maps = []
    for c in range(NCORE):
        sq, j = c // 4, c % 4
        a = j * 4096
        lo = a - 2048
        pos = np.arange(lo, lo + SW)
        ok = (pos >= 0) & (pos < 16384)
        xw = np.zeros((SW, D), f32)
        xw[ok] = xs[sq, pos[ok]]
        m = dict(shared)
        m["xp"] = np.ascontiguousarray(xp[c])
        m["xs"] = xw
        m["memp"] = np.ascontiguousarray(mp[c])
        m["mems"] = np.ascontiguousarray(ms[sq])
        m["valid_s"] = np.ascontiguousarray(ok.astype(f32).reshape(SW // 128, 128).T)
        m["cs_s"] = _rope_tables(np.where(ok, pos, 0))
        in_maps.append(m)
    return in_maps


def kernel(**inputs):
    in_maps = _host_prep(inputs)
    nc = build()
    res = run_bass_kernel_spmd(nc, in_maps, core_ids=list(range(NCORE)))
    yp = np.stack([res.results[c]["yp"] for c in range(NCORE)], axis=0)
    ysf = np.zeros((2, 16384, D), np.float32)
    for c in range(NCORE):
        sq, j = c // 4, c % 4
        ysf[sq, j * 4096:(j + 1) * 4096] = res.results[c]["ys"]
    return (yp.astype(np.float32), ysf)
```

```python
import numpy as np
from contextlib import ExitStack
import concourse.bass as bass
import concourse.mybir as mybir
from concourse.bass_utils import run_bass_kernel_spmd

F32 = mybir.dt.float32
BF16 = mybir.dt.bfloat16
AF = mybir.ActivationFunctionType
ALU = mybir.AluOpType

D = 2048
DEPTH = 2
NCORE = 8
D_CONV = 768
D_ATT = 768
D_MEM = 512
D_IN = 4352
D_EXT = D_IN + 2 * D_ATT
NMT = D_EXT // 128
D_FF = 5632
E_FF = 7168
NE = 8
ALPHA = (2 * DEPTH) ** 0.25
LN_EPS = 1e-5
ROPE_THETA = 500000.0
MASK_D0 = 1408
MASK_J = 2944
SW = 8192
PW = 2048


def _mult_mask():
    kk = np.arange(128)[:, None]
    j = np.arange(MASK_J)[None, :]
    o = kk - j + MASK_D0
    ao = np.abs(o)
    c = (ao <= 64).astype(np.float32) + ((o % 4 == 0) & (ao <= 256)) + ((o % 16 == 0) & (ao <= 1024))
    return c.astype(np.float32)


def _rope_tables(pos):
    half = 8
    inv = np.power(np.float32(ROPE_THETA), -np.arange(0, 16, 2, dtype=np.float32) / np.float32(16)).astype(np.float32)
    ang = pos.astype(np.float32)[None, :] * inv[:, None]
    cos = np.cos(ang).astype(np.float32)
    sin = np.sin(ang).astype(np.float32)
    W = pos.shape[0]
    C = np.ones((128, W), np.float32)
    S = np.zeros((128, W), np.float32)
    for hh in range(2):
        b = hh * 64
        C[b:b + 8] = cos
        C[b + 8:b + 16] = cos
        S[b:b + 8] = -sin
        S[b + 8:b + 16] = sin
    return np.stack([C, S], axis=0)


class Sem:
    def __init__(self, nc, name):
        self.h = nc.semaphore(name).__enter__()
        self.n = 0
        self.name = name


class Buf:
    def __init__(self, tile, sem=None):
        self.t = tile
        self.rd = []
        self.wr = []
        self.sem = sem


class KB:
    def __init__(self):
        self.nc = bass.Bass("TRN2", target_bir_lowering=False)
        nc = self.nc
        self.eng = {'pe': nc.tensor, 'act': nc.scalar, 'dve': nc.vector, 'pool': nc.gpsimd, 'sp': nc.sync}
        self.esem = {e: Sem(nc, "e_" + e) for e in ('pe', 'act', 'dve', 'pool')}
        self.dsems = [Sem(nc, f"d{i}") for i in range(72)]
        self.dfree = list(self.dsems)
        self.waited = {}
        self.pending = []
        self.uid = 0

    def sig(self, e, ins):
        s = self.esem[e]
        ins.then_inc(s.h, 1)
        s.n += 1
        return (s, s.n, e)

    def wait(self, e, tok, force=False):
        if tok is None or (tok[2] == e and not force):
            return
        key = (e, tok[0].name)
        if self.waited.get(key, 0) >= tok[1]:
            return
        self.waited[key] = tok[1]
        self.eng[e].wait_ge(tok[0].h, tok[1])

    def dma(self, q, out, in_, sem):
        ins = self.eng[q].dma_start(out=out, in_=in_)
        ins.then_inc(sem.h, 16)
        sem.n += 16
        return (sem, sem.n, None)

    def getsem(self):
        return self.dfree.pop()

    def acq_w(self, e, b):
        for t in b.rd + b.wr:
            self.wait(e, t)

    def set_w(self, b, tok, fresh=True):
        if fresh:
            b.rd = []
            b.wr = [tok]
        else:
            b.wr.append(tok)

    def acq_r(self, e, b):
        for t in b.wr:
            self.wait(e, t)

    def add_r(self, b, tok):
        b.rd.append(tok)

    def load(self, q, b, out, in_, fresh=True):
        self.acq_w(q, b)
        tok = self.dma(q, out, in_, b.sem)
        self.set_w(b, tok, fresh)
        return tok

    def iload(self, b, out, in_, idx_ap, elem_off=0, fresh=True, bounds=None):
        self.acq_w('pool', b)
        kw = {}
        if bounds is not None:
            kw = dict(bounds_check=bounds, oob_is_err=False)
        ins = self.nc.gpsimd.indirect_dma_start(out=out, out_offset=None, in_=in_,
                                                in_offset=bass.IndirectOffsetOnAxis(ap=idx_ap, axis=0),
                                                element_offset=elem_off, **kw)
        ins.then_inc(b.sem.h, 16)
        b.sem.n += 16
        tok = (b.sem, b.sem.n, None)
        self.set_w(b, tok, fresh)
        return tok

    def store(self, q, b, out, in_):
        self.acq_r(q, b)
        tok = self.dma(q, out, in_, b.sem)
        self.add_r(b, tok)
        self.pending.append((q, tok))
        return tok

    def phase_end(self):
        for q, tok in self.pending:
            self.wait(q, tok)
        self.pending = []
        self.nc.all_engine_barrier()


class Phase:
    def __init__(self, k):
        self.k = k
        self.es = ExitStack()
        self.sems = []

    def __enter__(self):
        self.es.__enter__()
        return self

    def __exit__(self, *a):
        self.k.phase_end()
        for s in self.sems:
            self.k.dfree.append(s)
        return self.es.__exit__(*a)

    def sb(self, shape, dt, dma=False, name=None):
        k = self.k
        k.uid += 1
        t = self.es.enter_context(k.nc.sbuf_tensor(f"{name or 't'}{k.uid}", list(shape), dt))
        s = None
        if dma:
            s = k.getsem()
            self.sems.append(s)
        return Buf(t, s)

    def ps(self, shape, dt, name=None):
        k = self.k
        k.uid += 1
        t = self.es.enter_context(k.nc.psum_tensor(f"{name or 'p'}{k.uid}", list(shape), dt))
        return Buf(t)

    def ring(self, n, shape, dt, dma=False, name=None):
        return [self.sb(shape, dt, dma, name) for _ in range(n)]

    def pring(self, n, shape, dt, name=None):
        return [self.ps(shape, dt, name) for _ in range(n)]


class Seg:
    pass


def build(dbg=False, stop=None, only_p=False):
    k = KB()
    nc = k.nc
    E = k.eng

    def din(name, shape, dt=F32):
        return nc.dram_tensor(name, list(shape), dt, kind="ExternalInput").ap()

    def dscr(name, shape, dt, out=False):
        return nc.dram_tensor(name, list(shape), dt, kind=("ExternalOutput" if out else "Internal")).ap()

    xp = din("xp", [PW, D])
    xs = din("xs", [SW, D])
    memp = din("memp", [256, D])
    mems = din("mems", [256, D])
    valid_s = din("valid_s", [128, SW // 128])
    valid_p = din("valid_p", [128, PW // 128])
    cs_p = din("cs_p", [2, 128, PW])
    cs_s = din("cs_s", [2, 128, SW])
    maskd = din("maskd", [128, MASK_J])
    identd = din("identd", [128, 128])
    triud = din("triud", [128, 128])
    iotad = din("iotad", [128, 1])
    w_in = din("w_in_ext", [DEPTH, D, D_EXT])
    cpar = din("cpar", [DEPTH, 128, 6, 34])
    w_memkv = din("w_mem_kv", [DEPTH, D, 1024])
    w_out = din("w_out", [DEPTH, D, D])
    ln1_g = din("ln1_g", [DEPTH, D]); ln1_b = din("ln1_b", [DEPTH, D])
    ln2_g = din("ln2_g", [DEPTH, D]); ln2_b = din("ln2_b", [DEPTH, D])
    ffn_w1 = din("ffn_w1", [1, D, D_FF]); ffn_w3 = din("ffn_w3", [1, D, D_FF]); ffn_w2 = din("ffn_w2", [1, D_FF, D])
    router = din("moe_router", [1, D, NE])
    moe_w1 = din("moe_w1", [1, NE, D, E_FF]); moe_w3 = din("moe_w3", [1, NE, D, E_FF]); moe_w2 = din("moe_w2", [1, NE, E_FF, D])
    yp = nc.dram_tensor("yp", [PW, D], F32, kind="ExternalOutput").ap()
    ys = nc.dram_tensor("ys", [4096, D], F32, kind="ExternalOutput").ap()

    win_b = [dscr(f"win_b{l}", [NMT, 128, 16 * 128], BF16) for l in range(DEPTH)]
    wkv_b = [dscr(f"wkv_b{l}", [8, 128, 16 * 128], BF16) for l in range(DEPTH)]
    wout_b = [dscr(f"wout_b{l}", [D, D], BF16) for l in range(DEPTH)]
    f1_b = dscr("f1_b", [D_FF // 128, 128, 2048], BF16)
    f3_b = dscr("f3_b", [D_FF // 128, 128, 2048], BF16)
    f2_b = dscr("f2_b", [D_FF, D], BF16)
    NFE = E_FF // 128
    FG = 4
    NFG = NFE // FG
    NFH = NFE // 2
    m13_h = [dscr(f"m13_b{h}", [NE * NFH * 128, 2 * 2048], BF16) for h in range(2)]
    m2_b = dscr("m2_b", [NE * 4 * NFG * 128, FG * 512], BF16)

    segs = []
    for nm, W, xin, mem, valid, cs, hr, orr, yout, yoff in (
            ("p", PW, xp, memp, valid_p, cs_p, [(0, PW), (0, PW)], [(0, PW), (0, PW)], yp, 0),
            ("s", SW, xs, mems, valid_s, cs_s, [(0, SW), (1024, 7168)], [(1024, 7168), (2048, 6144)], ys, 2048)):
        s = Seg()
        s.nm, s.W, s.x0, s.mem, s.valid, s.cs, s.hr, s.orr, s.yout, s.yoff = nm, W, xin, mem, valid, cs, hr, orr, yout, yoff
        s.u = dscr(f"u_{nm}", [D_CONV, W], F32, out=dbg)
        s.q = dscr(f"q_{nm}", [D_ATT, W], BF16, out=dbg)
        s.kk = dscr(f"k_{nm}", [D_ATT, W], BF16, out=dbg)
        s.qm = dscr(f"qm_{nm}", [D_MEM, W], BF16, out=dbg)
        s.v = dscr(f"v_{nm}", [W, 12 * 128], BF16, out=dbg)
        s.mix = dscr(f"mix_{nm}", [D, W], BF16, out=dbg)
        s.x1 = dscr(f"x1_{nm}", [W, D], F32, out=dbg)
        s.x2 = dscr(f"x2_{nm}", [W, D], F32, out=dbg)
        s.comb = dscr(f"comb_{nm}", [W, NE], F32, out=dbg)
        segs.append(s)
    if only_p:
        segs = segs[:1]

    wsem = k.getsem()
    wsemA = k.getsem()
    wsemB = k.getsem()
    bgq = []
    bgtok = {}

    def conv_tiled(src, dst, ncols, sem):
        for m in range(ncols // 128):
            s_ap = src[:, m * 128:(m + 1) * 128].rearrange("(k p) c -> p k c", p=128)
            d_ap = dst[m].rearrange("p (k c) -> p k c", c=128)
            bgq.append((d_ap, s_ap, sem))

    def conv_plain(src, dst, nrows, sem):
        for r in range(0, nrows, 128):
            bgq.append((dst[r:r + 128, :], src[r:r + 128, :], sem))

    def bg(n):
        for _ in range(n):
            if not bgq:
                return
            d_ap, s_ap, sem = bgq.pop(0)
            bgtok[sem.name] = k.dma('pool', d_ap, s_ap, sem)

    for l in range(DEPTH):
        sem = wsem if l == 0 else wsemA
        conv_tiled(w_in[l], win_b[l], D_EXT, sem)
        conv_tiled(w_memkv[l], wkv_b[l], 1024, sem)
        conv_plain(w_out[l], wout_b[l], D, sem)
        if l == 0:
            conv_tiled(ffn_w1[0], f1_b, D_FF, sem)
            conv_tiled(ffn_w3[0], f3_b, D_FF, sem)
            conv_plain(ffn_w2[0], f2_b, D_FF, sem)
            bg(len(bgq))
    if stop is None or stop > 10:
        for e in range(NE):
            for m in range(NFE):
                r0 = (e * NFH + (m % NFH)) * 128
                for wi, src in enumerate((moe_w1[0, e], moe_w3[0, e])):
                    s_ap = src[:, m * 128:(m + 1) * 128].rearrange("(k p) c -> p k c", p=128)
                    d_ap = m13_h[m // NFH][r0:r0 + 128, wi * 2048:(wi + 1) * 2048].rearrange("p (k c) -> p k c", c=128)
                    bgq.append((d_ap, s_ap, wsemB))
            for fg in range(NFG):
                for dch in range(4):
                    r0 = ((e * 4 + dch) * NFG + fg) * 128
                    s_ap = moe_w2[0, e][fg * 512:(fg + 1) * 512, dch * 512:(dch + 1) * 512].rearrange("(fi p) c -> p fi c", p=128)
                    d_ap = m2_b[r0:r0 + 128, :].rearrange("p (fi c) -> p fi c", c=512)
                    bgq.append((d_ap, s_ap, wsemB))
    k.pending = [('pool', bgtok[wsem.name])]
    k.phase_end()

    cst = ExitStack()
    ident_b = Buf(cst.enter_context(nc.sbuf_tensor("ident_b", [128, 128], BF16)), k.getsem())
    ident_f = Buf(cst.enter_context(nc.sbuf_tensor("ident_f", [128, 128], F32)), k.getsem())
    ones_b = Buf(cst.enter_context(nc.sbuf_tensor("ones_b", [128, 128], BF16)))
    ones_f = Buf(cst.enter_context(nc.sbuf_tensor("ones_f", [128, 128], F32)))
    epsb = Buf(cst.enter_context(nc.sbuf_tensor("epsb", [128, 1], F32)))
    k.load('pool', ident_b, ident_b.t[:], identd[:, :])
    k.load('sp', ident_f, ident_f.t[:], identd[:, :])
    k.set_w(ones_b, k.sig('dve', nc.vector.memset(ones_b.t[:], 1.0)))
    k.set_w(ones_f, k.sig('dve', nc.vector.memset(ones_f.t[:], 1.0)))
    k.set_w(epsb, k.sig('dve', nc.vector.memset(epsb.t[:], LN_EPS)))
    for e in ('pe', 'act', 'dve', 'pool'):
        k.acq_r(e, ident_b); k.acq_r(e, ident_f); k.acq_r(e, ones_b); k.acq_r(e, ones_f); k.acq_r(e, epsb)

    def load_xT(ph, src_rows, xb, xT, tp_ring, cnt):
        k.load('pool', xb, xb.t[:], src_rows.rearrange("(t p) d -> p t d", p=128))
        k.acq_r('pe', xb)
        k.acq_w('act', xT); k.acq_w('dve', xT)
        first = True
        tokp_last = [None]
        for kg in range(4):
            tp = tp_ring[cnt[0] % len(tp_ring)]; cnt[0] += 1
            k.acq_w('pe', tp)
            for kk in range(4):
                for t in range(4):
                    ins = nc.tensor.transpose(tp.t[:, kk, t * 128:(t + 1) * 128], xb.t[:, t, (kg * 4 + kk) * 128:(kg * 4 + kk + 1) * 128], ident_b.t[:])
            tokp_last[0] = k.sig('pe', ins)
            k.set_w(tp, tokp_last[0])
            e = 'act' if kg % 2 == 0 else 'dve'
            k.acq_r(e, tp)
            if e == 'act':
                ins = nc.scalar.activation(out=xT.t[:, kg * 4:(kg + 1) * 4, :], in_=tp.t[:], func=AF.Copy)
            else:
                ins = nc.vector.tensor_copy(out=xT.t[:, kg * 4:(kg + 1) * 4, :], in_=tp.t[:])
            tok = k.sig(e, ins)
            k.add_r(tp, tok)
            k.set_w(xT, tok, fresh=first)
            first = False
        k.add_r(xb, tokp_last[0])

    def ln_inplace(yt, gb, rs_pool, valid_ap=None):
        st, mv, rs, nb = rs_pool
        for ch in range(4):
            ins = nc.vector.bn_stats(out=st.t[:, ch, :], in_=yt[:, ch * 512:(ch + 1) * 512])
        k.wait('dve', k.sig('dve', ins), force=True)
        tok = k.sig('dve', nc.vector.bn_aggr(out=mv.t[:], in_=st.t[:].rearrange("p a b -> p (a b)")))
        k.wait('act', tok)
        tok = k.sig('act', nc.scalar.activation(out=rs.t[:], in_=mv.t[:, 1:2], func=AF.Sqrt, bias=epsb.t[:, 0:1], scale=1.0))
        k.wait('dve', tok)
        tok = k.sig('dve', nc.vector.reciprocal(out=rs.t[:], in_=rs.t[:]))
        k.wait('dve', tok, force=True)
        tok = k.sig('dve', nc.vector.tensor_scalar(out=nb.t[:], in0=mv.t[:, 0:1], scalar1=rs.t[:, 0:1], scalar2=-1.0, op0=ALU.mult, op1=ALU.mult))
        k.wait('act', tok)
        tok = k.sig('act', nc.scalar.activation(out=yt, in_=yt, func=AF.Identity, bias=nb.t[:, 0:1], scale=rs.t[:, 0:1]))
        k.wait('dve', tok)
        nc.vector.tensor_tensor(out=yt, in0=yt, in1=gb.t[:, 0, :], op=ALU.mult)
        ins = nc.vector.tensor_tensor(out=yt, in0=yt, in1=gb.t[:, 1, :], op=ALU.add)
        if valid_ap is not None:
            ins = nc.vector.tensor_scalar(out=yt, in0=yt, scalar1=valid_ap, scalar2=None, op0=ALU.mult)
        return k.sig('dve', ins)

    for l in range(DEPTH):
        if stop is not None and stop <= l * 10:
            break
        if l == 1:
            while bgq and bgq[0][2] is wsemA:
                bg(1)
            for q in ('sp', 'pool'):
                k.wait(q, bgtok.get(wsemA.name))
        for seg in segs:
            xin = seg.x0 if l == 0 else seg.x2
            hs, he = seg.hr[l]
            os_, oe = seg.orr[l]
            W = seg.W
            with Phase(k) as ph:
                xb = ph.sb([128, 4, 2048], BF16, dma=True, name="xb")
                xT = ph.sb([128, 16, 512], BF16, name="xT")
                wv = ph.sb([128, 6, 2048], BF16, dma=True, name="wv")
                wt = ph.ring(6, [128, 2048], BF16, dma=True, name="wt")
                cst_ = ph.ring(2, [128, 2, 512], F32, dma=True, name="cs")
                vt = ph.sb([128, W // 128], F32, dma=True, name="vt")
                onesv = ph.sb([128, 6, 64], BF16, name="onesv")
                sg = ph.ring(2, [128, 512], F32, name="sg")
                uo = ph.ring(2, [128, 512], F32, dma=True, name="uo")
                t1 = ph.ring(2, [128, 512], F32, name="t1")
                t2 = ph.ring(2, [128, 512], F32, name="t2")
                qo = ph.ring(2, [128, 512], BF16, dma=True, name="qo")
                qmo = ph.ring(2, [128, 512], BF16, dma=True, name="qmo")
                vx = ph.ring(2, [128, 12, 128], BF16, dma=True, name="vx")
                tpr = ph.pring(2, [128, 4, 512], BF16, name="tp")
                mm = ph.pring(4, [128, 512], F32, name="mm")
                cnt = [0]
                mmc = [0]
                wtc = [0]
                k.set_w(onesv, k.sig('pool', nc.gpsimd.memset(onesv.t[:], 1.0)))
                k.load('sp', vt, vt.t[:], seg.valid[:, :])
                for m in range(6):
                    k.load('sp', wv, wv.t[:, m, :], win_b[l][24 + m], fresh=(m == 0))
                k.acq_r('pe', wv)
                k.acq_r('pool', vt)

                def mtile(m, xTb):
                    wb = wt[wtc[0] % 6]; wtc[0] += 1
                    k.load('sp', wb, wb.t[:], win_b[l][m])
                    pb = mm[mmc[0] % 4]; mmc[0] += 1
                    k.acq_r('pe', wb); k.acq_w('pe', pb); k.acq_r('pe', xTb)
                    for kk in range(16):
                        ins = nc.tensor.matmul(pb.t[:], lhsT=wb.t[:, kk * 128:(kk + 1) * 128], rhs=xTb.t[:, kk, :], start=(kk == 0), stop=(kk == 15))
                    tok = k.sig('pe', ins)
                    k.set_w(pb, tok); k.add_r(wb, tok)
                    return pb, tok

                nblk = (he - hs) // 512
                for b in range(nblk):
                    t0 = hs + b * 512
                    load_xT(ph, xin[t0:t0 + 512, :], xb, xT, tpr, cnt)
                    bg(8)
                    csb = cst_[b % 2]
                    k.load('sp', csb, csb.t[:], seg.cs[:, :, t0:t0 + 512].rearrange("a p t -> p a t"))
                    lasttok = None
                    for c in range(6):
                        pa, _ = mtile(c, xT)
                        pg, _ = mtile(6 + c, xT)
                        sgb = sg[c % 2]; uob = uo[c % 2]
                        k.acq_r('act', pg); k.acq_w('act', sgb)
                        tok = k.sig('act', nc.scalar.activation(out=sgb.t[:], in_=pg.t[:], func=AF.Sigmoid))
                        k.add_r(pg, tok); k.set_w(sgb, tok)
                        k.acq_r('dve', pa); k.acq_r('dve', sgb); k.acq_w('dve', uob)
                        tok = k.sig('dve', nc.vector.tensor_tensor(out=uob.t[:], in0=pa.t[:], in1=sgb.t[:], op=ALU.mult))
                        k.add_r(pa, tok); k.add_r(sgb, tok); k.set_w(uob, tok)
                        k.store('pool', uob, seg.u[c * 128:(c + 1) * 128, t0:t0 + 512], uob.t[:])
                    k.acq_r('dve', csb)
                    for which, base, pbase, dst in (("q", 12, 34, seg.q), ("k", 18, 40, seg.kk)):
                        for hp in range(6):
                            pq, _ = mtile(base + hp, xT)
                            pp, _ = mtile(pbase + hp, xT)
                            i2 = hp % 2
                            k.acq_r('dve', pq); k.acq_w('dve', t1[i2])
                            tok = k.sig('dve', nc.vector.tensor_tensor(out=t1[i2].t[:], in0=pq.t[:], in1=csb.t[:, 0, :], op=ALU.mult))
                            k.add_r(pq, tok); k.set_w(t1[i2], tok)
                            k.acq_r('dve', pp); k.acq_w('dve', t2[i2])
                            tok = k.sig('dve', nc.vector.tensor_tensor(out=t2[i2].t[:], in0=pp.t[:], in1=csb.t[:, 1, :], op=ALU.mult))
                            k.add_r(pp, tok); k.set_w(t2[i2], tok); k.add_r(csb, tok)
                            k.acq_r('pool', t1[i2]); k.acq_r('pool', t2[i2]); k.acq_w('pool', qo[i2])
                            tok = k.sig('pool', nc.gpsimd.tensor_tensor(out=qo[i2].t[:], in0=t1[i2].t[:], in1=t2[i2].t[:], op=ALU.add))
                            k.add_r(t1[i2], tok); k.add_r(t2[i2], tok); k.set_w(qo[i2], tok)
                            k.store('pool', qo[i2], dst[hp * 128:(hp + 1) * 128, t0:t0 + 512], qo[i2].t[:])
                    for mh in range(4):
                        pq, _ = mtile(30 + mh, xT)
                        ob = qmo[mh % 2]
                        k.acq_r('act', pq); k.acq_w('act', ob)
                        tok = k.sig('act', nc.scalar.activation(out=ob.t[:], in_=pq.t[:], func=AF.Copy))
                        k.add_r(pq, tok); k.set_w(ob, tok)
                        k.store('pool', ob, seg.qm[mh * 128:(mh + 1) * 128, t0:t0 + 512], ob.t[:])
                    for t in range(4):
                        vb = vx[t % 2]
                        tile_idx = (t0 // 128) + t
                        k.acq_w('pool', vb)
                        v5 = vb.t[:].rearrange("p (a two) d -> p a two d", two=2)
                        nc.gpsimd.tensor_scalar(out=v5[:, :, 0, 64:128], in0=onesv.t[:], scalar1=vt.t[:, tile_idx:tile_idx + 1], scalar2=None, op0=ALU.mult)
                        tok = k.sig('pool', nc.gpsimd.tensor_scalar(out=v5[:, :, 1, 0:64], in0=onesv.t[:], scalar1=vt.t[:, tile_idx:tile_idx + 1], scalar2=None, op0=ALU.mult))
                        k.set_w(vb, tok)
                        for (c0, ncol, h0, nh) in ((0, 512, 0, 8), (512, 256, 8, 4)):
                            pb = mm[mmc[0] % 4]; mmc[0] += 1
                            k.acq_w('pe', pb); k.acq_r('pe', xT)
                            for kk in range(16):
                                ins = nc.tensor.matmul(pb.t[:, 0:ncol], lhsT=xT.t[:, kk, t * 128:(t + 1) * 128],
                                                       rhs=wv.t[:, c0 // 128:(c0 + ncol) // 128, kk * 128:(kk + 1) * 128],
                                                       start=(kk == 0), stop=(kk == 15))
                            tokp = k.sig('pe', ins)
                            lasttok = tokp
                            k.set_w(pb, tokp)
                            p4 = pb.t[:, 0:ncol].rearrange("p (a two d) -> p a two d", two=2, d=64)
                            d4 = vb.t[:, h0:h0 + nh, :].rearrange("p (a two) d -> p a two d", two=2)
                            k.acq_r('act', pb); k.acq_w('act', vb)
                            tok = k.sig('act', nc.scalar.activation(out=d4[:, :, 0, 0:64], in_=p4[:, :, 0, :], func=AF.Copy))
                            k.add_r(pb, tok); k.set_w(vb, tok, fresh=False)
                            k.acq_r('dve', pb); k.acq_w('dve', vb)
                            tok = k.sig('dve', nc.vector.tensor_copy(out=d4[:, :, 1, 64:128], in_=p4[:, :, 1, :]))
                            k.add_r(pb, tok); k.set_w(vb, tok, fresh=False)
                        k.store('pool', vb, seg.v[t0 + t * 128:t0 + (t + 1) * 128, :], vb.t[:].rearrange("p h d -> p (h d)"))
                    k.add_r(xT, lasttok)
            if stop is not None and stop <= l * 10 + 1:
                continue
            with Phase(k) as ph:
                cw = ph.sb([128, 6, 34], F32, dma=True, name="cw")
                ut = ph.ring(2, [128, 6, 544], F32, dma=True, name="ut")
                acc = ph.ring(2, [128, 6, 512], F32, name="acc")
                ysq = ph.ring(2, [128, 512], F32, name="ysq")
                mean = ph.sb([128, 512], F32, name="mean")
                msq = ph.sb([128, 512], F32, name="msq")
                rstd = ph.sb([128, 512], F32, name="rstd")
                zt = ph.ring(2, [128, 512], F32, name="zt")
                co = ph.ring(2, [128, 512], BF16, dma=True, name="co")
                sps = ph.pring(2, [128, 512], F32, name="sps")
                k.load('sp', cw, cw.t[:], cpar[l])
                k.acq_r('dve', cw); k.acq_r('act', cw)
                nblk = (oe - os_) // 512
                for b in range(nblk):
                    t0 = os_ + b * 512
                    ub = ut[b % 2]; ab = acc[b % 2]
                    lo = max(hs, t0 - 16); hi = min(he, t0 + 528)
                    fresh = True
                    if lo > t0 - 16 or hi < t0 + 528:
                        k.acq_w('pool', ub)
                        k.set_w(ub, k.sig('pool', nc.gpsimd.memset(ub.t[:], 0.0)))
                        fresh = False
                    for c in range(6):
                        k.load('sp', ub, ub.t[:, c, lo - (t0 - 16):hi - (t0 - 16)], seg.u[c * 128:(c + 1) * 128, lo:hi], fresh=(fresh and c == 0))
                    k.acq_r('dve', ub); k.acq_w('dve', ab)
                    k.acq_w('pe', sps[0]); k.acq_w('pe', sps[1])
                    for c in range(6):
                        nc.vector.tensor_scalar(out=ab.t[:, c, :], in0=ub.t[:, c, 1:513], scalar1=cw.t[:, c, 0:1], scalar2=cw.t[:, c, 31:32], op0=ALU.mult, op1=ALU.add)
                        for j in range(1, 31):
                            ins = nc.vector.scalar_tensor_tensor(out=ab.t[:, c, :], in0=ub.t[:, c, j + 1:j + 513], scalar=cw.t[:, c, j:j + 1], in1=ab.t[:, c, :], op0=ALU.mult, op1=ALU.add)
                        tok = k.sig('dve', ins)
                        k.set_w(ab, tok, fresh=(c == 0))
                        yb = ysq[c % 2]
                        k.wait('act', tok); k.acq_w('act', yb)
                        toka = k.sig('act', nc.scalar.activation(out=yb.t[:], in_=ab.t[:, c, :], func=AF.Square))
                        k.set_w(yb, toka)
                        k.wait('pe', tok)
                        nc.tensor.matmul(sps[0].t[:], lhsT=ones_f.t[:], rhs=ab.t[:, c, :], start=(c == 0), stop=(c == 5))
                        k.wait('pe', toka)
                        tokp = k.sig('pe', nc.tensor.matmul(sps[1].t[:], lhsT=ones_f.t[:], rhs=yb.t[:], start=(c == 0), stop=(c == 5)))
                        k.add_r(yb, tokp)
                    k.add_r(ub, tok)
                    k.set_w(sps[0], tokp); k.set_w(sps[1], tokp)
                    k.add_r(ab, tokp)
                    k.wait('act', tokp); k.acq_w('act', mean)
                    tokm = k.sig('act', nc.scalar.activation(out=mean.t[:], in_=sps[0].t[:], func=AF.Copy, scale=1.0 / D_CONV))
                    k.set_w(mean, tokm); k.add_r(sps[0], tokm)
                    k.wait('dve', tokm); k.wait('dve', tokp)
                    nc.vector.tensor_tensor(out=msq.t[:], in0=mean.t[:], in1=mean.t[:], op=ALU.mult)
                    tok = k.sig('dve', nc.vector.scalar_tensor_tensor(out=msq.t[:], in0=sps[1].t[:], scalar=1.0 / D_CONV, in1=msq.t[:], op0=ALU.mult, op1=ALU.subtract))
                    k.add_r(sps[1], tok)
                    k.wait('act', tok)
                    tok = k.sig('act', nc.scalar.activation(out=rstd.t[:], in_=msq.t[:], func=AF.Sqrt, bias=epsb.t[:, 0:1], scale=1.0))
                    k.wait('dve', tok)
                    nc.vector.reciprocal(out=rstd.t[:], in_=rstd.t[:])
                    for c in range(6):
                        zb = zt[c % 2]; cb = co[c % 2]
                        k.acq_w('dve', zb)
                        nc.vector.tensor_tensor(out=zb.t[:], in0=ab.t[:, c, :], in1=mean.t[:], op=ALU.subtract)
                        tok = k.sig('dve', nc.vector.tensor_tensor(out=zb.t[:], in0=zb.t[:], in1=rstd.t[:], op=ALU.mult))
                        k.set_w(zb, tok)
                        k.acq_r('act', zb); k.acq_w('act', cb)
                        toka = k.sig('act', nc.scalar.activation(out=cb.t[:], in_=zb.t[:], func=AF.Silu, bias=cw.t[:, c, 33:34], scale=cw.t[:, c, 32:33]))
                        k.add_r(zb, toka); k.set_w(cb, toka)
                        k.store('pool', cb, seg.mix[c * 128:(c + 1) * 128, t0:t0 + 512], cb.t[:])
                    k.add_r(ab, tok); k.add_r(mean, tok)
            if stop is not None and stop <= l * 10 + 2:
                continue
            with Phase(k) as ph:
                mk = ph.sb([128, MASK_J], BF16, dma=True, name="mk")
                qt = ph.ring(2, [128, 512], BF16, dma=True, name="qt")
                kt = ph.ring(2, [128, 2560], BF16, dma=True, name="kt")
                vxl = ph.ring(2, [128, 20, 256], BF16, dma=True, name="vxl")
                NS = 6
                LA = 3
                pe_t = ph.ring(NS, [128, 512], BF16, name="pexp")
                pm_t = ph.ring(NS, [128, 512], BF16, name="pmsk")
                rd = ph.ring(2, [128, 512], F32, name="rd")
                rd2 = ph.ring(2, [128, 512], F32, name="rd2")
                att = ph.ring(2, [128, 512], BF16, dma=True, name="att")
                sp_ = ph.pring(NS, [128, 512], F32, name="sps")
                ap_ = ph.pring(2, [128, 512], F32, name="aps")
                k.load('pool', mk, mk.t[:], maskd[:, :])
                k.acq_r('dve', mk)
                it = 0; sc = 0; ac = 0
                nqb = (oe - os_) // 512
                for qb in range(nqb):
                    q0 = os_ + qb * 512
                    k0 = max(hs, q0 - 1024); k1 = min(he, q0 + 512 + 1024)
                    nkb = (k1 - k0) // 128
                    for hp in range(6):
                        qb_ = qt[it % 2]; kb_ = kt[it % 2]; vb_ = vxl[it % 2]; ab_ = att[it % 2]; it += 1
                        bg(4)
                        k.load('sp', qb_, qb_.t[:], seg.q[hp * 128:(hp + 1) * 128, q0:q0 + 512])
                        k.load('sp', kb_, kb_.t[:, 0:k1 - k0], seg.kk[hp * 128:(hp + 1) * 128, k0:k1])
                        k.load('sp', vb_, vb_.t[:, 0:nkb, :], seg.v[k0:k1, hp * 256:(hp + 1) * 256].rearrange("(kb p) c -> p kb c", p=128))
                        k.acq_r('pe', qb_); k.acq_r('pe', kb_); k.acq_r('pe', vb_)
                        for hh in range(2):
                            r0 = hh * 64
                            accb = ap_[ac % 2]; ac += 1
                            k.acq_w('pe', accb)
                            pend = []
                            for kbi in range(nkb):
                                d = (k0 + kbi * 128) - q0
                                j0 = MASK_D0 - d
                                sb_ = sp_[sc % NS]; peb = pe_t[sc % NS]; pmb = pm_t[sc % NS]; sc += 1
                                k.acq_w('pe', sb_)
                                tok = k.sig('pe', nc.tensor.matmul(sb_.t[:], lhsT=kb_.t[r0:r0 + 64, kbi * 128:(kbi + 1) * 128], rhs=qb_.t[r0:r0 + 64, :], start=True, stop=True))
                                k.set_w(sb_, tok)
                                k.acq_r('act', sb_); k.acq_w('act', peb)
                                tok = k.sig('act', nc.scalar.activation(out=peb.t[:], in_=sb_.t[:], func=AF.Exp, scale=0.125))
                                k.add_r(sb_, tok); k.set_w(peb, tok)
                                k.acq_r('dve', peb); k.acq_w('dve', pmb)
                                tok = k.sig('dve', nc.vector.tensor_tensor(out=pmb.t[:], in0=peb.t[:], in1=mk.t[:, j0:j0 + 512], op=ALU.mult))
                                k.add_r(peb, tok); k.set_w(pmb, tok)
                                pend.append((pmb, kbi))
                                if len(pend) > LA:
                                    pb2, kb2 = pend.pop(0)
                                    k.acq_r('pe', pb2)
                                    tok = k.sig('pe', nc.tensor.matmul(accb.t[:], lhsT=vb_.t[:, kb2, hh * 128:(hh + 1) * 128], rhs=pb2.t[:], start=(kb2 == 0), stop=False))
                                    k.add_r(pb2, tok)
                            while pend:
                                pb2, kb2 = pend.pop(0)
                                k.acq_r('pe', pb2)
                                tok = k.sig('pe', nc.tensor.matmul(accb.t[:], lhsT=vb_.t[:, kb2, hh * 128:(hh + 1) * 128], rhs=pb2.t[:], start=(kb2 == 0), stop=(not pend)))
                                k.add_r(pb2, tok)
                            k.set_w(accb, tok)
                            nr = r0; dr = 64 - r0
                            rdb = rd[hh]; rd2b = rd2[hh]
                            k.acq_r('dve', accb); k.acq_w('dve', rdb)
                            tok = k.sig('dve', nc.vector.reciprocal(out=rdb.t[dr:dr + 64, :], in_=accb.t[dr:dr + 64, :]))
                            k.set_w(rdb, tok)
                            k.acq_r('act', rdb); k.acq_w('act', rd2b)
                            tok = k.sig('act', nc.scalar.activation(out=rd2b.t[nr:nr + 64, :], in_=rdb.t[dr:dr + 64, :], func=AF.Copy))
                            k.add_r(rdb, tok); k.set_w(rd2b, tok)
                            k.acq_r('dve', rd2b)
                            if hh == 0:
                                k.acq_w('dve', ab_)
                            tok = k.sig('dve', nc.vector.tensor_tensor(out=ab_.t[nr:nr + 64, :], in0=accb.t[nr:nr + 64, :], in1=rd2b.t[nr:nr + 64, :], op=ALU.mult))
                            k.add_r(accb, tok); k.add_r(rd2b, tok); k.set_w(ab_, tok, fresh=(hh == 0))
                        k.add_r(qb_, tok); k.add_r(kb_, tok); k.add_r(vb_, tok)
                        k.store('pool', ab_, seg.mix[(6 + hp) * 128:(7 + hp) * 128, q0:q0 + 512], ab_.t[:])
            if stop is not None and stop <= l * 10 + 3:
                continue
            with Phase(k) as ph:
                mb = ph.sb([128, 2, 2048], BF16, dma=True, name="mb")
                memT = ph.sb([128, 16, 256], BF16, name="memT")
                wkv = ph.sb([128, 8, 2048], BF16, dma=True, name="wkv")
                kmT = ph.sb([128, 4, 256], BF16, name="kmT")
                vm = ph.sb([128, 2, 512], BF16, name="vm")
                qmt = ph.ring(2, [128, 4, 512], BF16, dma=True, name="qmt")
                pe_t = ph.ring(3, [128, 512], BF16, name="pexp")
                rd = ph.ring(2, [128, 512], F32, name="rd")
                mo = ph.ring(2, [128, 512], BF16, dma=True, name="mo")
                tpm = ph.pring(2, [128, 4, 256], BF16, name="tpm")
                sp_ = ph.pring(2, [128, 512], F32, name="sps")
                np_ = ph.pring(2, [128, 512], F32, name="nps")
                dp_ = ph.pring(2, [128, 512], F32, name="dps")
                k.load('pool', mb, mb.t[:], seg.mem.rearrange("(t p) d -> p t d", p=128))
                for m in range(8):
                    k.load('sp', wkv, wkv.t[:, m, :], wkv_b[l][m], fresh=(m == 0))
                k.acq_r('pe', mb); k.acq_r('pe', wkv)
                for kg in range(4):
                    tp = tpm[kg % 2]
                    k.acq_w('pe', tp)
                    for kk in range(4):
                        for t in range(2):
                            ins = nc.tensor.transpose(tp.t[:, kk, t * 128:(t + 1) * 128], mb.t[:, t, (kg * 4 + kk) * 128:(kg * 4 + kk + 1) * 128], ident_b.t[:])
                    k.set_w(tp, k.sig('pe', ins))
                    k.acq_r('act', tp)
                    tok = k.sig('act', nc.scalar.activation(out=memT.t[:, kg * 4:(kg + 1) * 4, :], in_=tp.t[:], func=AF.Copy))
                    k.add_r(tp, tok); k.set_w(memT, tok, fresh=(kg == 0))
                k.acq_r('pe', memT)
                for mh in range(4):
                    pb = sp_[mh % 2]
                    k.acq_w('pe', pb)
                    for kk in range(16):
                        ins = nc.tensor.matmul(pb.t[:, 0:256], lhsT=wkv.t[:, mh, kk * 128:(kk + 1) * 128], rhs=memT.t[:, kk, :], start=(kk == 0), stop=(kk == 15))
                    k.set_w(pb, k.sig('pe', ins))
                    k.acq_r('act', pb)
                    tok = k.sig('act', nc.scalar.activation(out=kmT.t[:, mh, :], in_=pb.t[:, 0:256], func=AF.Copy))
                    k.add_r(pb, tok); k.set_w(kmT, tok, fresh=(mh == 0))
                i = 0
                for t in range(2):
                    for mh in range(4):
                        pb = np_[i % 2]; i += 1
                        k.acq_w('pe', pb)
                        for kk in range(16):
                            ins = nc.tensor.matmul(pb.t[:, 0:128], lhsT=memT.t[:, kk, t * 128:(t + 1) * 128], rhs=wkv.t[:, 4 + mh, kk * 128:(kk + 1) * 128], start=(kk == 0), stop=(kk == 15))
                        k.set_w(pb, k.sig('pe', ins))
                        k.acq_r('dve', pb)
                        tok = k.sig('dve', nc.vector.tensor_copy(out=vm.t[:, t, mh * 128:(mh + 1) * 128], in_=pb.t[:, 0:128]))
                        k.add_r(pb, tok); k.set_w(vm, tok, fresh=(i == 1))
                k.acq_r('pe', kmT); k.acq_r('pe', vm)
                nqb = (oe - os_) // 512
                sc = 0; it = 0
                for qb in range(nqb):
                    q0 = os_ + qb * 512
                    qb_ = qmt[qb % 2]
                    k.load('sp', qb_, qb_.t[:], seg.qm[:, q0:q0 + 512].rearrange("(h p) t -> p h t", p=128))
                    k.acq_r('pe', qb_)
                    for mh in range(4):
                        nb_ = np_[it % 2]; db_ = dp_[it % 2]; rdb = rd[it % 2]; ob = mo[it % 2]; it += 1
                        k.acq_w('pe', nb_); k.acq_w('pe', db_)
                        for t in range(2):
                            sb_ = sp_[sc % 2]; peb = pe_t[sc % 3]; sc += 1
                            k.acq_w('pe', sb_)
                            tok = k.sig('pe', nc.tensor.matmul(sb_.t[:], lhsT=kmT.t[:, mh, t * 128:(t + 1) * 128], rhs=qb_.t[:, mh, :], start=True, stop=True))
                            k.set_w(sb_, tok)
                            k.acq_r('act', sb_); k.acq_w('act', peb)
                            tok = k.sig('act', nc.scalar.activation(out=peb.t[:], in_=sb_.t[:], func=AF.Exp, scale=float(128 ** -0.5)))
                            k.add_r(sb_, tok); k.set_w(peb, tok)
                            k.acq_r('pe', peb)
                            nc.tensor.matmul(nb_.t[:], lhsT=vm.t[:, t, mh * 128:(mh + 1) * 128], rhs=peb.t[:], start=(t == 0), stop=(t == 1))
                            tok = k.sig('pe', nc.tensor.matmul(db_.t[:], lhsT=ones_b.t[:], rhs=peb.t[:], start=(t == 0), stop=(t == 1)))
                            k.add_r(peb, tok)
                        k.set_w(nb_, tok); k.set_w(db_, tok)
                        k.acq_r('dve', db_); k.acq_w('dve', rdb)
                        k.wait('dve', k.sig('dve', nc.vector.reciprocal(out=rdb.t[:], in_=db_.t[:])), force=True)
                        k.acq_w('dve', ob)
                        tok = k.sig('dve', nc.vector.tensor_tensor(out=ob.t[:], in0=nb_.t[:], in1=rdb.t[:], op=ALU.mult))
                        k.add_r(nb_, tok); k.add_r(db_, tok); k.set_w(ob, tok)
                        k.store('pool', ob, seg.mix[(12 + mh) * 128:(13 + mh) * 128, q0:q0 + 512], ob.t[:])
                    k.add_r(qb_, tok)
            if stop is not None and stop <= l * 10 + 4:
                continue
            with Phase(k) as ph:
                wo = ph.sb([128, 16, 2048], BF16, dma=True, name="wo")
                gb = ph.sb([128, 2, 2048], F32, dma=True, name="gb")
                mt = ph.ring(2, [128, 16, 512], BF16, dma=True, name="mixT")
                xr = ph.ring(2, [128, 2048], F32, dma=True, name="xr")
                yt = ph.ring(2, [128, 2048], F32, dma=True, name="yt")
                st = ph.sb([128, 4, 6], F32); mv = ph.sb([128, 2], F32); rs = ph.sb([128, 1], F32); nb = ph.sb([128, 1], F32)
                ops = ph.pring(8, [128, 512], F32, name="ops")
                k.load('sp', wo, wo.t[:], wout_b[l].rearrange("(k p) n -> p k n", p=128))
                k.load('sp', gb, gb.t[:, 0, :], ln1_g[l].partition_broadcast(128))
                k.load('sp', gb, gb.t[:, 1, :], ln1_b[l].partition_broadcast(128), fresh=False)
                k.acq_r('pe', wo); k.acq_r('dve', gb)
                nblk = (oe - os_) // 512
                oc = 0; ti = 0
                for b in range(nblk):
                    t0 = os_ + b * 512
                    mb_ = mt[b % 2]
                    k.load('sp', mb_, mb_.t[:], seg.mix[:, t0:t0 + 512].rearrange("(k p) t -> p k t", p=128))
                    k.acq_r('pe', mb_)
                    for t in range(4):
                        xb_ = xr[ti % 2]; yb = yt[ti % 2]; ti += 1
                        r0 = t0 + t * 128
                        k.load('sp', xb_, xb_.t[:], xin[r0:r0 + 128, :])
                        pbs = []
                        for ch in range(4):
                            pb = ops[oc % 8]; oc += 1
                            k.acq_w('pe', pb)
                            for kk in range(16):
                                ins = nc.tensor.matmul(pb.t[:], lhsT=mb_.t[:, kk, t * 128:(t + 1) * 128], rhs=wo.t[:, kk, ch * 512:(ch + 1) * 512], start=(kk == 0), stop=(kk == 15))
                            tokp = k.sig('pe', ins)
                            k.set_w(pb, tokp)
                            pbs.append(pb)
                        k.acq_r('dve', xb_); k.acq_w('dve', yb)
                        for ch in range(4):
                            k.acq_r('dve', pbs[ch])
                            tok = k.sig('dve', nc.vector.scalar_tensor_tensor(out=yb.t[:, ch * 512:(ch + 1) * 512], in0=xb_.t[:, ch * 512:(ch + 1) * 512], scalar=float(ALPHA), in1=pbs[ch].t[:], op0=ALU.mult, op1=ALU.add))
                            k.add_r(pbs[ch], tok)
                        k.add_r(xb_, tok)
                        tok = ln_inplace(yb.t[:], gb, (st, mv, rs, nb))
                        k.set_w(yb, tok)
                        k.store('pool', yb, seg.x1[r0:r0 + 128, :], yb.t[:])
                    k.add_r(mb_, tokp)
            if stop is not None and stop <= l * 10 + 5:
                continue
            is_moe = (l % 2 == 1)
            if is_moe:
                with Phase(k) as ph:
                    rt = ph.sb([128, 16, NE], F32, dma=True, name="rt")
                    xr = ph.ring(2, [128, 2048], F32, dma=True, name="xr")
                    xT32 = ph.ring(2, [128, 16, 128], F32, name="xT32")
                    lg = ph.ring(2, [128, NE], F32, name="lg")
                    m8 = ph.sb([128, 8], F32); nv1 = ph.sb([128, 1], F32); ex = ph.sb([128, 8], F32); msk = ph.sb([128, 8], F32)
                    den = ph.sb([128, 1], F32); rden = ph.sb([128, 1], F32)
                    cb = ph.ring(2, [128, NE], F32, dma=True, name="cb")
                    tps = ph.pring(4, [128, 4, 128], F32, name="tps")
                    lps = ph.pring(2, [128, NE], F32, name="lps")
                    k.load('sp', rt, rt.t[:], router[0].rearrange("(k p) e -> p k e", p=128))
                    k.acq_r('pe', rt)
                    ntile = (oe - os_) // 128
                    tc_ = 0
                    for ti in range(ntile):
                        r0 = os_ + ti * 128
                        xb_ = xr[ti % 2]; xtb = xT32[ti % 2]; lgb = lg[ti % 2]; cbb = cb[ti % 2]; lp = lps[ti % 2]
                        k.load('sp', xb_, xb_.t[:], seg.x1[r0:r0 + 128, :])
                        k.acq_r('pe', xb_)
                        for kg in range(4):
                            tp = tps[tc_ % 4]; tc_ += 1
                            k.acq_w('pe', tp)
                            for kk in range(4):
                                ins = nc.tensor.transpose(tp.t[:, kk, :], xb_.t[:, (kg * 4 + kk) * 128:(kg * 4 + kk + 1) * 128], ident_f.t[:])
                            tokp = k.sig('pe', ins)
                            k.set_w(tp, tokp)
                            e = 'act' if kg % 2 == 0 else 'dve'
                            k.acq_r(e, tp); k.acq_w(e, xtb)
                            if e == 'act':
                                ins = nc.scalar.activation(out=xtb.t[:, kg * 4:(kg + 1) * 4, :], in_=tp.t[:], func=AF.Copy)
                            else:
                                ins = nc.vector.tensor_copy(out=xtb.t[:, kg * 4:(kg + 1) * 4, :], in_=tp.t[:])
                            tok = k.sig(e, ins)
                            k.add_r(tp, tok); k.set_w(xtb, tok, fresh=(kg == 0))
                        k.add_r(xb_, tokp)
                        k.acq_r('pe', xtb); k.acq_w('pe', lp)
                        for kk in range(16):
                            ins = nc.tensor.matmul(lp.t[:], lhsT=xtb.t[:, kk, :], rhs=rt.t[:, kk, :], start=(kk == 0), stop=(kk == 15))
                        tokp = k.sig('pe', ins)
                        k.set_w(lp, tokp); k.add_r(xtb, tokp)
                        k.acq_r('dve', lp); k.acq_w('dve', lgb); k.acq_w('dve', cbb)
                        k.wait('dve', k.sig('dve', nc.vector.tensor_copy(out=lgb.t[:], in_=lp.t[:])), force=True)
                        tok = k.sig('dve', nc.vector.max(out=m8.t[:], in_=lgb.t[:]))
                        k.wait('dve', tok, force=True)
                        nc.vector.tensor_scalar(out=msk.t[:], in0=lgb.t[:], scalar1=m8.t[:, 1:2], scalar2=None, op0=ALU.is_ge)
                        tok = k.sig('dve', nc.vector.tensor_scalar(out=nv1.t[:], in0=m8.t[:, 0:1], scalar1=-1.0, scalar2=None, op0=ALU.mult))
                        k.add_r(lp, tok)
                        k.wait('act', tok)
                        toka = k.sig('act', nc.scalar.activation(out=ex.t[:], in_=lgb.t[:], func=AF.Exp, bias=nv1.t[:, 0:1], scale=1.0))
                        k.wait('dve', toka)
                        k.wait('dve', k.sig('dve', nc.vector.tensor_tensor(out=ex.t[:], in0=ex.t[:], in1=msk.t[:], op=ALU.mult)), force=True)
                        k.wait('dve', k.sig('dve', nc.vector.tensor_reduce(out=den.t[:], in_=ex.t[:], axis=mybir.AxisListType.X, op=ALU.add)), force=True)
                        tok = k.sig('dve', nc.vector.reciprocal(out=rden.t[:], in_=den.t[:]))
                        k.wait('dve', tok, force=True)
                        tok = k.sig('dve', nc.vector.tensor_scalar(out=cbb.t[:], in0=ex.t[:], scalar1=rden.t[:, 0:1], scalar2=None, op0=ALU.mult))
                        k.set_w(cbb, tok); k.set_w(lgb, tok)
                        k.store('pool', cbb, seg.comb[r0:r0 + 128, :], cbb.t[:])
            last = (l == DEPTH - 1)
            if is_moe:
                continue
            with Phase(k) as ph:
                nexp = NE if is_moe else 1
                NF = (E_FF if is_moe else D_FF) // 128
                FG = 4
                xb = ph.sb([128, 4, 2048], BF16, dma=True, name="xb")
                xT = ph.sb([128, 16, 512], BF16, name="xT")
                gT = ph.sb([128, NF, 512], BF16, name="gT")
                accs = ph.sb([128, 4, 2048], F32, dma=True, name="acc")
                w13 = ph.ring(2, [128, 2, 2048], BF16, dma=True, name="w13")
                w2r = ph.ring(3, [128, FG, 512], BF16, dma=True, name="w2r")
                xr = ph.sb([128, 2048], F32, dma=True, name="xr")
                gb = ph.sb([128, 2, 2048], F32, dma=True, name="gb")
                stt = ph.ring(2, [128, 512], F32, name="silu")
                cbt = ph.sb([128, 4, NE], F32, dma=True, name="cbt")
                vt = ph.sb([128, W // 128], F32, dma=True, name="vt")
                st = ph.sb([128, 4, 6], F32); mv = ph.sb([128, 2], F32); rs = ph.sb([128, 1], F32); nb = ph.sb([128, 1], F32)
                tpr = ph.pring(1, [128, 4, 512], BF16, name="tp")
                hps = ph.pring(2, [128, 512], F32, name="hps")
                ops = ph.pring(4, [128, 512], F32, name="ops")
                k.load('sp', vt, vt.t[:], seg.valid[:, :])
                k.load('sp', gb, gb.t[:, 0, :], (ln2_g if True else ln1_g)[l].partition_broadcast(128))
                k.load('sp', gb, gb.t[:, 1, :], ln2_b[l].partition_broadcast(128), fresh=False)
                k.acq_r('dve', gb); k.acq_r('dve', vt)
                nblk = (oe - os_) // 512
                cnt = [0]
                wc = 0; w2c = 0; hc = 0
                for b in range(nblk):
                    t0 = os_ + b * 512
                    load_xT(ph, seg.x1[t0:t0 + 512, :], xb, xT, tpr, cnt)
                    if is_moe:
                        k.load('sp', cbt, cbt.t[:], seg.comb[t0:t0 + 512, :].rearrange("(t p) e -> p t e", p=128))
                        k.acq_r('dve', cbt)
                    for e in range(nexp):
                        w1s = f1_b; w3s = f3_b; w2s = f2_b
                        for f in range(NF):
                            bg(1)
                            wb = w13[wc % 2]; wc += 1
                            k.load('sp', wb, wb.t[:, 0, :], w1s[f])
                            k.load('sp', wb, wb.t[:, 1, :], w3s[f], fresh=False)
                            k.acq_r('pe', wb); k.acq_r('pe', xT)
                            h1 = hps[0]; h3 = hps[1]
                            k.acq_w('pe', h1)
                            for kk in range(16):
                                ins = nc.tensor.matmul(h1.t[:], lhsT=wb.t[:, 0, kk * 128:(kk + 1) * 128], rhs=xT.t[:, kk, :], start=(kk == 0), stop=(kk == 15))
                            k.set_w(h1, k.sig('pe', ins))
                            k.acq_w('pe', h3)
                            for kk in range(16):
                                ins = nc.tensor.matmul(h3.t[:], lhsT=wb.t[:, 1, kk * 128:(kk + 1) * 128], rhs=xT.t[:, kk, :], start=(kk == 0), stop=(kk == 15))
                            tokp = k.sig('pe', ins)
                            k.set_w(h3, tokp); k.add_r(wb, tokp)
                            sb_ = stt[hc % 2]; hc += 1
                            k.acq_r('act', h1); k.acq_w('act', sb_)
                            tok = k.sig('act', nc.scalar.activation(out=sb_.t[:], in_=h1.t[:], func=AF.Silu))
                            k.add_r(h1, tok); k.set_w(sb_, tok)
                            k.acq_r('dve', sb_); k.acq_r('dve', h3)
                            if f == 0:
                                k.acq_w('dve', gT)
                            tok = k.sig('dve', nc.vector.tensor_tensor(out=gT.t[:, f, :], in0=sb_.t[:], in1=h3.t[:], op=ALU.mult))
                            k.add_r(sb_, tok); k.add_r(h3, tok); k.set_w(gT, tok, fresh=(f == 0))
                        if e == nexp - 1:
                            k.add_r(xT, tokp)
                        k.acq_r('pe', gT)
                        for dch in range(4):
                            for t in range(4):
                                k.acq_w('pe', ops[t])
                            for fg in range(NF // FG):
                                wb = w2r[w2c % 3]; w2c += 1
                                k.load('sp', wb, wb.t[:], w2s[fg * FG * 128:(fg + 1) * FG * 128, dch * 512:(dch + 1) * 512].rearrange("(f p) d -> p f d", p=128))
                                k.acq_r('pe', wb)
                                for fi in range(FG):
                                    f = fg * FG + fi
                                    for t in range(4):
                                        ins = nc.tensor.matmul(ops[t].t[:], lhsT=gT.t[:, f, t * 128:(t + 1) * 128], rhs=wb.t[:, fi, :], start=(f == 0), stop=(f == NF - 1))
                                tokp = k.sig('pe', ins)
                                k.add_r(wb, tokp)
                            for t in range(4):
                                k.set_w(ops[t], tokp)
                            if e == 0 and dch == 0:
                                k.acq_w('dve', accs)
                            for t in range(4):
                                k.acq_r('dve', ops[t])
                                dst = accs.t[:, t, dch * 512:(dch + 1) * 512]
                                if not is_moe:
                                    ins = nc.vector.tensor_copy(out=dst, in_=ops[t].t[:])
                                elif e == 0:
                                    ins = nc.vector.tensor_scalar(out=dst, in0=ops[t].t[:], scalar1=cbt.t[:, t, e:e + 1], scalar2=None, op0=ALU.mult)
                                else:
                                    ins = nc.vector.scalar_tensor_tensor(out=dst, in0=ops[t].t[:], scalar=cbt.t[:, t, e:e + 1], in1=dst, op0=ALU.mult, op1=ALU.add)
                                tok = k.sig('dve', ins)
                                k.add_r(ops[t], tok)
                            k.set_w(accs, tok, fresh=(e == 0 and dch == 0))
                        k.add_r(gT, tokp)
                    if is_moe:
                        k.add_r(cbt, tok)
                    for t in range(4):
                        r0 = t0 + t * 128
                        k.load('sp', xr, xr.t[:], seg.x1[r0:r0 + 128, :])
                        k.acq_r('dve', xr)
                        yt = accs.t[:, t, :]
                        tok = k.sig('dve', nc.vector.scalar_tensor_tensor(out=yt, in0=xr.t[:], scalar=float(ALPHA), in1=yt, op0=ALU.mult, op1=ALU.add))
                        k.add_r(xr, tok)
                        va = None if last else vt.t[:, r0 // 128:r0 // 128 + 1]
                        tok = ln_inplace(yt, gb, (st, mv, rs, nb), valid_ap=va)
                        k.set_w(accs, tok, fresh=False)
                        if last:
                            if os_ <= r0 < oe:
                                k.store('pool', accs, seg.yout[r0 - seg.yoff:r0 - seg.yoff + 128, :], yt)
                        else:
                            k.store('pool', accs, seg.x2[r0:r0 + 128, :], yt)

        if l % 2 == 1 and (stop is None or stop > l * 10 + 5):
            I32 = mybir.dt.int32
            AX = mybir.AxisListType.X
            tiles = []
            for seg in segs:
                o0, o1 = seg.orr[l]
                for r0 in range(o0, o1, 128):
                    tiles.append((seg, r0))
            NT = len(tiles)
            NB = NT // 2 + NE
            NROW = NB * 512
            xsort = dscr(f"xsort{l}", [NROW + 128, D], BF16)
            ysort = dscr(f"ysort{l}", [NROW + 128, D], F32)
            outer = ExitStack()

            def osb(shape, dt, name):
                k.uid += 1
                return Buf(outer.enter_context(nc.sbuf_tensor(f"{name}{k.uid}", list(shape), dt)))

            IDX = osb([128, NT * 2], I32, "IDX")
            G12 = osb([128, NT, 2], F32, "G12")
            WI13 = osb([128, NB], I32, "WI13")
            WI2 = osb([128, NB * 4], I32, "WI2")

            def fw(ins):
                tok = k.sig('dve', ins)
                k.wait('dve', tok, force=True)
                return tok

            with Phase(k) as ph:
                U = ph.sb([128, 128], F32, dma=True, name="U")
                iot = ph.sb([128, 1], F32, dma=True, name="iot")
                CB = ph.sb([128, NT, 8], F32, dma=True, name="CB")
                Mt = ph.sb([128, NT, 8], F32, name="Mt")
                RK = ph.sb([128, NT, 8], F32, name="RK")
                carry = ph.sb([128, 8], F32); cmpn = ph.sb([128, 8, 12], F32); pe_ = ph.sb([128, 8], F32)
                o1_ = ph.sb([128, 8], F32); end_ = ph.sb([128, 8], F32)
                cmpE = ph.sb([128, NB, 8], F32); EJ = ph.sb([128, NB], F32); wf = ph.sb([128, NB], F32)
                key = ph.sb([128, 8], F32); m8 = ph.sb([128, 8], F32); eq = ph.sb([128, 8], F32)
                rps = ph.pring(2, [128, 8], F32, name="rps"); cps = ph.pring(2, [128, 8], F32, name="cps")
                k.load('sp', U, U.t[:], triud[:, :])
                k.load('sp', iot, iot.t[:], iotad[:, :])
                i = 0
                for seg in segs:
                    o0, o1 = seg.orr[l]
                    n = (o1 - o0) // 128
                    k.load('sp', CB, CB.t[:, i:i + n, :], seg.comb[o0:o1, :].rearrange("(t p) e -> p t e", p=128), fresh=(i == 0))
                    i += n
                k.acq_r('dve', CB); k.acq_r('dve', iot); k.acq_r('pe', U)
                tokM = fw(nc.vector.tensor_scalar(out=Mt.t[:], in0=CB.t[:], scalar1=0.0, scalar2=None, op0=ALU.is_gt))
                k.wait('pe', tokM)
                fw(nc.vector.memset(carry.t[:], 0.0))
                for i in range(NT):
                    rp = rps[i % 2]; cp = cps[i % 2]
                    k.acq_w('pe', rp); k.acq_w('pe', cp)
                    nc.tensor.matmul(rp.t[:], lhsT=U.t[:], rhs=Mt.t[:, i, :], start=True, stop=True)
                    tok = k.sig('pe', nc.tensor.matmul(cp.t[:], lhsT=ones_f.t[:], rhs=Mt.t[:, i, :], start=True, stop=True))
                    k.set_w(rp, tok); k.set_w(cp, tok)
                    k.acq_r('dve', rp)
                    nc.vector.tensor_tensor(out=RK.t[:, i, :], in0=rp.t[:], in1=carry.t[:], op=ALU.add)
                    tok = fw(nc.vector.tensor_tensor(out=carry.t[:], in0=cp.t[:], in1=carry.t[:], op=ALU.add))
                    k.add_r(rp, tok); k.add_r(cp, tok)
                for j in range(12):
                    ins = nc.vector.tensor_scalar(out=cmpn.t[:, :, j], in0=carry.t[:], scalar1=float(512 * j), scalar2=None, op0=ALU.is_gt)
                fw(ins)
                fw(nc.vector.tensor_reduce(out=pe_.t[:], in_=cmpn.t[:], axis=AX, op=ALU.add))
                fw(nc.vector.tensor_scalar(out=pe_.t[:], in0=pe_.t[:], scalar1=512.0, scalar2=None, op0=ALU.mult))
                fw(nc.vector.tensor_copy(out=end_.t[:, 0:1], in_=pe_.t[:, 0:1]))
                for e in range(1, NE):
                    fw(nc.vector.tensor_tensor(out=end_.t[:, e:e + 1], in0=end_.t[:, e - 1:e], in1=pe_.t[:, e:e + 1], op=ALU.add))
                fw(nc.vector.tensor_tensor(out=o1_.t[:], in0=end_.t[:], in1=pe_.t[:], op=ALU.subtract))
                fw(nc.vector.tensor_scalar(out=o1_.t[:], in0=o1_.t[:], scalar1=1.0, scalar2=None, op0=ALU.add))
                for j in range(NB):
                    ins = nc.vector.tensor_scalar(out=cmpE.t[:, j, :], in0=end_.t[:], scalar1=float(512 * j), scalar2=None, op0=ALU.is_le)
                fw(ins)
                fw(nc.vector.tensor_reduce(out=EJ.t[:], in_=cmpE.t[:], axis=AX, op=ALU.add))
                fw(nc.vector.tensor_scalar(out=EJ.t[:], in0=EJ.t[:], scalar1=float(NE - 1), scalar2=None, op0=ALU.min))
                fw(nc.vector.tensor_scalar(out=wf.t[:], in0=EJ.t[:], scalar1=float(NFH * 128), scalar2=iot.t[:, 0:1], op0=ALU.mult, op1=ALU.add))
                fw(nc.vector.tensor_copy(out=WI13.t[:], in_=wf.t[:]))
                for dch in range(4):
                    fw(nc.vector.tensor_scalar(out=wf.t[:], in0=EJ.t[:], scalar1=float(4 * NFG * 128), scalar2=float(dch * NFG * 128), op0=ALU.mult, op1=ALU.add))
                    fw(nc.vector.tensor_scalar(out=wf.t[:], in0=wf.t[:], scalar1=iot.t[:, 0:1], scalar2=None, op0=ALU.add))
                    fw(nc.vector.tensor_copy(out=WI2.t[:].rearrange("p (b d) -> p b d", d=4)[:, :, dch], in_=wf.t[:]))
                for i in range(NT):
                    fw(nc.vector.tensor_tensor(out=key.t[:], in0=RK.t[:, i, :], in1=o1_.t[:], op=ALU.add))
                    fw(nc.vector.tensor_tensor(out=key.t[:], in0=key.t[:], in1=Mt.t[:, i, :], op=ALU.mult))
                    fw(nc.vector.tensor_scalar(out=key.t[:], in0=key.t[:], scalar1=-1.0, scalar2=None, op0=ALU.add))
                    fw(nc.vector.max(out=m8.t[:], in_=key.t[:]))
                    fw(nc.vector.tensor_scalar(out=eq.t[:, 0:2], in0=m8.t[:, 0:2], scalar1=0.0, scalar2=float(NROW + 1), op0=ALU.is_lt, op1=ALU.mult))
                    fw(nc.vector.tensor_tensor(out=eq.t[:, 0:2], in0=eq.t[:, 0:2], in1=m8.t[:, 0:2], op=ALU.add))
                    fw(nc.vector.tensor_copy(out=IDX.t[:, 2 * i:2 * i + 2], in_=eq.t[:, 0:2]))
                    for c in range(2):
                        fw(nc.vector.tensor_scalar(out=eq.t[:], in0=key.t[:], scalar1=m8.t[:, c:c + 1], scalar2=None, op0=ALU.is_equal))
                        fw(nc.vector.tensor_tensor(out=eq.t[:], in0=eq.t[:], in1=CB.t[:, i, :], op=ALU.mult))
                        fw(nc.vector.tensor_reduce(out=G12.t[:, i, c:c + 1], in_=eq.t[:], axis=AX, op=ALU.add))
            with Phase(k) as ph:
                xr = ph.ring(3, [128, 2048], BF16, dma=True, name="xsc")
                for i, (seg, r0) in enumerate(tiles):
                    b = xr[i % 3]
                    k.load('pool', b, b.t[:], seg.x1[r0:r0 + 128, :])
                    k.acq_r('pool', b)
                    for c in range(2):
                        ins = nc.gpsimd.indirect_dma_start(out=xsort[:, :], out_offset=bass.IndirectOffsetOnAxis(ap=IDX.t[:, 2 * i + c:2 * i + c + 1], axis=0),
                                                           in_=b.t[:], in_offset=None)
                        ins.then_inc(b.sem.h, 16)
                        b.sem.n += 16
                        tok = (b.sem, b.sem.n, None)
                        k.add_r(b, tok)
                        k.pending.append(('pool', tok))
            bg(len(bgq))
            for q in ('sp', 'pool'):
                k.wait(q, bgtok.get(wsemB.name))
            with Phase(k) as ph:
                xb = ph.sb([128, 4, 2048], BF16, dma=True, name="xb")
                xT = ph.sb([128, 16, 512], BF16, name="xT")
                gT = ph.sb([128, NFE, 512], BF16, name="gT")
                w13 = ph.ring(3, [128, 4096], BF16, dma=True, name="w13")
                w2r = ph.ring(4, [128, FG, 512], BF16, dma=True, name="w2r")
                yst = ph.ring(2, [128, 4, 512], F32, dma=True, name="yst")
                stt = ph.ring(2, [128, 512], F32, name="silu")
                tpr = ph.pring(1, [128, 4, 512], BF16, name="tp")
                hps = ph.pring(2, [128, 512], F32, name="hps")
                ops = ph.pring(4, [128, 512], F32, name="ops")
                cnt = [0]; wc = 0; w2c = 0; hc = 0; yc = 0
                for j in range(NB):
                    load_xT(ph, xsort[j * 512:(j + 1) * 512, :], xb, xT, tpr, cnt)
                    for f in range(NFE):
                        wb = w13[wc % 3]; wc += 1
                        k.iload(wb, wb.t[:], m13_h[f // NFH][:, :], WI13.t[:, j:j + 1], elem_off=(f % NFH) * 128 * 4096)
                        k.acq_r('pe', wb); k.acq_r('pe', xT)
                        h1 = hps[0]; h3 = hps[1]
                        k.acq_w('pe', h1)
                        for kk in range(16):
                            ins = nc.tensor.matmul(h1.t[:], lhsT=wb.t[:, kk * 128:(kk + 1) * 128], rhs=xT.t[:, kk, :], start=(kk == 0), stop=(kk == 15))
                        k.set_w(h1, k.sig('pe', ins))
                        k.acq_w('pe', h3)
                        for kk in range(16):
                            ins = nc.tensor.matmul(h3.t[:], lhsT=wb.t[:, 2048 + kk * 128:2048 + (kk + 1) * 128], rhs=xT.t[:, kk, :], start=(kk == 0), stop=(kk == 15))
                        tokp = k.sig('pe', ins)
                        k.set_w(h3, tokp); k.add_r(wb, tokp)
                        sb_ = stt[hc % 2]; hc += 1
                        k.acq_r('act', h1); k.acq_w('act', sb_)
                        tok = k.sig('act', nc.scalar.activation(out=sb_.t[:], in_=h1.t[:], func=AF.Silu))
                        k.add_r(h1, tok); k.set_w(sb_, tok)
                        k.acq_r('dve', sb_); k.acq_r('dve', h3)
                        if f == 0:
                            k.acq_w('dve', gT)
                        tok = k.sig('dve', nc.vector.tensor_tensor(out=gT.t[:, f, :], in0=sb_.t[:], in1=h3.t[:], op=ALU.mult))
                        k.add_r(sb_, tok); k.add_r(h3, tok); k.set_w(gT, tok, fresh=(f == 0))
                    k.add_r(xT, tokp)
                    k.acq_r('pe', gT)
                    for dch in range(4):
                        for t in range(4):
                            k.acq_w('pe', ops[t])
                        for fg in range(NFG):
                            wb = w2r[w2c % 4]; w2c += 1
                            k.iload(wb, wb.t[:].rearrange("p f c -> p (f c)"), m2_b[:, :], WI2.t[:, 4 * j + dch:4 * j + dch + 1], elem_off=fg * 128 * 2048)
                            k.acq_r('pe', wb)
                            for fi in range(FG):
                                f = fg * FG + fi
                                for t in range(4):
                                    ins = nc.tensor.matmul(ops[t].t[:], lhsT=gT.t[:, f, t * 128:(t + 1) * 128], rhs=wb.t[:, fi, :], start=(f == 0), stop=(f == NFE - 1))
                            tokp = k.sig('pe', ins)
                            k.add_r(wb, tokp)
                        ys = yst[yc % 2]; yc += 1
                        for t in range(4):
                            k.set_w(ops[t], tokp)
                        for t in range(4):
                            e_ = 'act' if t % 2 == 0 else 'dve'
                            k.acq_r(e_, ops[t]); k.acq_w(e_, ys)
                            if e_ == 'act':
                                ins = nc.scalar.activation(out=ys.t[:, t, :], in_=ops[t].t[:], func=AF.Copy)
                            else:
                                ins = nc.vector.tensor_copy(out=ys.t[:, t, :], in_=ops[t].t[:])
                            tok = k.sig(e_, ins)
                            k.add_r(ops[t], tok); k.set_w(ys, tok, fresh=(t == 0))
                        k.store('sp', ys, ysort[j * 512:(j + 1) * 512, dch * 512:(dch + 1) * 512].rearrange("(t p) c -> p t c", p=128), ys.t[:])
                    k.add_r(gT, tokp)
            with Phase(k) as ph:
                gb = ph.sb([128, 2, 2048], F32, dma=True, name="gb")
                Y1 = ph.ring(2, [128, 2048], F32, dma=True, name="Y1")
                Y2 = ph.ring(2, [128, 2048], F32, dma=True, name="Y2")
                xr = ph.ring(2, [128, 2048], F32, dma=True, name="xr")
                st = ph.sb([128, 4, 6], F32); mv = ph.sb([128, 2], F32); rs = ph.sb([128, 1], F32); nb = ph.sb([128, 1], F32)
                k.load('sp', gb, gb.t[:, 0, :], ln2_g[l].partition_broadcast(128))
                k.load('sp', gb, gb.t[:, 1, :], ln2_b[l].partition_broadcast(128), fresh=False)
                k.acq_r('dve', gb)
                zt_ = Y1[0]
                k.set_w(zt_, k.sig('dve', nc.vector.memset(zt_.t[:], 0.0)))
                ztok = k.store('sp', zt_, ysort[NROW:NROW + 128, :], zt_.t[:])
                k.wait('pool', ztok)
                for i, (seg, r0) in enumerate(tiles):
                    y1 = Y1[i % 2]; y2 = Y2[i % 2]; xb_ = xr[i % 2]
                    for yb, c in ((y1, 0), (y2, 1)):
                        k.iload(yb, yb.t[:], ysort[:, :], IDX.t[:, 2 * i + c:2 * i + c + 1])
                    k.load('sp', xb_, xb_.t[:], seg.x1[r0:r0 + 128, :])
                    k.acq_r('dve', y1); k.acq_r('dve', y2); k.acq_r('dve', xb_)
                    nc.vector.tensor_scalar(out=y1.t[:], in0=y1.t[:], scalar1=G12.t[:, i, 0:1], scalar2=None, op0=ALU.mult)
                    nc.vector.scalar_tensor_tensor(out=y1.t[:], in0=y2.t[:], scalar=G12.t[:, i, 1:2], in1=y1.t[:], op0=ALU.mult, op1=ALU.add)
                    tok = k.sig('dve', nc.vector.scalar_tensor_tensor(out=y1.t[:], in0=xb_.t[:], scalar=float(ALPHA), in1=y1.t[:], op0=ALU.mult, op1=ALU.add))
                    k.add_r(y2, tok); k.add_r(xb_, tok)
                    tok = ln_inplace(y1.t[:], gb, (st, mv, rs, nb))
                    k.set_w(y1, tok, fresh=False)
                    k.store('sp', y1, seg.yout[r0 - seg.yoff:r0 - seg.yoff + 128, :], y1.t[:])
            outer.close()
    return nc


def _host_prep(inputs):
    f32 = np.float32
    w_in = np.asarray(inputs["w_in"], f32)
    L = w_in.shape[0]
    idx = np.arange(64)
    perm = idx.copy()
    perm[0:8] = idx[8:16]
    perm[8:16] = idx[0:8]
    qcols = 1536 + (np.arange(12)[:, None] * 64 + perm[None, :]).reshape(-1)
    kcols = 2304 + (np.arange(12)[:, None] * 64 + perm[None, :]).reshape(-1)
    w_in_ext = np.concatenate([w_in, w_in[:, :, qcols], w_in[:, :, kcols]], axis=-1)
    cpar = np.zeros((L, 128, 6, 34), f32)
    for l in range(L):
        cpar[l, :, :, 0:31] = np.asarray(inputs["conv_w"], f32)[l].T.reshape(6, 128, 31).transpose(1, 0, 2)
        cpar[l, :, :, 31] = np.asarray(inputs["conv_b"], f32)[l].reshape(6, 128).T
        cpar[l, :, :, 32] = np.asarray(inputs["conv_ln_g"], f32)[l].reshape(6, 128).T
        cpar[l, :, :, 33] = np.asarray(inputs["conv_ln_b"], f32)[l].reshape(6, 128).T
    shared = {
        "w_in_ext": np.ascontiguousarray(w_in_ext), "cpar": cpar,
        "maskd": _mult_mask(), "identd": np.eye(128, dtype=f32),
        "triud": np.triu(np.ones((128, 128), f32), 1), "iotad": np.arange(128, dtype=f32).reshape(128, 1),
        "cs_p": _rope_tables(np.arange(PW)), "valid_p": np.ones((128, PW // 128), f32),
    }
    for n in ("w_mem_kv", "w_out", "ln1_g", "ln1_b", "ln2_g", "ln2_b", "ffn_w1", "ffn_w3", "ffn_w2",
              "moe_router", "moe_w1", "moe_w3", "moe_w2"):
        shared[n] = np.ascontiguousarray(np.asarray(inputs[n], f32))
    xp = np.asarray(inputs["x_prompt"], f32)
    xs = np.asarray(inputs["x_sample"], f32)
    mp = np.asarray(inputs["mem_prompt"], f32)
    ms = np.asarray(inputs["mem_sample"], f32)
    in_maps = []
    for c in range(NCORE):
        sq, j = c // 4, c % 4
        a = j * 4096
        lo = a - 2048
        pos = np.arange(lo, lo + SW)
        ok = (pos >= 0) & (pos < 16384)
        xw = np.zeros((SW, D), f32)
        xw[ok] = xs[sq, pos[ok]]
        m = dict(shared)
        m["xp"] = np.ascontiguousarray(xp[c])
        m["xs"] = xw
        m["memp"] = np.ascontiguousarray(mp[c])
        m["mems"] = np.ascontiguousarray(ms[sq])
        m["valid_s"] = np.ascontiguousarray(ok.astype(f32).reshape(SW // 128, 128).T)
        m["cs_s"] = _rope_tables(np.where(ok, pos, 0))
        in_maps.append(m)
    return in_maps


def kernel(**inputs):
    in_maps = _host_prep(inputs)
    nc = build()
    res = run_bass_kernel_spmd(nc, in_maps, core_ids=list(range(NCORE)))
    yp = np.stack([res.results[c]["yp"] for c in range(NCORE)], axis=0)
    ysf = np.zeros((2, 16384, D), np.float32)
    for c in range(NCORE):
        sq, j = c // 4, c % 4
        ysf[sq, j * 4096:(j + 1) * 4096] = res.results[c]["ys"]
    return (yp.astype(np.float32), ysf)
```

```python
import numpy as np
from contextlib import ExitStack
import concourse.bass as bass
import concourse.mybir as mybir
from concourse.bass_utils import run_bass_kernel_spmd

F32 = mybir.dt.float32
BF16 = mybir.dt.bfloat16
AF = mybir.ActivationFunctionType
ALU = mybir.AluOpType

D = 2048
DEPTH = 2
NCORE = 8
D_CONV = 768
D_ATT = 768
D_MEM = 512
D_IN = 4352
D_EXT = D_IN + 2 * D_ATT
NMT = D_EXT // 128
D_FF = 5632
E_FF = 7168
NE = 8
ALPHA = (2 * DEPTH) ** 0.25
LN_EPS = 1e-5
ROPE_THETA = 500000.0
MASK_D0 = 1408
MASK_J = 2944
SW = 8192
PW = 2048


def _mult_mask():
    kk = np.arange(128)[:, None]
    j = np.arange(MASK_J)[None, :]
    o = kk - j + MASK_D0
    ao = np.abs(o)
    c = (ao <= 64).astype(np.float32) + ((o % 4 == 0) & (ao <= 256)) + ((o % 16 == 0) & (ao <= 1024))
    return c.astype(np.float32)


def _rope_tables(pos):
    half = 8
    inv = np.power(np.float32(ROPE_THETA), -np.arange(0, 16, 2, dtype=np.float32) / np.float32(16)).astype(np.float32)
    ang = pos.astype(np.float32)[None, :] * inv[:, None]
    cos = np.cos(ang).astype(np.float32)
    sin = np.sin(ang).astype(np.float32)
    W = pos.shape[0]
    C = np.ones((128, W), np.float32)
    S = np.zeros((128, W), np.float32)
    for hh in range(2):
        b = hh * 64
        C[b:b + 8] = cos
        C[b + 8:b + 16] = cos
        S[b:b + 8] = -sin
        S[b + 8:b + 16] = sin
    return np.stack([C, S], axis=0)


class Sem:
    def __init__(self, nc, name):
        self.h = nc.semaphore(name).__enter__()
        self.n = 0
        self.name = name


class Buf:
    def __init__(self, tile, sem=None):
        self.t = tile
        self.rd = []
        self.wr = []
        self.sem = sem


class KB:
    def __init__(self):
        self.nc = bass.Bass("TRN2", target_bir_lowering=False)
        nc = self.nc
        self.eng = {'pe': nc.tensor, 'act': nc.scalar, 'dve': nc.vector, 'pool': nc.gpsimd, 'sp': nc.sync}
        self.esem = {e: Sem(nc, "e_" + e) for e in ('pe', 'act', 'dve', 'pool')}
        self.dsems = [Sem(nc, f"d{i}") for i in range(72)]
        self.dfree = list(self.dsems)
        self.waited = {}
        self.pending = []
        self.uid = 0

    def sig(self, e, ins):
        s = self.esem[e]
        ins.then_inc(s.h, 1)
        s.n += 1
        return (s, s.n, e)

    def wait(self, e, tok, force=False):
        if tok is None or (tok[2] == e and not force):
            return
        key = (e, tok[0].name)
        if self.waited.get(key, 0) >= tok[1]:
            return
        self.waited[key] = tok[1]
        self.eng[e].wait_ge(tok[0].h, tok[1])

    def dma(self, q, out, in_, sem):
        ins = self.eng[q].dma_start(out=out, in_=in_)
        ins.then_inc(sem.h, 16)
        sem.n += 16
        return (sem, sem.n, None)

    def getsem(self):
        return self.dfree.pop()

    def acq_w(self, e, b):
        for t in b.rd + b.wr:
            self.wait(e, t)

    def set_w(self, b, tok, fresh=True):
        if fresh:
            b.rd = []
            b.wr = [tok]
        else:
            b.wr.append(tok)

    def acq_r(self, e, b):
        for t in b.wr:
            self.wait(e, t)

    def add_r(self, b, tok):
        b.rd.append(tok)

    def load(self, q, b, out, in_, fresh=True):
        self.acq_w(q, b)
        tok = self.dma(q, out, in_, b.sem)
        self.set_w(b, tok, fresh)
        return tok

    def iload(self, b, out, in_, idx_ap, elem_off=0, fresh=True, bounds=None):
        self.acq_w('pool', b)
        kw = {}
        if bounds is not None:
            kw = dict(bounds_check=bounds, oob_is_err=False)
        ins = self.nc.gpsimd.indirect_dma_start(out=out, out_offset=None, in_=in_,
                                                in_offset=bass.IndirectOffsetOnAxis(ap=idx_ap, axis=0),
                                                element_offset=elem_off, **kw)
        ins.then_inc(b.sem.h, 16)
        b.sem.n += 16
        tok = (b.sem, b.sem.n, None)
        self.set_w(b, tok, fresh)
        return tok

    def store(self, q, b, out, in_):
        self.acq_r(q, b)
        tok = self.dma(q, out, in_, b.sem)
        self.add_r(b, tok)
        self.pending.append((q, tok))
        return tok

    def phase_end(self):
        for q, tok in self.pending:
            self.wait(q, tok)
        self.pending = []
        self.nc.all_engine_barrier()


class Phase:
    def __init__(self, k):
        self.k = k
        self.es = ExitStack()
        self.sems = []

    def __enter__(self):
        self.es.__enter__()
        return self

    def __exit__(self, *a):
        self.k.phase_end()
        for s in self.sems:
            self.k.dfree.append(s)
        return self.es.__exit__(*a)

    def sb(self, shape, dt, dma=False, name=None):
        k = self.k
        k.uid += 1
        t = self.es.enter_context(k.nc.sbuf_tensor(f"{name or 't'}_{k.uid}", list(shape), dt))
        s = None
        if dma:
            s = k.getsem()
            self.sems.append(s)
        return Buf(t, s)

    def ps(self, shape, dt, name=None):
        k = self.k
        k.uid += 1
        t = self.es.enter_context(k.nc.psum_tensor(f"{name or 'p'}_{k.uid}", list(shape), dt))
        return Buf(t)

    def ring(self, n, shape, dt, dma=False, name=None):
        return [self.sb(shape, dt, dma, name) for _ in range(n)]

    def pring(self, n, shape, dt, name=None):
        return [self.ps(shape, dt, name) for _ in range(n)]


class Seg:
    pass


def build(dbg=False, stop=None, only_p=False):
    k = KB()
    nc = k.nc
    E = k.eng

    def din(name, shape, dt=F32):
        return nc.dram_tensor(name, list(shape), dt, kind="ExternalInput").ap()

    def dscr(name, shape, dt, out=False):
        return nc.dram_tensor(name, list(shape), dt, kind=("ExternalOutput" if out else "Internal")).ap()

    xp = din("xp", [PW, D])
    xs = din("xs", [SW, D])
    memp = din("memp", [256, D])
    mems = din("mems", [256, D])
    valid_s = din("valid_s", [128, SW // 128])
    valid_p = din("valid_p", [128, PW // 128])
    cs_p = din("cs_p", [2, 128, PW])
    cs_s = din("cs_s", [2, 128, SW])
    maskd = din("maskd", [128, MASK_J])
    identd = din("identd", [128, 128])
    triud = din("triud", [128, 128])
    iotad = din("iotad", [128, 1])
    w_in = din("w_in_ext", [DEPTH, D, D_EXT])
    cpar = din("cpar", [DEPTH, 128, 6, 34])
    w_memkv = din("w_mem_kv", [DEPTH, D, 1024])
    w_out = din("w_out", [DEPTH, D, D])
    ln1_g = din("ln1_g", [DEPTH, D]); ln1_b = din("ln1_b", [DEPTH, D])
    ln2_g = din("ln2_g", [DEPTH, D]); ln2_b = din("ln2_b", [DEPTH, D])
    ffn_w1 = din("ffn_w1", [1, D, D_FF]); ffn_w3 = din("ffn_w3", [1, D, D_FF]); ffn_w2 = din("ffn_w2", [1, D_FF, D])
    router = din("moe_router", [1, D, NE])
    moe_w1 = din("moe_w1", [1, NE, D, E_FF]); moe_w3 = din("moe_w3", [1, NE, D, E_FF]); moe_w2 = din("moe_w2", [1, NE, E_FF, D])
    yp = nc.dram_tensor("yp", [PW, D], F32, kind="ExternalOutput").ap()
    ys = nc.dram_tensor("ys", [4096, D], F32, kind="ExternalOutput").ap()

    win_b = [dscr(f"win_b{l}", [NMT, 128, 16 * 128], BF16) for l in range(DEPTH)]
    wkv_b = [dscr(f"wkv_b{l}", [8, 128, 16 * 128], BF16) for l in range(DEPTH)]
    wout_b = [dscr(f"wout_b{l}", [D, D], BF16) for l in range(DEPTH)]
    f1_b = dscr("f1_b", [D_FF // 128, 128, 2048], BF16)
    f3_b = dscr("f3_b", [D_FF // 128, 128, 2048], BF16)
    f2_b = dscr("f2_b", [D_FF, D], BF16)
    NFE = E_FF // 128
    FG = 4
    NFG = NFE // FG
    NFH = NFE // 2
    m13_h = [dscr(f"m13_b{h}", [NE * NFH * 128, 2 * 2048], BF16) for h in range(2)]
    m2_b = dscr("m2_b", [NE * 4 * NFG * 128, FG * 512], BF16)

    segs = []
    for nm, W, xin, mem, valid, cs, hr, orr, yout, yoff in (
            ("p", PW, xp, memp, valid_p, cs_p, [(0, PW), (0, PW)], [(0, PW), (0, PW)], yp, 0),
            ("s", SW, xs, mems, valid_s, cs_s, [(0, SW), (1024, 7168)], [(1024, 7168), (2048, 6144)], ys, 2048)):
        s = Seg()
        s.nm, s.W, s.x0, s.mem, s.valid, s.cs, s.hr, s.orr, s.yout, s.yoff = nm, W, xin, mem, valid, cs, hr, orr, yout, yoff
        s.u = dscr(f"u_{nm}", [D_CONV, W], BF16, out=dbg)
        s.q = dscr(f"q_{nm}", [D_ATT, W], BF16, out=dbg)
        s.kk = dscr(f"k_{nm}", [D_ATT, W], BF16, out=dbg)
        s.qm = dscr(f"qm_{nm}", [D_MEM, W], BF16, out=dbg)
        s.v = dscr(f"v_{nm}", [W, 12 * 128], BF16, out=dbg)
        s.mix = dscr(f"mix_{nm}", [D, W], BF16, out=dbg)
        s.x1 = dscr(f"x1_{nm}", [W, D], F32, out=dbg)
        s.x2 = dscr(f"x2_{nm}", [W, D], F32, out=dbg)
        s.comb = dscr(f"comb_{nm}", [W, NE], F32, out=dbg)
        segs.append(s)
    if only_p:
        segs = segs[:1]

    wsem = k.getsem()
    wsemA = k.getsem()
    wsemB = k.getsem()
    wsemF = k.getsem()
    bgq = []
    bgtok = {}

    def conv_tiled(src, dst, ncols, sem):
        for m in range(ncols // 128):
            s_ap = src[:, m * 128:(m + 1) * 128].rearrange("(k p) c -> p k c", p=128)
            d_ap = dst[m].rearrange("p (k c) -> p k c", c=128)
            bgq.append((d_ap, s_ap, sem))

    def conv_plain(src, dst, nrows, sem):
        for r in range(0, nrows, 128):
            bgq.append((dst[r:r + 128, :], src[r:r + 128, :], sem))

    def bg(n):
        for _ in range(n):
            if not bgq:
                return
            d_ap, s_ap, sem = bgq.pop(0)
            bgtok[sem.name] = k.dma('pool', d_ap, s_ap, sem)

    for l in range(DEPTH):
        sem = wsem if l == 0 else wsemA
        conv_tiled(w_in[l], win_b[l], D_EXT, sem)
        conv_tiled(w_memkv[l], wkv_b[l], 1024, sem)
        conv_plain(w_out[l], wout_b[l], D, sem)
        if l == 0:
            bg(len(bgq))
            conv_tiled(ffn_w1[0], f1_b, D_FF, wsemF)
            conv_tiled(ffn_w3[0], f3_b, D_FF, wsemF)
            conv_plain(ffn_w2[0], f2_b, D_FF, wsemF)
    if stop is None or stop > 10:
        for e in range(NE):
            for m in range(NFE):
                r0 = (e * NFH + (m % NFH)) * 128
                for wi, src in enumerate((moe_w1[0, e], moe_w3[0, e])):
                    s_ap = src[:, m * 128:(m + 1) * 128].rearrange("(k p) c -> p k c", p=128)
                    d_ap = m13_h[m // NFH][r0:r0 + 128, wi * 2048:(wi + 1) * 2048].rearrange("p (k c) -> p k c", c=128)
                    bgq.append((d_ap, s_ap, wsemB))
            for fg in range(NFG):
                for dch in range(4):
                    r0 = ((e * 4 + dch) * NFG + fg) * 128
                    s_ap = moe_w2[0, e][fg * 512:(fg + 1) * 512, dch * 512:(dch + 1) * 512].rearrange("(fi p) c -> p fi c", p=128)
                    d_ap = m2_b[r0:r0 + 128, :].rearrange("p (fi c) -> p fi c", c=512)
                    bgq.append((d_ap, s_ap, wsemB))
    k.pending = [('pool', bgtok[wsem.name])]
    k.phase_end()

    cst = ExitStack()
    ident_b = Buf(cst.enter_context(nc.sbuf_tensor("ident_b", [128, 128], BF16)), k.getsem())
    ident_f = Buf(cst.enter_context(nc.sbuf_tensor("ident_f", [128, 128], F32)), k.getsem())
    ones_b = Buf(cst.enter_context(nc.sbuf_tensor("ones_b", [128, 128], BF16)))
    ones_f = Buf(cst.enter_context(nc.sbuf_tensor("ones_f", [128, 128], F32)))
    epsb = Buf(cst.enter_context(nc.sbuf_tensor("epsb", [128, 1], F32)))
    k.load('pool', ident_b, ident_b.t[:], identd[:, :])
    k.load('sp', ident_f, ident_f.t[:], identd[:, :])
    k.set_w(ones_b, k.sig('dve', nc.vector.memset(ones_b.t[:], 1.0)))
    k.set_w(ones_f, k.sig('dve', nc.vector.memset(ones_f.t[:], 1.0)))
    k.set_w(epsb, k.sig('dve', nc.vector.memset(epsb.t[:], LN_EPS)))
    for e in ('pe', 'act', 'dve', 'pool'):
        k.acq_r(e, ident_b); k.acq_r(e, ident_f); k.acq_r(e, ones_b); k.acq_r(e, ones_f); k.acq_r(e, epsb)

    def load_xT(ph, src_rows, xb, xT, tp_ring, cnt):
        k.load('pool', xb, xb.t[:], src_rows.rearrange("(t p) d -> p t d", p=128))
        k.acq_r('pe', xb)
        k.acq_w('act', xT); k.acq_w('dve', xT)
        first = True
        tokp_last = [None]
        for kg in range(4):
            tp = tp_ring[cnt[0] % len(tp_ring)]; cnt[0] += 1
            k.acq_w('pe', tp)
            for kk in range(4):
                for t in range(4):
                    ins = nc.tensor.transpose(tp.t[:, kk, t * 128:(t + 1) * 128], xb.t[:, t, (kg * 4 + kk) * 128:(kg * 4 + kk + 1) * 128], ident_b.t[:])
            tokp_last[0] = k.sig('pe', ins)
            k.set_w(tp, tokp_last[0])
            e = 'act' if kg % 2 == 0 else 'dve'
            k.acq_r(e, tp)
            if e == 'act':
                ins = nc.scalar.activation(out=xT.t[:, kg * 4:(kg + 1) * 4, :], in_=tp.t[:], func=AF.Copy)
            else:
                ins = nc.vector.tensor_copy(out=xT.t[:, kg * 4:(kg + 1) * 4, :], in_=tp.t[:])
            tok = k.sig(e, ins)
            k.add_r(tp, tok)
            k.set_w(xT, tok, fresh=first)
            first = False
        k.add_r(xb, tokp_last[0])

    def ln_inplace(yt, gb, rs_pool, valid_ap=None):
        st, mv, rs, nb = rs_pool
        for ch in range(4):
            ins = nc.vector.bn_stats(out=st.t[:, ch, :], in_=yt[:, ch * 512:(ch + 1) * 512])
        k.wait('dve', k.sig('dve', ins), force=True)
        tok = k.sig('dve', nc.vector.bn_aggr(out=mv.t[:], in_=st.t[:].rearrange("p a b -> p (a b)")))
        k.wait('act', tok)
        tok = k.sig('act', nc.scalar.activation(out=rs.t[:], in_=mv.t[:, 1:2], func=AF.Sqrt, bias=epsb.t[:, 0:1], scale=1.0))
        k.wait('dve', tok)
        tok = k.sig('dve', nc.vector.reciprocal(out=rs.t[:], in_=rs.t[:]))
        k.wait('dve', tok, force=True)
        tok = k.sig('dve', nc.vector.tensor_scalar(out=nb.t[:], in0=mv.t[:, 0:1], scalar1=rs.t[:, 0:1], scalar2=-1.0, op0=ALU.mult, op1=ALU.mult))
        k.wait('act', tok)
        tok = k.sig('act', nc.scalar.activation(out=yt, in_=yt, func=AF.Identity, bias=nb.t[:, 0:1], scale=rs.t[:, 0:1]))
        k.wait('dve', tok)
        nc.vector.tensor_tensor(out=yt, in0=yt, in1=gb.t[:, 0, :], op=ALU.mult)
        ins = nc.vector.tensor_tensor(out=yt, in0=yt, in1=gb.t[:, 1, :], op=ALU.add)
        if valid_ap is not None:
            ins = nc.vector.tensor_scalar(out=yt, in0=yt, scalar1=valid_ap, scalar2=None, op0=ALU.mult)
        return k.sig('dve', ins)

    for l in range(DEPTH):
        if stop is not None and stop <= l * 10:
            break
        if l == 1:
            while bgq and bgq[0][2] is wsemA:
                bg(1)
            for q in ('sp', 'pool'):
                k.wait(q, bgtok.get(wsemA.name))
        for seg in segs:
            xin = seg.x0 if l == 0 else seg.x2
            hs, he = seg.hr[l]
            os_, oe = seg.orr[l]
            W = seg.W
            with Phase(k) as ph:
                xb = ph.sb([128, 4, 2048], BF16, dma=True, name="xb")
                xT = ph.sb([128, 16, 512], BF16, name="xT")
                wv = ph.sb([128, 6, 2048], BF16, dma=True, name="wv")
                wt = ph.ring(6, [128, 2048], BF16, dma=True, name="wt")
                cst_ = ph.ring(2, [128, 2, 512], F32, dma=True, name="cs")
                vt = ph.sb([128, W // 128], F32, dma=True, name="vt")
                onesv = ph.sb([128, 6, 64], BF16, name="onesv")
                sg = ph.ring(2, [128, 512], F32, name="sg")
                uo = ph.ring(2, [128, 512], BF16, dma=True, name="uo")
                t1 = ph.ring(2, [128, 512], F32, name="t1")
                t2 = ph.ring(2, [128, 512], F32, name="t2")
                qo = ph.ring(2, [128, 512], BF16, dma=True, name="qo")
                qmo = ph.ring(2, [128, 512], BF16, dma=True, name="qmo")
                vx = ph.ring(2, [128, 12, 128], BF16, dma=True, name="vx")
                tpr = ph.pring(2, [128, 4, 512], BF16, name="tp")
                mm = ph.pring(4, [128, 512], F32, name="mm")
                cnt = [0]
                mmc = [0]
                wtc = [0]
                k.set_w(onesv, k.sig('pool', nc.gpsimd.memset(onesv.t[:], 1.0)))
                k.load('sp', vt, vt.t[:], seg.valid[:, :])
                for m in range(6):
                    k.load('sp', wv, wv.t[:, m, :], win_b[l][24 + m], fresh=(m == 0))
                k.acq_r('pe', wv)
                k.acq_r('pool', vt)

                def mtile(m, xTb):
                    wb = wt[wtc[0] % 6]; wtc[0] += 1
                    k.load('sp', wb, wb.t[:], win_b[l][m])
                    pb = mm[mmc[0] % 4]; mmc[0] += 1
                    k.acq_r('pe', wb); k.acq_w('pe', pb); k.acq_r('pe', xTb)
                    for kk in range(16):
                        ins = nc.tensor.matmul(pb.t[:], lhsT=wb.t[:, kk * 128:(kk + 1) * 128], rhs=xTb.t[:, kk, :], start=(kk == 0), stop=(kk == 15))
                    tok = k.sig('pe', ins)
                    k.set_w(pb, tok); k.add_r(wb, tok)
                    return pb, tok

                nblk = (he - hs) // 512
                for b in range(nblk):
                    t0 = hs + b * 512
                    load_xT(ph, xin[t0:t0 + 512, :], xb, xT, tpr, cnt)
                    bg(8)
                    csb = cst_[b % 2]
                    k.load('sp', csb, csb.t[:], seg.cs[:, :, t0:t0 + 512].rearrange("a p t -> p a t"))
                    lasttok = None
                    for c in range(6):
                        pa, _ = mtile(c, xT)
                        pg, _ = mtile(6 + c, xT)
                        sgb = sg[c % 2]; uob = uo[c % 2]
                        k.acq_r('act', pg); k.acq_w('act', sgb)
                        tok = k.sig('act', nc.scalar.activation(out=sgb.t[:], in_=pg.t[:], func=AF.Sigmoid))
                        k.add_r(pg, tok); k.set_w(sgb, tok)
                        k.acq_r('dve', pa); k.acq_r('dve', sgb); k.acq_w('dve', uob)
                        tok = k.sig('dve', nc.vector.tensor_tensor(out=uob.t[:], in0=pa.t[:], in1=sgb.t[:], op=ALU.mult))
                        k.add_r(pa, tok); k.add_r(sgb, tok); k.set_w(uob, tok)
                        k.store('pool', uob, seg.u[c * 128:(c + 1) * 128, t0:t0 + 512], uob.t[:])
                    k.acq_r('dve', csb)
                    for which, base, pbase, dst in (("q", 12, 34, seg.q), ("k", 18, 40, seg.kk)):
                        for hp in range(6):
                            pq, _ = mtile(base + hp, xT)
                            pp, _ = mtile(pbase + hp, xT)
                            i2 = hp % 2
                            k.acq_r('dve', pq); k.acq_w('dve', t1[i2])
                            tok = k.sig('dve', nc.vector.tensor_tensor(out=t1[i2].t[:], in0=pq.t[:], in1=csb.t[:, 0, :], op=ALU.mult))
                            k.add_r(pq, tok); k.set_w(t1[i2], tok)
                            k.acq_r('dve', pp); k.acq_w('dve', t2[i2])
                            tok = k.sig('dve', nc.vector.tensor_tensor(out=t2[i2].t[:], in0=pp.t[:], in1=csb.t[:, 1, :], op=ALU.mult))
                            k.add_r(pp, tok); k.set_w(t2[i2], tok); k.add_r(csb, tok)
                            k.acq_r('pool', t1[i2]); k.acq_r('pool', t2[i2]); k.acq_w('pool', qo[i2])
                            tok = k.sig('pool', nc.gpsimd.tensor_tensor(out=qo[i2].t[:], in0=t1[i2].t[:], in1=t2[i2].t[:], op=ALU.add))
                            k.add_r(t1[i2], tok); k.add_r(t2[i2], tok); k.set_w(qo[i2], tok)
                            k.store('pool', qo[i2], dst[hp * 128:(hp + 1) * 128, t0:t0 + 512], qo[i2].t[:])
                    for mh in range(4):
                        pq, _ = mtile(30 + mh, xT)
                        ob = qmo[mh % 2]
                        k.acq_r('act', pq); k.acq_w('act', ob)
                        tok = k.sig('act', nc.scalar.activation(out=ob.t[:], in_=pq.t[:], func=AF.Copy))
                        k.add_r(pq, tok); k.set_w(ob, tok)
                        k.store('pool', ob, seg.qm[mh * 128:(mh + 1) * 128, t0:t0 + 512], ob.t[:])
                    for t in range(4):
                        vb = vx[t % 2]
                        tile_idx = (t0 // 128) + t
                        k.acq_w('pool', vb)
                        v5 = vb.t[:].rearrange("p (a two) d -> p a two d", two=2)
                        nc.gpsimd.tensor_scalar(out=v5[:, :, 0, 64:128], in0=onesv.t[:], scalar1=vt.t[:, tile_idx:tile_idx + 1], scalar2=None, op0=ALU.mult)
                        tok = k.sig('pool', nc.gpsimd.tensor_scalar(out=v5[:, :, 1, 0:64], in0=onesv.t[:], scalar1=vt.t[:, tile_idx:tile_idx + 1], scalar2=None, op0=ALU.mult))
                        k.set_w(vb, tok)
                        for (c0, ncol, h0, nh) in ((0, 512, 0, 8), (512, 256, 8, 4)):
                            pb = mm[mmc[0] % 4]; mmc[0] += 1
                            k.acq_w('pe', pb); k.acq_r('pe', xT)
                            for kk in range(16):
                                ins = nc.tensor.matmul(pb.t[:, 0:ncol], lhsT=xT.t[:, kk, t * 128:(t + 1) * 128],
                                                       rhs=wv.t[:, c0 // 128:(c0 + ncol) // 128, kk * 128:(kk + 1) * 128],
                                                       start=(kk == 0), stop=(kk == 15))
                            tokp = k.sig('pe', ins)
                            lasttok = tokp
                            k.set_w(pb, tokp)
                            p4 = pb.t[:, 0:ncol].rearrange("p (a two d) -> p a two d", two=2, d=64)
                            d4 = vb.t[:, h0:h0 + nh, :].rearrange("p (a two) d -> p a two d", two=2)
                            k.acq_r('act', pb); k.acq_w('act', vb)
                            tok = k.sig('act', nc.scalar.activation(out=d4[:, :, 0, 0:64], in_=p4[:, :, 0, :], func=AF.Copy))
                            k.add_r(pb, tok); k.set_w(vb, tok, fresh=False)
                            k.acq_r('dve', pb); k.acq_w('dve', vb)
                            tok = k.sig('dve', nc.vector.tensor_copy(out=d4[:, :, 1, 64:128], in_=p4[:, :, 1, :]))
                            k.add_r(pb, tok); k.set_w(vb, tok, fresh=False)
                        k.store('pool', vb, seg.v[t0 + t * 128:t0 + (t + 1) * 128, :], vb.t[:].rearrange("p h d -> p (h d)"))
                    k.add_r(xT, lasttok)
            if stop is not None and stop <= l * 10 + 1:
                continue
            with Phase(k) as ph:
                cw = ph.sb([128, 6, 34], F32, dma=True, name="cw")
                dg = ph.sb([128, 6, 31, 128], BF16, name="dg")
                ut = ph.ring(2, [128, 6, 544], BF16, dma=True, name="ut")
                acc = ph.ring(2, [128, 6, 512], F32, name="acc")
                ysq = ph.ring(2, [128, 512], F32, name="ysq")
                mean = ph.sb([128, 512], F32, name="mean")
                msq = ph.sb([128, 512], F32, name="msq")
                rstd = ph.sb([128, 512], F32, name="rstd")
                zt = ph.ring(2, [128, 512], F32, name="zt")
                co = ph.ring(2, [128, 512], BF16, dma=True, name="co")
                cps = ph.pring(3, [128, 512], F32, name="cps")
                sps = ph.pring(2, [128, 512], F32, name="sps")
                k.load('sp', cw, cw.t[:], cpar[l])
                k.acq_r('dve', cw); k.acq_r('act', cw)
                for c in range(6):
                    for j in range(31):
                        ins = nc.vector.tensor_scalar(out=dg.t[:, c, j, :], in0=ident_f.t[:], scalar1=cw.t[:, c, j:j + 1], scalar2=None, op0=ALU.mult)
                k.set_w(dg, k.sig('dve', ins))
                k.acq_r('pe', dg)
                nblk = (oe - os_) // 512
                cc = 0
                for b in range(nblk):
                    t0 = os_ + b * 512
                    ub = ut[b % 2]; ab = acc[b % 2]
                    lo = max(hs, t0 - 16); hi = min(he, t0 + 528)
                    fresh = True
                    if lo > t0 - 16 or hi < t0 + 528:
                        k.acq_w('pool', ub)
                        k.set_w(ub, k.sig('pool', nc.gpsimd.memset(ub.t[:], 0.0)))
                        fresh = False
                    for c in range(6):
                        k.load('sp', ub, ub.t[:, c, lo - (t0 - 16):hi - (t0 - 16)], seg.u[c * 128:(c + 1) * 128, lo:hi], fresh=(fresh and c == 0))
                    k.acq_r('pe', ub)
                    k.acq_w('pe', sps[0]); k.acq_w('pe', sps[1])
                    k.acq_w('act', ab)
                    for c in range(6):
                        pb = cps[cc % 3]; cc += 1
                        k.acq_w('pe', pb)
                        for j in range(31):
                            ins = nc.tensor.matmul(pb.t[:], lhsT=dg.t[:, c, j, :], rhs=ub.t[:, c, j + 1:j + 513], start=(j == 0), stop=(j == 30))
                        tokc = k.sig('pe', ins)
                        k.set_w(pb, tokc)
                        k.acq_r('act', pb)
                        tok = k.sig('act', nc.scalar.activation(out=ab.t[:, c, :], in_=pb.t[:], func=AF.Identity, bias=cw.t[:, c, 31:32], scale=1.0))
                        k.add_r(pb, tok); k.set_w(ab, tok, fresh=(c == 0))
                        k.wait('act', tok, force=True)
                        yb = ysq[c % 2]
                        k.acq_w('act', yb)
                        toka = k.sig('act', nc.scalar.activation(out=yb.t[:], in_=ab.t[:, c, :], func=AF.Square))
                        k.set_w(yb, toka)
                        k.wait('pe', tok)
                        nc.tensor.matmul(sps[0].t[:], lhsT=ones_f.t[:], rhs=ab.t[:, c, :], start=(c == 0), stop=(c == 5))
                        k.wait('pe', toka)
                        tokp = k.sig('pe', nc.tensor.matmul(sps[1].t[:], lhsT=ones_f.t[:], rhs=yb.t[:], start=(c == 0), stop=(c == 5)))
                        k.add_r(yb, tokp)
                    k.add_r(ub, tokc)
                    k.set_w(sps[0], tokp); k.set_w(sps[1], tokp)
                    k.add_r(ab, tokp)
                    k.wait('act', tokp); k.acq_w('act', mean)
                    tokm = k.sig('act', nc.scalar.activation(out=mean.t[:], in_=sps[0].t[:], func=AF.Copy, scale=1.0 / D_CONV))
                    k.set_w(mean, tokm); k.add_r(sps[0], tokm)
                    k.wait('dve', tokm); k.wait('dve', tokp)
                    nc.vector.tensor_tensor(out=msq.t[:], in0=mean.t[:], in1=mean.t[:], op=ALU.mult)
                    tok = k.sig('dve', nc.vector.scalar_tensor_tensor(out=msq.t[:], in0=sps[1].t[:], scalar=1.0 / D_CONV, in1=msq.t[:], op0=ALU.mult, op1=ALU.subtract))
                    k.add_r(sps[1], tok)
                    k.wait('act', tok)
                    tok = k.sig('act', nc.scalar.activation(out=rstd.t[:], in_=msq.t[:], func=AF.Sqrt, bias=epsb.t[:, 0:1], scale=1.0))
                    k.wait('dve', tok)
                    nc.vector.reciprocal(out=rstd.t[:], in_=rstd.t[:])
                    k.acq_r('dve', ab)
                    for c in range(6):
                        zb = zt[c % 2]; cb = co[c % 2]
                        k.acq_w('dve', zb)
                        nc.vector.tensor_tensor(out=zb.t[:], in0=ab.t[:, c, :], in1=mean.t[:], op=ALU.subtract)
                        tok = k.sig('dve', nc.vector.tensor_tensor(out=zb.t[:], in0=zb.t[:], in1=rstd.t[:], op=ALU.mult))
                        k.set_w(zb, tok)
                        k.acq_r('act', zb); k.acq_w('act', cb)
                        toka = k.sig('act', nc.scalar.activation(out=cb.t[:], in_=zb.t[:], func=AF.Silu, bias=cw.t[:, c, 33:34], scale=cw.t[:, c, 32:33]))
                        k.add_r(zb, toka); k.set_w(cb, toka)
                        k.store('pool', cb, seg.mix[c * 128:(c + 1) * 128, t0:t0 + 512], cb.t[:])
                    k.add_r(ab, tok); k.add_r(mean, tok)
            if stop is not None and stop <= l * 10 + 2:
                continue
            with Phase(k) as ph:
                mk = ph.sb([128, MASK_J], BF16, dma=True, name="mk")
                qt = ph.ring(2, [128, 512], BF16, dma=True, name="qt")
                kt = ph.ring(2, [128, 2560], BF16, dma=True, name="kt")
                vxl = ph.ring(2, [128, 20, 256], BF16, dma=True, name="vxl")
                NS = 6
                LA = 3
                pe_t = ph.ring(NS, [128, 512], BF16, name="pexp")
                pm_t = ph.ring(NS, [128, 512], BF16, name="pmsk")
                rd = ph.ring(2, [128, 512], F32, name="rd")
                rd2 = ph.ring(2, [128, 512], F32, name="rd2")
                att = ph.ring(2, [128, 512], BF16, dma=True, name="att")
                sp_ = ph.pring(NS, [128, 512], F32, name="sps")
                ap_ = ph.pring(2, [128, 512], F32, name="aps")
                k.load('pool', mk, mk.t[:], maskd[:, :])
                k.acq_r('dve', mk)
                it = 0; sc = 0; ac = 0
                nqb = (oe - os_) // 512
                for qb in range(nqb):
                    q0 = os_ + qb * 512
                    k0 = max(hs, q0 - 1024); k1 = min(he, q0 + 512 + 1024)
                    nkb = (k1 - k0) // 128
                    for hp in range(6):
                        qb_ = qt[it % 2]; kb_ = kt[it % 2]; vb_ = vxl[it % 2]; ab_ = att[it % 2]; it += 1
                        bg(4)
                        k.load('sp', qb_, qb_.t[:], seg.q[hp * 128:(hp + 1) * 128, q0:q0 + 512])
                        k.load('sp', kb_, kb_.t[:, 0:k1 - k0], seg.kk[hp * 128:(hp + 1) * 128, k0:k1])
                        k.load('sp', vb_, vb_.t[:, 0:nkb, :], seg.v[k0:k1, hp * 256:(hp + 1) * 256].rearrange("(kb p) c -> p kb c", p=128))
                        k.acq_r('pe', qb_); k.acq_r('pe', kb_); k.acq_r('pe', vb_)
                        for hh in range(2):
                            r0 = hh * 64
                            accb = ap_[ac % 2]; ac += 1
                            k.acq_w('pe', accb)
                            pend = []
                            for kbi in range(nkb):
                                d = (k0 + kbi * 128) - q0
                                j0 = MASK_D0 - d
                                sb_ = sp_[sc % NS]; peb = pe_t[sc % NS]; pmb = pm_t[sc % NS]; sc += 1
                                k.acq_w('pe', sb_)
                                tok = k.sig('pe', nc.tensor.matmul(sb_.t[:], lhsT=kb_.t[r0:r0 + 64, kbi * 128:(kbi + 1) * 128], rhs=qb_.t[r0:r0 + 64, :], start=True, stop=True))
                                k.set_w(sb_, tok)
                                k.acq_r('act', sb_); k.acq_w('act', peb)
                                tok = k.sig('act', nc.scalar.activation(out=peb.t[:], in_=sb_.t[:], func=AF.Exp, scale=0.125))
                                k.add_r(sb_, tok); k.set_w(peb, tok)
                                k.acq_r('dve', peb); k.acq_w('dve', pmb)
                                tok = k.sig('dve', nc.vector.tensor_tensor(out=pmb.t[:], in0=peb.t[:], in1=mk.t[:, j0:j0 + 512], op=ALU.mult))
                                k.add_r(peb, tok); k.set_w(pmb, tok)
                                pend.append((pmb, kbi))
                                if len(pend) > LA:
                                    pb2, kb2 = pend.pop(0)
                                    k.acq_r('pe', pb2)
                                    tok = k.sig('pe', nc.tensor.matmul(accb.t[:], lhsT=vb_.t[:, kb2, hh * 128:(hh + 1) * 128], rhs=pb2.t[:], start=(kb2 == 0), stop=False))
                                    k.add_r(pb2, tok)
                            while pend:
                                pb2, kb2 = pend.pop(0)
                                k.acq_r('pe', pb2)
                                tok = k.sig('pe', nc.tensor.matmul(accb.t[:], lhsT=vb_.t[:, kb2, hh * 128:(hh + 1) * 128], rhs=pb2.t[:], start=(kb2 == 0), stop=(not pend)))
                                k.add_r(pb2, tok)
                            k.set_w(accb, tok)
                            nr = r0; dr = 64 - r0
                            rdb = rd[hh]; rd2b = rd2[hh]
                            k.acq_r('dve', accb); k.acq_w('dve', rdb)
                            tok = k.sig('dve', nc.vector.reciprocal(out=rdb.t[dr:dr + 64, :], in_=accb.t[dr:dr + 64, :]))
                            k.set_w(rdb, tok)
                            k.acq_r('act', rdb); k.acq_w('act', rd2b)
                            tok = k.sig('act', nc.scalar.activation(out=rd2b.t[nr:nr + 64, :], in_=rdb.t[dr:dr + 64, :], func=AF.Copy))
                            k.add_r(rdb, tok); k.set_w(rd2b, tok)
                            k.acq_r('dve', rd2b)
                            if hh == 0:
                                k.acq_w('dve', ab_)
                            tok = k.sig('dve', nc.vector.tensor_tensor(out=ab_.t[nr:nr + 64, :], in0=accb.t[nr:nr + 64, :], in1=rd2b.t[nr:nr + 64, :], op=ALU.mult))
                            k.add_r(accb, tok); k.add_r(rd2b, tok); k.set_w(ab_, tok, fresh=(hh == 0))
                        k.add_r(qb_, tok); k.add_r(kb_, tok); k.add_r(vb_, tok)
                        k.store('pool', ab_, seg.mix[(6 + hp) * 128:(7 + hp) * 128, q0:q0 + 512], ab_.t[:])
            if stop is not None and stop <= l * 10 + 3:
                continue
            with Phase(k) as ph:
                mb = ph.sb([128, 2, 2048], BF16, dma=True, name="mb")
                memT = ph.sb([128, 16, 256], BF16, name="memT")
                wkv = ph.sb([128, 8, 2048], BF16, dma=True, name="wkv")
                kmT = ph.sb([128, 4, 256], BF16, name="kmT")
                vm = ph.sb([128, 2, 512], BF16, name="vm")
                qmt = ph.ring(2, [128, 4, 512], BF16, dma=True, name="qmt")
                pe_t = ph.ring(3, [128, 512], BF16, name="pexp")
                rd = ph.ring(2, [128, 512], F32, name="rd")
                mo = ph.ring(2, [128, 512], BF16, dma=True, name="mo")
                tpm = ph.pring(2, [128, 4, 256], BF16, name="tpm")
                sp_ = ph.pring(2, [128, 512], F32, name="sps")
                np_ = ph.pring(2, [128, 512], F32, name="nps")
                dp_ = ph.pring(2, [128, 512], F32, name="dps")
                k.load('pool', mb, mb.t[:], seg.mem.rearrange("(t p) d -> p t d", p=128))
                for m in range(8):
                    k.load('sp', wkv, wkv.t[:, m, :], wkv_b[l][m], fresh=(m == 0))
                k.acq_r('pe', mb); k.acq_r('pe', wkv)
                for kg in range(4):
                    tp = tpm[kg % 2]
                    k.acq_w('pe', tp)
                    for kk in range(4):
                        for t in range(2):
                            ins = nc.tensor.transpose(tp.t[:, kk, t * 128:(t + 1) * 128], mb.t[:, t, (kg * 4 + kk) * 128:(kg * 4 + kk + 1) * 128], ident_b.t[:])
                    k.set_w(tp, k.sig('pe', ins))
                    k.acq_r('act', tp)
                    tok = k.sig('act', nc.scalar.activation(out=memT.t[:, kg * 4:(kg + 1) * 4, :], in_=tp.t[:], func=AF.Copy))
                    k.add_r(tp, tok); k.set_w(memT, tok, fresh=(kg == 0))
                k.acq_r('pe', memT)
                for mh in range(4):
                    pb = sp_[mh % 2]
                    k.acq_w('pe', pb)
                    for kk in range(16):
                        ins = nc.tensor.matmul(pb.t[:, 0:256], lhsT=wkv.t[:, mh, kk * 128:(kk + 1) * 128], rhs=memT.t[:, kk, :], start=(kk == 0), stop=(kk == 15))
                    k.set_w(pb, k.sig('pe', ins))
                    k.acq_r('act', pb)
                    tok = k.sig('act', nc.scalar.activation(out=kmT.t[:, mh, :], in_=pb.t[:, 0:256], func=AF.Copy))
                    k.add_r(pb, tok); k.set_w(kmT, tok, fresh=(mh == 0))
                i = 0
                for t in range(2):
                    for mh in range(4):
                        pb = np_[i % 2]; i += 1
                        k.acq_w('pe', pb)
                        for kk in range(16):
                            ins = nc.tensor.matmul(pb.t[:, 0:128], lhsT=memT.t[:, kk, t * 128:(t + 1) * 128], rhs=wkv.t[:, 4 + mh, kk * 128:(kk + 1) * 128], start=(kk == 0), stop=(kk == 15))
                        k.set_w(pb, k.sig('pe', ins))
                        k.acq_r('dve', pb)
                        tok = k.sig('dve', nc.vector.tensor_copy(out=vm.t[:, t, mh * 128:(mh + 1) * 128], in_=pb.t[:, 0:128]))
                        k.add_r(pb, tok); k.set_w(vm, tok, fresh=(i == 1))
                k.acq_r('pe', kmT); k.acq_r('pe', vm)
                nqb = (oe - os_) // 512
                sc = 0; it = 0
                for qb in range(nqb):
                    q0 = os_ + qb * 512
                    qb_ = qmt[qb % 2]
                    k.load('sp', qb_, qb_.t[:], seg.qm[:, q0:q0 + 512].rearrange("(h p) t -> p h t", p=128))
                    k.acq_r('pe', qb_)
                    for mh in range(4):
                        nb_ = np_[it % 2]; db_ = dp_[it % 2]; rdb = rd[it % 2]; ob = mo[it % 2]; it += 1
                        k.acq_w('pe', nb_); k.acq_w('pe', db_)
                        for t in range(2):
                            sb_ = sp_[sc % 2]; peb = pe_t[sc % 3]; sc += 1
                            k.acq_w('pe', sb_)
                            tok = k.sig('pe', nc.tensor.matmul(sb_.t[:], lhsT=kmT.t[:, mh, t * 128:(t + 1) * 128], rhs=qb_.t[:, mh, :], start=True, stop=True))
                            k.set_w(sb_, tok)
                            k.acq_r('act', sb_); k.acq_w('act', peb)
                            tok = k.sig('act', nc.scalar.activation(out=peb.t[:], in_=sb_.t[:], func=AF.Exp, scale=float(128 ** -0.5)))
                            k.add_r(sb_, tok); k.set_w(peb, tok)
                            k.acq_r('pe', peb)
                            nc.tensor.matmul(nb_.t[:], lhsT=vm.t[:, t, mh * 128:(mh + 1) * 128], rhs=peb.t[:], start=(t == 0), stop=(t == 1))
                            tok = k.sig('pe', nc.tensor.matmul(db_.t[:], lhsT=ones_b.t[:], rhs=peb.t[:], start=(t == 0), stop=(t == 1)))
                            k.add_r(peb, tok)
                        k.set_w(nb_, tok); k.set_w(db_, tok)
                        k.acq_r('dve', db_); k.acq_w('dve', rdb)
                        k.wait('dve', k.sig('dve', nc.vector.reciprocal(out=rdb.t[:], in_=db_.t[:])), force=True)
                        k.acq_w('dve', ob)
                        tok = k.sig('dve', nc.vector.tensor_tensor(out=ob.t[:], in0=nb_.t[:], in1=rdb.t[:], op=ALU.mult))
                        k.add_r(nb_, tok); k.add_r(db_, tok); k.set_w(ob, tok)
                        k.store('pool', ob, seg.mix[(12 + mh) * 128:(13 + mh) * 128, q0:q0 + 512], ob.t[:])
                    k.add_r(qb_, tok)
            if stop is not None and stop <= l * 10 + 4:
                continue
            with Phase(k) as ph:
                wo = ph.sb([128, 16, 2048], BF16, dma=True, name="wo")
                gb = ph.sb([128, 2, 2048], F32, dma=True, name="gb")
                mt = ph.ring(2, [128, 16, 512], BF16, dma=True, name="mixT")
                xr = ph.ring(2, [128, 2048], F32, dma=True, name="xr")
                yt = ph.ring(2, [128, 2048], F32, dma=True, name="yt")
                st = ph.sb([128, 4, 6], F32); mv = ph.sb([128, 2], F32); rs = ph.sb([128, 1], F32); nb = ph.sb([128, 1], F32)
                ops = ph.pring(8, [128, 512], F32, name="ops")
                k.load('sp', wo, wo.t[:], wout_b[l].rearrange("(k p) n -> p k n", p=128))
                k.load('sp', gb, gb.t[:, 0, :], ln1_g[l].partition_broadcast(128))
                k.load('sp', gb, gb.t[:, 1, :], ln1_b[l].partition_broadcast(128), fresh=False)
                k.acq_r('pe', wo); k.acq_r('dve', gb)
                nblk = (oe - os_) // 512
                oc = 0; ti = 0
                for b in range(nblk):
                    t0 = os_ + b * 512
                    mb_ = mt[b % 2]
                    k.load('sp', mb_, mb_.t[:], seg.mix[:, t0:t0 + 512].rearrange("(k p) t -> p k t", p=128))
                    k.acq_r('pe', mb_)
                    for t in range(4):
                        xb_ = xr[ti % 2]; yb = yt[ti % 2]; ti += 1
                        r0 = t0 + t * 128
                        k.load('sp', xb_, xb_.t[:], xin[r0:r0 + 128, :])
                        pbs = []
                        for ch in range(4):
                            pb = ops[oc % 8]; oc += 1
                            k.acq_w('pe', pb)
                            for kk in range(16):
                                ins = nc.tensor.matmul(pb.t[:], lhsT=mb_.t[:, kk, t * 128:(t + 1) * 128], rhs=wo.t[:, kk, ch * 512:(ch + 1) * 512], start=(kk == 0), stop=(kk == 15))
                            tokp = k.sig('pe', ins)
                            k.set_w(pb, tokp)
                            pbs.append(pb)
                        k.acq_r('dve', xb_); k.acq_w('dve', yb)
                        for ch in range(4):
                            k.acq_r('dve', pbs[ch])
                            tok = k.sig('dve', nc.vector.scalar_tensor_tensor(out=yb.t[:, ch * 512:(ch + 1) * 512], in0=xb_.t[:, ch * 512:(ch + 1) * 512], scalar=float(ALPHA), in1=pbs[ch].t[:], op0=ALU.mult, op1=ALU.add))
                            k.add_r(pbs[ch], tok)
                        k.add_r(xb_, tok)
                        tok = ln_inplace(yb.t[:], gb, (st, mv, rs, nb))
                        k.set_w(yb, tok)
                        k.store('pool', yb, seg.x1[r0:r0 + 128, :], yb.t[:])
                    k.add_r(mb_, tokp)
            if stop is not None and stop <= l * 10 + 5:
                continue
            is_moe = (l % 2 == 1)
            if is_moe:
                with Phase(k) as ph:
                    rt = ph.sb([128, 16, NE], F32, dma=True, name="rt")
                    xr = ph.ring(2, [128, 2048], F32, dma=True, name="xr")
                    xT32 = ph.ring(2, [128, 16, 128], F32, name="xT32")
                    lg = ph.ring(2, [128, NE], F32, name="lg")
                    m8 = ph.sb([128, 8], F32); nv1 = ph.sb([128, 1], F32); ex = ph.sb([128, 8], F32); msk = ph.sb([128, 8], F32)
                    den = ph.sb([128, 1], F32); rden = ph.sb([128, 1], F32)
                    cb = ph.ring(2, [128, NE], F32, dma=True, name="cb")
                    tps = ph.pring(4, [128, 4, 128], F32, name="tps")
                    lps = ph.pring(2, [128, NE], F32, name="lps")
                    k.load('sp', rt, rt.t[:], router[0].rearrange("(k p) e -> p k e", p=128))
                    k.acq_r('pe', rt)
                    ntile = (oe - os_) // 128
                    tc_ = 0
                    for ti in range(ntile):
                        r0 = os_ + ti * 128
                        xb_ = xr[ti % 2]; xtb = xT32[ti % 2]; lgb = lg[ti % 2]; cbb = cb[ti % 2]; lp = lps[ti % 2]
                        k.load('sp', xb_, xb_.t[:], seg.x1[r0:r0 + 128, :])
                        k.acq_r('pe', xb_)
                        for kg in range(4):
                            tp = tps[tc_ % 4]; tc_ += 1
                            k.acq_w('pe', tp)
                            for kk in range(4):
                                ins = nc.tensor.transpose(tp.t[:, kk, :], xb_.t[:, (kg * 4 + kk) * 128:(kg * 4 + kk + 1) * 128], ident_f.t[:])
                            tokp = k.sig('pe', ins)
                            k.set_w(tp, tokp)
                            e = 'act' if kg % 2 == 0 else 'dve'
                            k.acq_r(e, tp); k.acq_w(e, xtb)
                            if e == 'act':
                                ins = nc.scalar.activation(out=xtb.t[:, kg * 4:(kg + 1) * 4, :], in_=tp.t[:], func=AF.Copy)
                            else:
                                ins = nc.vector.tensor_copy(out=xtb.t[:, kg * 4:(kg + 1) * 4, :], in_=tp.t[:])
                            tok = k.sig(e, ins)
                            k.add_r(tp, tok); k.set_w(xtb, tok, fresh=(kg == 0))
                        k.add_r(xb_, tokp)
                        k.acq_r('pe', xtb); k.acq_w('pe', lp)
                        for kk in range(16):
                            ins = nc.tensor.matmul(lp.t[:], lhsT=xtb.t[:, kk, :], rhs=rt.t[:, kk, :], start=(kk == 0), stop=(kk == 15))
                        tokp = k.sig('pe', ins)
                        k.set_w(lp, tokp); k.add_r(xtb, tokp)
                        k.acq_r('dve', lp); k.acq_w('dve', lgb); k.acq_w('dve', cbb)
                        k.wait('dve', k.sig('dve', nc.vector.tensor_copy(out=lgb.t[:], in_=lp.t[:])), force=True)
                        tok = k.sig('dve', nc.vector.max(out=m8.t[:], in_=lgb.t[:]))
                        k.wait('dve', tok, force=True)
                        nc.vector.tensor_scalar(out=msk.t[:], in0=lgb.t[:], scalar1=m8.t[:, 1:2], scalar2=None, op0=ALU.is_ge)
                        tok = k.sig('dve', nc.vector.tensor_scalar(out=nv1.t[:], in0=m8.t[:, 0:1], scalar1=-1.0, scalar2=None, op0=ALU.mult))
                        k.add_r(lp, tok)
                        k.wait('act', tok)
                        toka = k.sig('act', nc.scalar.activation(out=ex.t[:], in_=lgb.t[:], func=AF.Exp, bias=nv1.t[:, 0:1], scale=1.0))
                        k.wait('dve', toka)
                        k.wait('dve', k.sig('dve', nc.vector.tensor_tensor(out=ex.t[:], in0=ex.t[:], in1=msk.t[:], op=ALU.mult)), force=True)
                        k.wait('dve', k.sig('dve', nc.vector.tensor_reduce(out=den.t[:], in_=ex.t[:], axis=mybir.AxisListType.X, op=ALU.add)), force=True)
                        tok = k.sig('dve', nc.vector.reciprocal(out=rden.t[:], in_=den.t[:]))
                        k.wait('dve', tok, force=True)
                        tok = k.sig('dve', nc.vector.tensor_scalar(out=cbb.t[:], in0=ex.t[:], scalar1=rden.t[:, 0:1], scalar2=None, op0=ALU.mult))
                        k.set_w(cbb, tok); k.set_w(lgb, tok)
                        k.store('pool', cbb, seg.comb[r0:r0 + 128, :], cbb.t[:])
            last = (l == DEPTH - 1)
            if is_moe:
                continue
            while bgq and bgq[0][2] is wsemF:
                bg(1)
            for q in ('sp', 'pool'):
                k.wait(q, bgtok.get(wsemF.name))
            with Phase(k) as ph:
                nexp = NE if is_moe else 1
                NF = (E_FF if is_moe else D_FF) // 128
                FG = 4
                xb = ph.sb([128, 4, 2048], BF16, dma=True, name="xb")
                xT = ph.sb([128, 16, 512], BF16, name="xT")
                gT = ph.sb([128, NF, 512], BF16, name="gT")
                accs = ph.sb([128, 4, 2048], F32, dma=True, name="acc")
                w13 = ph.ring(3, [128, 2, 2048], BF16, dma=True, name="w13")
                w2r = ph.ring(4, [128, FG, 512], BF16, dma=True, name="w2r")
                xr = ph.sb([128, 2048], F32, dma=True, name="xr")
                gb = ph.sb([128, 2, 2048], F32, dma=True, name="gb")
                stt = ph.ring(2, [128, 512], F32, name="silu")
                cbt = ph.sb([128, 4, NE], F32, dma=True, name="cbt")
                vt = ph.sb([128, W // 128], F32, dma=True, name="vt")
                st = ph.sb([128, 4, 6], F32); mv = ph.sb([128, 2], F32); rs = ph.sb([128, 1], F32); nb = ph.sb([128, 1], F32)
                tpr = ph.pring(1, [128, 4, 512], BF16, name="tp")
                hps = ph.pring(2, [128, 512], F32, name="hps")
                ops = ph.pring(4, [128, 512], F32, name="ops")
                k.load('sp', vt, vt.t[:], seg.valid[:, :])
                k.load('sp', gb, gb.t[:, 0, :], (ln2_g if True else ln1_g)[l].partition_broadcast(128))
                k.load('sp', gb, gb.t[:, 1, :], ln2_b[l].partition_broadcast(128), fresh=False)
                k.acq_r('dve', gb); k.acq_r('dve', vt)
                nblk = (oe - os_) // 512
                cnt = [0]
                wc = 0; w2c = 0; hc = 0
                for b in range(nblk):
                    t0 = os_ + b * 512
                    load_xT(ph, seg.x1[t0:t0 + 512, :], xb, xT, tpr, cnt)
                    if is_moe:
                        k.load('sp', cbt, cbt.t[:], seg.comb[t0:t0 + 512, :].rearrange("(t p) e -> p t e", p=128))
                        k.acq_r('dve', cbt)
                    for e in range(nexp):
                        w1s = f1_b; w3s = f3_b; w2s = f2_b
                        for f in range(NF):
                            bg(1)
                            wb = w13[wc % 3]; wc += 1
                            k.load('sp', wb, wb.t[:, 0, :], w1s[f])
                            k.load('sp', wb, wb.t[:, 1, :], w3s[f], fresh=False)
                            k.acq_r('pe', wb); k.acq_r('pe', xT)
                            h1 = hps[0]; h3 = hps[1]
                            k.acq_w('pe', h1)
                            for kk in range(16):
                                ins = nc.tensor.matmul(h1.t[:], lhsT=wb.t[:, 0, kk * 128:(kk + 1) * 128], rhs=xT.t[:, kk, :], start=(kk == 0), stop=(kk == 15))
                            k.set_w(h1, k.sig('pe', ins))
                            k.acq_w('pe', h3)
                            for kk in range(16):
                                ins = nc.tensor.matmul(h3.t[:], lhsT=wb.t[:, 1, kk * 128:(kk + 1) * 128], rhs=xT.t[:, kk, :], start=(kk == 0), stop=(kk == 15))
                            tokp = k.sig('pe', ins)
                            k.set_w(h3, tokp); k.add_r(wb, tokp)
                            sb_ = stt[hc % 2]; hc += 1
                            k.acq_r('act', h1); k.acq_w('act', sb_)
                            tok = k.sig('act', nc.scalar.activation(out=sb_.t[:], in_=h1.t[:], func=AF.Silu))
                            k.add_r(h1, tok); k.set_w(sb_, tok)
                            k.acq_r('dve', sb_); k.acq_r('dve', h3)
                            if f == 0:
                                k.acq_w('dve', gT)
                            tok = k.sig('dve', nc.vector.tensor_tensor(out=gT.t[:, f, :], in0=sb_.t[:], in1=h3.t[:], op=ALU.mult))
                            k.add_r(sb_, tok); k.add_r(h3, tok); k.set_w(gT, tok, fresh=(f == 0))
                        if e == nexp - 1:
                            k.add_r(xT, tokp)
                        k.acq_r('pe', gT)
                        for dch in range(4):
                            for t in range(4):
                                k.acq_w('pe', ops[t])
                            for fg in range(NF // FG):
                                wb = w2r[w2c % 4]; w2c += 1
                                k.load('sp', wb, wb.t[:], w2s[fg * FG * 128:(fg + 1) * FG * 128, dch * 512:(dch + 1) * 512].rearrange("(f p) d -> p f d", p=128))
                                k.acq_r('pe', wb)
                                for fi in range(FG):
                                    f = fg * FG + fi
                                    for t in range(4):
                                        ins = nc.tensor.matmul(ops[t].t[:], lhsT=gT.t[:, f, t * 128:(t + 1) * 128], rhs=wb.t[:, fi, :], start=(f == 0), stop=(f == NF - 1))
                                tokp = k.sig('pe', ins)
                                k.add_r(wb, tokp)
                            for t in range(4):
                                k.set_w(ops[t], tokp)
                            if e == 0 and dch == 0:
                                k.acq_w('dve', accs)
                            for t in range(4):
                                k.acq_r('dve', ops[t])
                                dst = accs.t[:, t, dch * 512:(dch + 1) * 512]
                                if not is_moe:
                                    ins = nc.vector.tensor_copy(out=dst, in_=ops[t].t[:])
                                elif e == 0:
                                    ins = nc.vector.tensor_scalar(out=dst, in0=ops[t].t[:], scalar1=cbt.t[:, t, e:e + 1], scalar2=None, op0=ALU.mult)
                                else:
                                    ins = nc.vector.scalar_tensor_tensor(out=dst, in0=ops[t].t[:], scalar=cbt.t[:, t, e:e + 1], in1=dst, op0=ALU.mult, op1=ALU.add)
                                tok = k.sig('dve', ins)
                                k.add_r(ops[t], tok)
                            k.set_w(accs, tok, fresh=(e == 0 and dch == 0))
                        k.add_r(gT, tokp)
                    if is_moe:
                        k.add_r(cbt, tok)
                    for t in range(4):
                        r0 = t0 + t * 128
                        k.load('sp', xr, xr.t[:], seg.x1[r0:r0 + 128, :])
                        k.acq_r('dve', xr)
                        yt = accs.t[:, t, :]
                        tok = k.sig('dve', nc.vector.scalar_tensor_tensor(out=yt, in0=xr.t[:], scalar=float(ALPHA), in1=yt, op0=ALU.mult, op1=ALU.add))
                        k.add_r(xr, tok)
                        va = None if last else vt.t[:, r0 // 128:r0 // 128 + 1]
                        tok = ln_inplace(yt, gb, (st, mv, rs, nb), valid_ap=va)
                        k.set_w(accs, tok, fresh=False)
                        if last:
                            if os_ <= r0 < oe:
                                k.store('pool', accs, seg.yout[r0 - seg.yoff:r0 - seg.yoff + 128, :], yt)
                        else:
                            k.store('pool', accs, seg.x2[r0:r0 + 128, :], yt)

        if l % 2 == 1 and (stop is None or stop > l * 10 + 5):
            I32 = mybir.dt.int32
            AX = mybir.AxisListType.X
            tiles = []
            for seg in segs:
                o0, o1 = seg.orr[l]
                for r0 in range(o0, o1, 128):
                    tiles.append((seg, r0))
            NT = len(tiles)
            NB = NT // 2 + NE
            NROW = NB * 512
            xsort = dscr(f"xsort{l}", [NROW + 128, D], BF16)
            ysort = dscr(f"ysort{l}", [NROW + 128, D], F32)
            outer = ExitStack()

            def osb(shape, dt, name):
                k.uid += 1
                return Buf(outer.enter_context(nc.sbuf_tensor(f"{name}{k.uid}", list(shape), dt)))

            IDX = osb([128, NT * 2], I32, "IDX")
            G12 = osb([128, NT, 2], F32, "G12")
            WI13 = osb([128, NB], I32, "WI13")
            WI2 = osb([128, NB * 4], I32, "WI2")

            def fw(ins):
                tok = k.sig('dve', ins)
                k.wait('dve', tok, force=True)
                return tok

            with Phase(k) as ph:
                U = ph.sb([128, 128], F32, dma=True, name="U")
                iot = ph.sb([128, 1], F32, dma=True, name="iot")
                CB = ph.sb([128, NT, 8], F32, dma=True, name="CB")
                Mt = ph.sb([128, NT, 8], F32, name="Mt")
                RK = ph.sb([128, NT, 8], F32, name="RK")
                carry = ph.sb([128, 8], F32); cmpn = ph.sb([128, 8, 12], F32); pe_ = ph.sb([128, 8], F32)
                o1_ = ph.sb([128, 8], F32); end_ = ph.sb([128, 8], F32)
                cmpE = ph.sb([128, NB, 8], F32); EJ = ph.sb([128, NB], F32); wf = ph.sb([128, NB], F32)
                key = ph.sb([128, 8], F32); m8 = ph.sb([128, 8], F32); eq = ph.sb([128, 8], F32)
                rps = ph.pring(2, [128, 8], F32, name="rps"); cps = ph.pring(2, [128, 8], F32, name="cps")
                k.load('sp', U, U.t[:], triud[:, :])
                k.load('sp', iot, iot.t[:], iotad[:, :])
                i = 0
                for seg in segs:
                    o0, o1 = seg.orr[l]
                    n = (o1 - o0) // 128
                    k.load('sp', CB, CB.t[:, i:i + n, :], seg.comb[o0:o1, :].rearrange("(t p) e -> p t e", p=128), fresh=(i == 0))
                    i += n
                k.acq_r('dve', CB); k.acq_r('dve', iot); k.acq_r('pe', U)
                tokM = fw(nc.vector.tensor_scalar(out=Mt.t[:], in0=CB.t[:], scalar1=0.0, scalar2=None, op0=ALU.is_gt))
                k.wait('pe', tokM)
                fw(nc.vector.memset(carry.t[:], 0.0))
                for i in range(NT):
                    rp = rps[i % 2]; cp = cps[i % 2]
                    k.acq_w('pe', rp); k.acq_w('pe', cp)
                    nc.tensor.matmul(rp.t[:], lhsT=U.t[:], rhs=Mt.t[:, i, :], start=True, stop=True)
                    tok = k.sig('pe', nc.tensor.matmul(cp.t[:], lhsT=ones_f.t[:], rhs=Mt.t[:, i, :], start=True, stop=True))
                    k.set_w(rp, tok); k.set_w(cp, tok)
                    k.acq_r('dve', rp)
                    nc.vector.tensor_tensor(out=RK.t[:, i, :], in0=rp.t[:], in1=carry.t[:], op=ALU.add)
                    tok = fw(nc.vector.tensor_tensor(out=carry.t[:], in0=cp.t[:], in1=carry.t[:], op=ALU.add))
                    k.add_r(rp, tok); k.add_r(cp, tok)
                for j in range(12):
                    ins = nc.vector.tensor_scalar(out=cmpn.t[:, :, j], in0=carry.t[:], scalar1=float(512 * j), scalar2=None, op0=ALU.is_gt)
                fw(ins)
                fw(nc.vector.tensor_reduce(out=pe_.t[:], in_=cmpn.t[:], axis=AX, op=ALU.add))
                fw(nc.vector.tensor_scalar(out=pe_.t[:], in0=pe_.t[:], scalar1=512.0, scalar2=None, op0=ALU.mult))
                fw(nc.vector.tensor_copy(out=end_.t[:, 0:1], in_=pe_.t[:, 0:1]))
                for e in range(1, NE):
                    fw(nc.vector.tensor_tensor(out=end_.t[:, e:e + 1], in0=end_.t[:, e - 1:e], in1=pe_.t[:, e:e + 1], op=ALU.add))
                fw(nc.vector.tensor_tensor(out=o1_.t[:], in0=end_.t[:], in1=pe_.t[:], op=ALU.subtract))
                fw(nc.vector.tensor_scalar(out=o1_.t[:], in0=o1_.t[:], scalar1=1.0, scalar2=None, op0=ALU.add))
                for j in range(NB):
                    ins = nc.vector.tensor_scalar(out=cmpE.t[:, j, :], in0=end_.t[:], scalar1=float(512 * j), scalar2=None, op0=ALU.is_le)
                fw(ins)
                fw(nc.vector.tensor_reduce(out=EJ.t[:], in_=cmpE.t[:], axis=AX, op=ALU.add))
                fw(nc.vector.tensor_scalar(out=EJ.t[:], in0=EJ.t[:], scalar1=float(NE - 1), scalar2=None, op0=ALU.min))
                fw(nc.vector.tensor_scalar(out=wf.t[:], in0=EJ.t[:], scalar1=float(NFH * 128), scalar2=iot.t[:, 0:1], op0=ALU.mult, op1=ALU.add))
                fw(nc.vector.tensor_copy(out=WI13.t[:], in_=wf.t[:]))
                for dch in range(4):
                    fw(nc.vector.tensor_scalar(out=wf.t[:], in0=EJ.t[:], scalar1=float(4 * NFG * 128), scalar2=float(dch * NFG * 128), op0=ALU.mult, op1=ALU.add))
                    fw(nc.vector.tensor_scalar(out=wf.t[:], in0=wf.t[:], scalar1=iot.t[:, 0:1], scalar2=None, op0=ALU.add))
                    fw(nc.vector.tensor_copy(out=WI2.t[:].rearrange("p (b d) -> p b d", d=4)[:, :, dch], in_=wf.t[:]))
                for i in range(NT):
                    fw(nc.vector.tensor_tensor(out=key.t[:], in0=RK.t[:, i, :], in1=o1_.t[:], op=ALU.add))
                    fw(nc.vector.tensor_tensor(out=key.t[:], in0=key.t[:], in1=Mt.t[:, i, :], op=ALU.mult))
                    fw(nc.vector.tensor_scalar(out=key.t[:], in0=key.t[:], scalar1=-1.0, scalar2=None, op0=ALU.add))
                    fw(nc.vector.max(out=m8.t[:], in_=key.t[:]))
                    fw(nc.vector.tensor_scalar(out=eq.t[:, 0:2], in0=m8.t[:, 0:2], scalar1=0.0, scalar2=float(NROW + 1), op0=ALU.is_lt, op1=ALU.mult))
                    fw(nc.vector.tensor_tensor(out=eq.t[:, 0:2], in0=eq.t[:, 0:2], in1=m8.t[:, 0:2], op=ALU.add))
                    fw(nc.vector.tensor_copy(out=IDX.t[:, 2 * i:2 * i + 2], in_=eq.t[:, 0:2]))
                    for c in range(2):
                        fw(nc.vector.tensor_scalar(out=eq.t[:], in0=key.t[:], scalar1=m8.t[:, c:c + 1], scalar2=None, op0=ALU.is_equal))
                        fw(nc.vector.tensor_tensor(out=eq.t[:], in0=eq.t[:], in1=CB.t[:, i, :], op=ALU.mult))
                        fw(nc.vector.tensor_reduce(out=G12.t[:, i, c:c + 1], in_=eq.t[:], axis=AX, op=ALU.add))
            with Phase(k) as ph:
                xr = ph.ring(3, [128, 2048], BF16, dma=True, name="xsc")
                for i, (seg, r0) in enumerate(tiles):
                    b = xr[i % 3]
                    k.load('pool', b, b.t[:], seg.x1[r0:r0 + 128, :])
                    k.acq_r('pool', b)
                    for c in range(2):
                        ins = nc.gpsimd.indirect_dma_start(out=xsort[:, :], out_offset=bass.IndirectOffsetOnAxis(ap=IDX.t[:, 2 * i + c:2 * i + c + 1], axis=0),
                                                           in_=b.t[:], in_offset=None)
                        ins.then_inc(b.sem.h, 16)
                        b.sem.n += 16
                        tok = (b.sem, b.sem.n, None)
                        k.add_r(b, tok)
                        k.pending.append(('pool', tok))
            bg(len(bgq))
            for q in ('sp', 'pool'):
                k.wait(q, bgtok.get(wsemB.name))
            with Phase(k) as ph:
                xb = ph.sb([128, 4, 2048], BF16, dma=True, name="xb")
                xT = ph.sb([128, 16, 512], BF16, name="xT")
                gT = ph.sb([128, NFE, 512], BF16, name="gT")
                w13 = ph.ring(3, [128, 4096], BF16, dma=True, name="w13")
                w2r = ph.ring(4, [128, FG, 512], BF16, dma=True, name="w2r")
                yst = ph.ring(2, [128, 4, 512], F32, dma=True, name="yst")
                stt = ph.ring(2, [128, 512], F32, name="silu")
                tpr = ph.pring(1, [128, 4, 512], BF16, name="tp")
                hps = ph.pring(2, [128, 512], F32, name="hps")
                ops = ph.pring(4, [128, 512], F32, name="ops")
                cnt = [0]; wc = 0; w2c = 0; hc = 0; yc = 0
                for j in range(NB):
                    load_xT(ph, xsort[j * 512:(j + 1) * 512, :], xb, xT, tpr, cnt)
                    for f in range(NFE):
                        wb = w13[wc % 3]; wc += 1
                        k.iload(wb, wb.t[:], m13_h[f // NFH][:, :], WI13.t[:, j:j + 1], elem_off=(f % NFH) * 128 * 4096)
                        k.acq_r('pe', wb); k.acq_r('pe', xT)
                        h1 = hps[0]; h3 = hps[1]
                        k.acq_w('pe', h1)
                        for kk in range(16):
                            ins = nc.tensor.matmul(h1.t[:], lhsT=wb.t[:, kk * 128:(kk + 1) * 128], rhs=xT.t[:, kk, :], start=(kk == 0), stop=(kk == 15))
                        k.set_w(h1, k.sig('pe', ins))
                        k.acq_w('pe', h3)
                        for kk in range(16):
                            ins = nc.tensor.matmul(h3.t[:], lhsT=wb.t[:, 2048 + kk * 128:2048 + (kk + 1) * 128], rhs=xT.t[:, kk, :], start=(kk == 0), stop=(kk == 15))
                        tokp = k.sig('pe', ins)
                        k.set_w(h3, tokp); k.add_r(wb, tokp)
                        sb_ = stt[hc % 2]; hc += 1
                        k.acq_r('act', h1); k.acq_w('act', sb_)
                        tok = k.sig('act', nc.scalar.activation(out=sb_.t[:], in_=h1.t[:], func=AF.Silu))
                        k.add_r(h1, tok); k.set_w(sb_, tok)
                        k.acq_r('dve', sb_); k.acq_r('dve', h3)
                        if f == 0:
                            k.acq_w('dve', gT)
                        tok = k.sig('dve', nc.vector.tensor_tensor(out=gT.t[:, f, :], in0=sb_.t[:], in1=h3.t[:], op=ALU.mult))
                        k.add_r(sb_, tok); k.add_r(h3, tok); k.set_w(gT, tok, fresh=(f == 0))
                    k.add_r(xT, tokp)
                    k.acq_r('pe', gT)
                    for dch in range(4):
                        for t in range(4):
                            k.acq_w('pe', ops[t])
                        for fg in range(NFG):
                            wb = w2r[w2c % 4]; w2c += 1
                            k.iload(wb, wb.t[:].rearrange("p f c -> p (f c)"), m2_b[:, :], WI2.t[:, 4 * j + dch:4 * j + dch + 1], elem_off=fg * 128 * 2048)
                            k.acq_r('pe', wb)
                            for fi in range(FG):
                                f = fg * FG + fi
                                for t in range(4):
                                    ins = nc.tensor.matmul(ops[t].t[:], lhsT=gT.t[:, f, t * 128:(t + 1) * 128], rhs=wb.t[:, fi, :], start=(f == 0), stop=(f == NFE - 1))
                            tokp = k.sig('pe', ins)
                            k.add_r(wb, tokp)
                        ys = yst[yc % 2]; yc += 1
                        for t in range(4):
                            k.set_w(ops[t], tokp)
                        for t in range(4):
                            e_ = 'act' if t % 2 == 0 else 'dve'
                            k.acq_r(e_, ops[t]); k.acq_w(e_, ys)
                            if e_ == 'act':
                                ins = nc.scalar.activation(out=ys.t[:, t, :], in_=ops[t].t[:], func=AF.Copy)
                            else:
                                ins = nc.vector.tensor_copy(out=ys.t[:, t, :], in_=ops[t].t[:])
                            tok = k.sig(e_, ins)
                            k.add_r(ops[t], tok); k.set_w(ys, tok, fresh=(t == 0))
                        k.store('sp', ys, ysort[j * 512:(j + 1) * 512, dch * 512:(dch + 1) * 512].rearrange("(t p) c -> p t c", p=128), ys.t[:])
                    k.add_r(gT, tokp)
            with Phase(k) as ph:
                gb = ph.sb([128, 2, 2048], F32, dma=True, name="gb")
                Y1 = ph.ring(2, [128, 2048], F32, dma=True, name="Y1")
                Y2 = ph.ring(2, [128, 2048], F32, dma=True, name="Y2")
                xr = ph.ring(2, [128, 2048], F32, dma=True, name="xr")
                st = ph.sb([128, 4, 6], F32); mv = ph.sb([128, 2], F32); rs = ph.sb([128, 1], F32); nb = ph.sb([128, 1], F32)
                k.load('sp', gb, gb.t[:, 0, :], ln2_g[l].partition_broadcast(128))
                k.load('sp', gb, gb.t[:, 1, :], ln2_b[l].partition_broadcast(128), fresh=False)
                k.acq_r('dve', gb)
                zt_ = Y1[0]
                k.set_w(zt_, k.sig('dve', nc.vector.memset(zt_.t[:], 0.0)))
                ztok = k.store('sp', zt_, ysort[NROW:NROW + 128, :], zt_.t[:])
                k.wait('pool', ztok)
                for i, (seg, r0) in enumerate(tiles):
                    y1 = Y1[i % 2]; y2 = Y2[i % 2]; xb_ = xr[i % 2]
                    for yb, c in ((y1, 0), (y2, 1)):
                        k.iload(yb, yb.t[:], ysort[:, :], IDX.t[:, 2 * i + c:2 * i + c + 1])
                    k.load('sp', xb_, xb_.t[:], seg.x1[r0:r0 + 128, :])
                    k.acq_r('dve', y1); k.acq_r('dve', y2); k.acq_r('dve', xb_)
                    nc.vector.tensor_scalar(out=y1.t[:], in0=y1.t[:], scalar1=G12.t[:, i, 0:1], scalar2=None, op0=ALU.mult)
                    nc.vector.scalar_tensor_tensor(out=y1.t[:], in0=y2.t[:], scalar=G12.t[:, i, 1:2], in1=y1.t[:], op0=ALU.mult, op1=ALU.add)
                    tok = k.sig('dve', nc.vector.scalar_tensor_tensor(out=y1.t[:], in0=xb_.t[:], scalar=float(ALPHA), in1=y1.t[:], op0=ALU.mult, op1=ALU.add))
                    k.add_r(y2, tok); k.add_r(xb_, tok)
                    tok = ln_inplace(y1.t[:], gb, (st, mv, rs, nb))
                    k.set_w(y1, tok, fresh=False)
                    k.store('sp', y1, seg.yout[r0 - seg.yoff:r0 - seg.yoff + 128, :], y1.t[:])
            outer.close()
    return nc


def _host_prep(inputs):
    f32 = np.float32
    w_in = np.asarray(inputs["w_in"], f32)
    L = w_in.shape[0]
    idx = np.arange(64)
    perm = idx.copy()
    perm[0:8] = idx[8:16]
    perm[8:16] = idx[0:8]
    qcols = 1536 + (np.arange(12)[:, None] * 64 + perm[None, :]).reshape(-1)
    kcols = 2304 + (np.arange(12)[:, None] * 64 + perm[None, :]).reshape(-1)
    w_in_ext = np.concatenate([w_in, w_in[:, :, qcols], w_in[:, :, kcols]], axis=-1)
    cpar = np.zeros((L, 128, 6, 34), f32)
    for l in range(L):
        cpar[l, :, :, 0:31] = np.asarray(inputs["conv_w"], f32)[l].T.reshape(6, 128, 31).transpose(1, 0, 2)
        cpar[l, :, :, 31] = np.asarray(inputs["conv_b"], f32)[l].reshape(6, 128).T
        cpar[l, :, :, 32] = np.asarray(inputs["conv_ln_g"], f32)[l].reshape(6, 128).T
        cpar[l, :, :, 33] = np.asarray(inputs["conv_ln_b"], f32)[l].reshape(6, 128).T
    shared = {
        "w_in_ext": np.ascontiguousarray(w_in_ext), "cpar": cpar,
        "maskd": _mult_mask(), "identd": np.eye(128, dtype=f32),
        "triud": np.triu(np.ones((128, 128), f32), 1), "iotad": np.arange(128, dtype=f32).reshape(128, 1),
        "cs_p": _rope_tables(np.arange(PW)), "valid_p": np.ones((128, PW // 128), f32),
    }
    for n in ("w_mem_kv", "w_out", "ln1_g", "ln1_b", "ln2_g", "ln2_b", "ffn_w1", "ffn_w3", "ffn_w2",
              "moe_router", "moe_w1", "moe_w3", "moe_w2"):
        shared[n] = np.ascontiguousarray(np.asarray(inputs[n], f32))
    xp = np.asarray(inputs["x_prompt"], f32)
    xs = np.asarray(inputs["x_sample"], f32)
    mp = np.asarray(inputs["mem_prompt"], f32)
    ms = np.asarray(inputs["mem_sample"], f32)
    in_maps = []
    for c in range(NCORE):
        sq, j = c // 4, c % 4
        a = j * 4096
        lo = a - 2048
        pos = np.arange(lo, lo + SW)
        ok = (pos >= 0) & (pos < 16384)
        xw = np.zeros((SW, D), f32)
        xw[ok] = xs[sq, pos[ok]]
        m = dict(shared)
        m["xp"] = np.ascontiguousarray(xp[c])
        m["xs"] = xw
        m["memp"] = np.ascontiguousarray(mp[c])
        m["mems"] = np.ascontiguousarray(ms[sq])
        m["valid_s"] = np.ascontiguousarray(ok.astype(f32).reshape(SW // 128, 128).T)
        m["cs_s"] = _rope_tables(np.where(ok, pos, 0))
        in_maps.append(m)
    return in_maps


def kernel(**inputs):
    in_maps = _host_prep(inputs)
    nc = build()
    res = run_bass_kernel_spmd(nc, in_maps, core_ids=list(range(NCORE)))
    yp = np.stack([res.results[c]["yp"] for c in range(NCORE)], axis=0)
    ysf = np.zeros((2, 16384, D), np.float32)
    for c in range(NCORE):
        sq, j = c // 4, c % 4
        ysf[sq, j * 4096:(j + 1) * 4096] = res.results[c]["ys"]
    return (yp.astype(np.float32), ysf)
```

```python
import numpy as np
from contextlib import ExitStack
import concourse.bass as bass
import concourse.mybir as mybir
from concourse.bass_utils import run_bass_kernel_spmd

F32 = mybir.dt.float32
BF16 = mybir.dt.bfloat16
AF = mybir.ActivationFunctionType
ALU = mybir.AluOpType

D = 2048
DEPTH = 2
NCORE = 8
D_CONV = 768
D_ATT = 768
D_MEM = 512
D_IN = 4352
D_EXT = D_IN + 2 * D_ATT
NMT = D_EXT // 128
D_FF = 5632
E_FF = 7168
NE = 8
ALPHA = (2 * DEPTH) ** 0.25
LN_EPS = 1e-5
ROPE_THETA = 500000.0
MASK_D0 = 1408
MASK_J = 2944
SW = 8192
PW = 2048


def _mult_mask():
    kk = np.arange(128)[:, None]
    j = np.arange(MASK_J)[None, :]
    o = kk - j + MASK_D0
    ao = np.abs(o)
    c = (ao <= 64).astype(np.float32) + ((o % 4 == 0) & (ao <= 256)) + ((o % 16 == 0) & (ao <= 1024))
    return c.astype(np.float32)


def _rope_tables(pos):
    half = 8
    inv = np.power(np.float32(ROPE_THETA), -np.arange(0, 16, 2, dtype=np.float32) / np.float32(16)).astype(np.float32)
    ang = pos.astype(np.float32)[None, :] * inv[:, None]
    cos = np.cos(ang).astype(np.float32)
    sin = np.sin(ang).astype(np.float32)
    W = pos.shape[0]
    C = np.ones((128, W), np.float32)
    S = np.zeros((128, W), np.float32)
    for hh in range(2):
        b = hh * 64
        C[b:b + 8] = cos
        C[b + 8:b + 16] = cos
        S[b:b + 8] = -sin
        S[b + 8:b + 16] = sin
    return np.stack([C, S], axis=0)


class Sem:
    def __init__(self, nc, name):
        self.h = nc.semaphore(name).__enter__()
        self.n = 0
        self.name = name


class Buf:
    def __init__(self, tile, sem=None):
        self.t = tile
        self.rd = []
        self.wr = []
        self.sem = sem


class KB:
    def __init__(self):
        self.nc = bass.Bass("TRN2", target_bir_lowering=False)
        nc = self.nc
        self.eng = {'pe': nc.tensor, 'act': nc.scalar, 'dve': nc.vector, 'pool': nc.gpsimd, 'sp': nc.sync}
        self.esem = {e: Sem(nc, "e_" + e) for e in ('pe', 'act', 'dve', 'pool')}
        self.dsems = [Sem(nc, f"d{i}") for i in range(72)]
        self.dfree = list(self.dsems)
        self.waited = {}
        self.pending = []
        self.uid = 0

    def sig(self, e, ins):
        s = self.esem[e]
        ins.then_inc(s.h, 1)
        s.n += 1
        return (s, s.n, e)

    def wait(self, e, tok, force=False):
        if tok is None or (tok[2] == e and not force):
            return
        key = (e, tok[0].name)
        if self.waited.get(key, 0) >= tok[1]:
            return
        self.waited[key] = tok[1]
        self.eng[e].wait_ge(tok[0].h, tok[1])

    def dma(self, q, out, in_, sem):
        ins = self.eng[q].dma_start(out=out, in_=in_)
        ins.then_inc(sem.h, 16)
        sem.n += 16
        return (sem, sem.n, None)

    def getsem(self):
        return self.dfree.pop()

    def acq_w(self, e, b):
        for t in b.rd + b.wr:
            self.wait(e, t)

    def set_w(self, b, tok, fresh=True):
        if fresh:
            b.rd = []
            b.wr = [tok]
        else:
            b.wr.append(tok)

    def acq_r(self, e, b):
        for t in b.wr:
            self.wait(e, t)

    def add_r(self, b, tok):
        b.rd.append(tok)

    def load(self, q, b, out, in_, fresh=True):
        self.acq_w(q, b)
        tok = self.dma(q, out, in_, b.sem)
        self.set_w(b, tok, fresh)
        return tok

    def iload(self, b, out, in_, idx_ap, elem_off=0, fresh=True, bounds=None):
        self.acq_w('pool', b)
        kw = {}
        if bounds is not None:
            kw = dict(bounds_check=bounds, oob_is_err=False)
        ins = self.nc.gpsimd.indirect_dma_start(out=out, out_offset=None, in_=in_,
                                                in_offset=bass.IndirectOffsetOnAxis(ap=idx_ap, axis=0),
                                                element_offset=elem_off, **kw)
        ins.then_inc(b.sem.h, 16)
        b.sem.n += 16
        tok = (b.sem, b.sem.n, None)
        self.set_w(b, tok, fresh)
        return tok

    def store(self, q, b, out, in_):
        self.acq_r(q, b)
        tok = self.dma(q, out, in_, b.sem)
        self.add_r(b, tok)
        self.pending.append((q, tok))
        return tok

    def phase_end(self):
        for q, tok in self.pending:
            self.wait(q, tok)
        self.pending = []
        self.nc.all_engine_barrier()


class Phase:
    def __init__(self, k):
        self.k = k
        self.es = ExitStack()
        self.sems = []

    def __enter__(self):
        self.es.__enter__()
        return self

    def __exit__(self, *a):
        self.k.phase_end()
        for s in self.sems:
            self.k.dfree.append(s)
        return self.es.__exit__(*a)

    def sb(self, shape, dt, dma=False, name=None):
        k = self.k
        k.uid += 1
        t = self.es.enter_context(k.nc.sbuf_tensor(f"{name or 't'}_{k.uid}", list(shape), dt))
        s = None
        if dma:
            s = k.getsem()
            self.sems.append(s)
        return Buf(t, s)

    def ps(self, shape, dt, name=None):
        k = self.k
        k.uid += 1
        t = self.es.enter_context(k.nc.psum_tensor(f"{name or 'p'}_{k.uid}", list(shape), dt))
        return Buf(t)

    def ring(self, n, shape, dt, dma=False, name=None):
        return [self.sb(shape, dt, dma, name) for _ in range(n)]

    def pring(self, n, shape, dt, name=None):
        return [self.ps(shape, dt, name) for _ in range(n)]


class Seg:
    pass


def build(dbg=False, stop=None, only_p=False):
    k = KB()
    nc = k.nc
    E = k.eng

    def din(name, shape, dt=F32):
        return nc.dram_tensor(name, list(shape), dt, kind="ExternalInput").ap()

    def dscr(name, shape, dt, out=False):
        return nc.dram_tensor(name, list(shape), dt, kind=("ExternalOutput" if out else "Internal")).ap()

    xp = din("xp", [PW, D])
    xs = din("xs", [SW, D])
    memp = din("memp", [256, D])
    mems = din("mems", [256, D])
    valid_s = din("valid_s", [128, SW // 128])
    valid_p = din("valid_p", [128, PW // 128])
    cs_p = din("cs_p", [2, 128, PW])
    cs_s = din("cs_s", [2, 128, SW])
    maskd = din("maskd", [128, MASK_J])
    identd = din("identd", [128, 128])
    triud = din("triud", [128, 128])
    iotad = din("iotad", [128, 1])
    w_in = din("w_in_ext", [DEPTH, D, D_EXT])
    cpar = din("cpar", [DEPTH, 128, 6, 34])
    w_memkv = din("w_mem_kv", [DEPTH, D, 1024])
    w_out = din("w_out", [DEPTH, D, D])
    ln1_g = din("ln1_g", [DEPTH, D]); ln1_b = din("ln1_b", [DEPTH, D])
    ln2_g = din("ln2_g", [DEPTH, D]); ln2_b = din("ln2_b", [DEPTH, D])
    ffn_w1 = din("ffn_w1", [1, D, D_FF]); ffn_w3 = din("ffn_w3", [1, D, D_FF]); ffn_w2 = din("ffn_w2", [1, D_FF, D])
    router = din("moe_router", [1, D, NE])
    moe_w1 = din("moe_w1", [1, NE, D, E_FF]); moe_w3 = din("moe_w3", [1, NE, D, E_FF]); moe_w2 = din("moe_w2", [1, NE, E_FF, D])
    yp = nc.dram_tensor("yp", [PW, D], F32, kind="ExternalOutput").ap()
    ys = nc.dram_tensor("ys", [4096, D], F32, kind="ExternalOutput").ap()

    win_b = [dscr(f"win_b{l}", [NMT, 128, 16 * 128], BF16) for l in range(DEPTH)]
    wkv_b = [dscr(f"wkv_b{l}", [8, 128, 16 * 128], BF16) for l in range(DEPTH)]
    wout_b = [dscr(f"wout_b{l}", [D, D], BF16) for l in range(DEPTH)]
    f1_b = dscr("f1_b", [D_FF // 128, 128, 2048], BF16)
    f3_b = dscr("f3_b", [D_FF // 128, 128, 2048], BF16)
    f2_b = dscr("f2_b", [D_FF, D], BF16)
    NFE = E_FF // 128
    FG = 4
    NFG = NFE // FG
    NFH = NFE // 2
    m13_h = [dscr(f"m13_b{h}", [NE * NFH * 128, 2 * 2048], BF16) for h in range(2)]
    m2_b = dscr("m2_b", [NE * 4 * NFG * 128, FG * 512], BF16)

    segs = []
    for nm, W, xin, mem, valid, cs, hr, orr, yout, yoff in (
            ("p", PW, xp, memp, valid_p, cs_p, [(0, PW), (0, PW)], [(0, PW), (0, PW)], yp, 0),
            ("s", SW, xs, mems, valid_s, cs_s, [(0, SW), (1024, 7168)], [(1024, 7168), (2048, 6144)], ys, 2048)):
        s = Seg()
        s.nm, s.W, s.x0, s.mem, s.valid, s.cs, s.hr, s.orr, s.yout, s.yoff = nm, W, xin, mem, valid, cs, hr, orr, yout, yoff
        s.u = dscr(f"u_{nm}", [D_CONV, W], BF16, out=dbg)
        s.q = dscr(f"q_{nm}", [D_ATT, W], BF16, out=dbg)
        s.kk = dscr(f"k_{nm}", [D_ATT, W], BF16, out=dbg)
        s.qm = dscr(f"qm_{nm}", [D_MEM, W], BF16, out=dbg)
        s.v = dscr(f"v_{nm}", [W, 12 * 128], BF16, out=dbg)
        s.mix = dscr(f"mix_{nm}", [D, W], BF16, out=dbg)
        s.x1 = dscr(f"x1_{nm}", [W, D], F32, out=dbg)
        s.x2 = dscr(f"x2_{nm}", [W, D], F32, out=dbg)
        s.comb = dscr(f"comb_{nm}", [W, NE], F32, out=dbg)
        segs.append(s)
    if only_p:
        segs = segs[:1]

    wsem = k.getsem()
    wsemA = k.getsem()
    wsemB = k.getsem()
    wsemF = k.getsem()
    bgq = []
    bgtok = {}

    def conv_tiled(src, dst, ncols, sem):
        for m in range(ncols // 128):
            s_ap = src[:, m * 128:(m + 1) * 128].rearrange("(k p) c -> p k c", p=128)
            d_ap = dst[m].rearrange("p (k c) -> p k c", c=128)
            bgq.append((d_ap, s_ap, sem))

    def conv_plain(src, dst, nrows, sem):
        for r in range(0, nrows, 128):
            bgq.append((dst[r:r + 128, :], src[r:r + 128, :], sem))

    def bg(n):
        for _ in range(n):
            if not bgq:
                return
            d_ap, s_ap, sem = bgq.pop(0)
            bgtok[sem.name] = k.dma('pool', d_ap, s_ap, sem)

    for l in range(DEPTH):
        sem = wsem if l == 0 else wsemA
        conv_tiled(w_in[l], win_b[l], D_EXT, sem)
        conv_tiled(w_memkv[l], wkv_b[l], 1024, sem)
        conv_plain(w_out[l], wout_b[l], D, sem)
        if l == 0:
            bg(len(bgq))
            conv_tiled(ffn_w1[0], f1_b, D_FF, wsemF)
            conv_tiled(ffn_w3[0], f3_b, D_FF, wsemF)
            conv_plain(ffn_w2[0], f2_b, D_FF, wsemF)
    if stop is None or stop > 10:
        for e in range(NE):
            for m in range(NFE):
                r0 = (e * NFH + (m % NFH)) * 128
                for wi, src in enumerate((moe_w1[0, e], moe_w3[0, e])):
                    s_ap = src[:, m * 128:(m + 1) * 128].rearrange("(k p) c -> p k c", p=128)
                    d_ap = m13_h[m // NFH][r0:r0 + 128, wi * 2048:(wi + 1) * 2048].rearrange("p (k c) -> p k c", c=128)
                    bgq.append((d_ap, s_ap, wsemB))
            for fg in range(NFG):
                for dch in range(4):
                    r0 = ((e * 4 + dch) * NFG + fg) * 128
                    s_ap = moe_w2[0, e][fg * 512:(fg + 1) * 512, dch * 512:(dch + 1) * 512].rearrange("(fi p) c -> p fi c", p=128)
                    d_ap = m2_b[r0:r0 + 128, :].rearrange("p (fi c) -> p fi c", c=512)
                    bgq.append((d_ap, s_ap, wsemB))
    k.pending = [('pool', bgtok[wsem.name])]
    k.phase_end()

    cst = ExitStack()
    ident_b = Buf(cst.enter_context(nc.sbuf_tensor("ident_b", [128, 128], BF16)), k.getsem())
    ident_f = Buf(cst.enter_context(nc.sbuf_tensor("ident_f", [128, 128], F32)), k.getsem())
    ones_b = Buf(cst.enter_context(nc.sbuf_tensor("ones_b", [128, 128], BF16)))
    ones_f = Buf(cst.enter_context(nc.sbuf_tensor("ones_f", [128, 128], F32)))
    epsb = Buf(cst.enter_context(nc.sbuf_tensor("epsb", [128, 1], F32)))
    k.load('pool', ident_b, ident_b.t[:], identd[:, :])
    k.load('sp', ident_f, ident_f.t[:], identd[:, :])
    k.set_w(ones_b, k.sig('dve', nc.vector.memset(ones_b.t[:], 1.0)))
    k.set_w(ones_f, k.sig('dve', nc.vector.memset(ones_f.t[:], 1.0)))
    k.set_w(epsb, k.sig('dve', nc.vector.memset(epsb.t[:], LN_EPS)))
    for e in ('pe', 'act', 'dve', 'pool'):
        k.acq_r(e, ident_b); k.acq_r(e, ident_f); k.acq_r(e, ones_b); k.acq_r(e, ones_f); k.acq_r(e, epsb)

    def xload(src_rows, xb):
        k.load('pool', xb, xb.t[:], src_rows.rearrange("(t p) d -> p t d", p=128))

    def load_xT(ph, src_rows, xb, xT, tp_ring, cnt):
        xload(src_rows, xb)
        xtrans(xb, xT, tp_ring, cnt)

    def xtrans(xb, xT, tp_ring, cnt):
        k.acq_r('pe', xb)
        k.acq_w('act', xT); k.acq_w('dve', xT)
        first = True
        tokp_last = [None]
        for kg in range(4):
            tp = tp_ring[cnt[0] % len(tp_ring)]; cnt[0] += 1
            k.acq_w('pe', tp)
            for kk in range(4):
                for t in range(4):
                    ins = nc.tensor.transpose(tp.t[:, kk, t * 128:(t + 1) * 128], xb.t[:, t, (kg * 4 + kk) * 128:(kg * 4 + kk + 1) * 128], ident_b.t[:])
            tokp_last[0] = k.sig('pe', ins)
            k.set_w(tp, tokp_last[0])
            e = 'act' if kg % 2 == 0 else 'dve'
            k.acq_r(e, tp)
            if e == 'act':
                ins = nc.scalar.activation(out=xT.t[:, kg * 4:(kg + 1) * 4, :], in_=tp.t[:], func=AF.Copy)
            else:
                ins = nc.vector.tensor_copy(out=xT.t[:, kg * 4:(kg + 1) * 4, :], in_=tp.t[:])
            tok = k.sig(e, ins)
            k.add_r(tp, tok)
            k.set_w(xT, tok, fresh=first)
            first = False
        k.add_r(xb, tokp_last[0])

    def ln_inplace(yt, gb, rs_pool, valid_ap=None):
        st, mv, rs, nb = rs_pool
        for ch in range(4):
            ins = nc.vector.bn_stats(out=st.t[:, ch, :], in_=yt[:, ch * 512:(ch + 1) * 512])
        k.wait('dve', k.sig('dve', ins), force=True)
        tok = k.sig('dve', nc.vector.bn_aggr(out=mv.t[:], in_=st.t[:].rearrange("p a b -> p (a b)")))
        k.wait('act', tok)
        tok = k.sig('act', nc.scalar.activation(out=rs.t[:], in_=mv.t[:, 1:2], func=AF.Sqrt, bias=epsb.t[:, 0:1], scale=1.0))
        k.wait('dve', tok)
        tok = k.sig('dve', nc.vector.reciprocal(out=rs.t[:], in_=rs.t[:]))
        k.wait('dve', tok, force=True)
        tok = k.sig('dve', nc.vector.tensor_scalar(out=nb.t[:], in0=mv.t[:, 0:1], scalar1=rs.t[:, 0:1], scalar2=-1.0, op0=ALU.mult, op1=ALU.mult))
        k.wait('act', tok)
        tok = k.sig('act', nc.scalar.activation(out=yt, in_=yt, func=AF.Identity, bias=nb.t[:, 0:1], scale=rs.t[:, 0:1]))
        k.wait('dve', tok)
        nc.vector.tensor_tensor(out=yt, in0=yt, in1=gb.t[:, 0, :], op=ALU.mult)
        ins = nc.vector.tensor_tensor(out=yt, in0=yt, in1=gb.t[:, 1, :], op=ALU.add)
        if valid_ap is not None:
            ins = nc.vector.tensor_scalar(out=yt, in0=yt, scalar1=valid_ap, scalar2=None, op0=ALU.mult)
        return k.sig('dve', ins)

    for l in range(DEPTH):
        if stop is not None and stop <= l * 10:
            break
        if l == 1:
            while bgq and bgq[0][2] is wsemA:
                bg(1)
            for q in ('sp', 'pool'):
                k.wait(q, bgtok.get(wsemA.name))
        for seg in segs:
            xin = seg.x0 if l == 0 else seg.x2
            hs, he = seg.hr[l]
            os_, oe = seg.orr[l]
            W = seg.W
            with Phase(k) as ph:
                xbs = ph.ring(2, [128, 4, 2048], BF16, dma=True, name="xb")
                xTs = ph.ring(2, [128, 16, 512], BF16, name="xT")
                wv = ph.sb([128, 6, 2048], BF16, dma=True, name="wv")
                wt = ph.ring(6, [128, 2048], BF16, dma=True, name="wt")
                cst_ = ph.ring(2, [128, 2, 512], F32, dma=True, name="cs")
                vt = ph.sb([128, W // 128], F32, dma=True, name="vt")
                onesv = ph.sb([128, 6, 64], BF16, name="onesv")
                sg = ph.ring(2, [128, 512], F32, name="sg")
                uo = ph.ring(2, [128, 512], BF16, dma=True, name="uo")
                t1 = ph.ring(2, [128, 512], F32, name="t1")
                t2 = ph.ring(2, [128, 512], F32, name="t2")
                qo = ph.ring(2, [128, 512], BF16, dma=True, name="qo")
                qmo = ph.ring(2, [128, 512], BF16, dma=True, name="qmo")
                vx = ph.ring(2, [128, 12, 128], BF16, dma=True, name="vx")
                tpr = ph.pring(2, [128, 4, 512], BF16, name="tp")
                mm = ph.pring(4, [128, 512], F32, name="mm")
                cnt = [0]
                mmc = [0]
                wtc = [0]
                k.set_w(onesv, k.sig('pool', nc.gpsimd.memset(onesv.t[:], 1.0)))
                k.load('sp', vt, vt.t[:], seg.valid[:, :])
                for m in range(6):
                    k.load('sp', wv, wv.t[:, m, :], win_b[l][24 + m], fresh=(m == 0))
                k.acq_r('pe', wv)
                k.acq_r('pool', vt)

                def mtile(m, xTb):
                    wb = wt[wtc[0] % 6]; wtc[0] += 1
                    k.load('sp', wb, wb.t[:], win_b[l][m])
                    pb = mm[mmc[0] % 4]; mmc[0] += 1
                    k.acq_r('pe', wb); k.acq_w('pe', pb); k.acq_r('pe', xTb)
                    for kk in range(16):
                        ins = nc.tensor.matmul(pb.t[:], lhsT=wb.t[:, kk * 128:(kk + 1) * 128], rhs=xTb.t[:, kk, :], start=(kk == 0), stop=(kk == 15))
                    tok = k.sig('pe', ins)
                    k.set_w(pb, tok); k.add_r(wb, tok)
                    return pb, tok

                nblk = (he - hs) // 512
                load_xT(ph, xin[hs:hs + 512, :], xbs[0], xTs[0], tpr, cnt)
                for b in range(nblk):
                    t0 = hs + b * 512
                    xT = xTs[b % 2]
                    if b + 1 < nblk:
                        xload(xin[t0 + 512:t0 + 1024, :], xbs[(b + 1) % 2])
                    bg(8)
                    csb = cst_[b % 2]
                    k.load('sp', csb, csb.t[:], seg.cs[:, :, t0:t0 + 512].rearrange("a p t -> p a t"))
                    lasttok = None
                    for c in range(6):
                        pa, _ = mtile(c, xT)
                        pg, _ = mtile(6 + c, xT)
                        sgb = sg[c % 2]; uob = uo[c % 2]
                        k.acq_r('act', pg); k.acq_w('act', sgb)
                        tok = k.sig('act', nc.scalar.activation(out=sgb.t[:], in_=pg.t[:], func=AF.Sigmoid))
                        k.add_r(pg, tok); k.set_w(sgb, tok)
                        k.acq_r('dve', pa); k.acq_r('dve', sgb); k.acq_w('dve', uob)
                        tok = k.sig('dve', nc.vector.tensor_tensor(out=uob.t[:], in0=pa.t[:], in1=sgb.t[:], op=ALU.mult))
                        k.add_r(pa, tok); k.add_r(sgb, tok); k.set_w(uob, tok)
                        k.store('pool', uob, seg.u[c * 128:(c + 1) * 128, t0:t0 + 512], uob.t[:])
                    k.acq_r('dve', csb)
                    for which, base, pbase, dst in (("q", 12, 34, seg.q), ("k", 18, 40, seg.kk)):
                        for hp in range(6):
                            pq, _ = mtile(base + hp, xT)
                            pp, _ = mtile(pbase + hp, xT)
                            i2 = hp % 2
                            k.acq_r('dve', pq); k.acq_w('dve', t1[i2])
                            tok = k.sig('dve', nc.vector.tensor_tensor(out=t1[i2].t[:], in0=pq.t[:], in1=csb.t[:, 0, :], op=ALU.mult))
                            k.add_r(pq, tok); k.set_w(t1[i2], tok)
                            k.acq_r('dve', pp); k.acq_w('dve', t2[i2])
                            tok = k.sig('dve', nc.vector.tensor_tensor(out=t2[i2].t[:], in0=pp.t[:], in1=csb.t[:, 1, :], op=ALU.mult))
                            k.add_r(pp, tok); k.set_w(t2[i2], tok); k.add_r(csb, tok)
                            k.acq_r('pool', t1[i2]); k.acq_r('pool', t2[i2]); k.acq_w('pool', qo[i2])
                            tok = k.sig('pool', nc.gpsimd.tensor_tensor(out=qo[i2].t[:], in0=t1[i2].t[:], in1=t2[i2].t[:], op=ALU.add))
                            k.add_r(t1[i2], tok); k.add_r(t2[i2], tok); k.set_w(qo[i2], tok)
                            k.store('pool', qo[i2], dst[hp * 128:(hp + 1) * 128, t0:t0 + 512], qo[i2].t[:])
                    if b + 1 < nblk:
                        xtrans(xbs[(b + 1) % 2], xTs[(b + 1) % 2], tpr, cnt)
                    for mh in range(4):
                        pq, _ = mtile(30 + mh, xT)
                        ob = qmo[mh % 2]
                        k.acq_r('act', pq); k.acq_w('act', ob)
                        tok = k.sig('act', nc.scalar.activation(out=ob.t[:], in_=pq.t[:], func=AF.Copy))
                        k.add_r(pq, tok); k.set_w(ob, tok)
                        k.store('pool', ob, seg.qm[mh * 128:(mh + 1) * 128, t0:t0 + 512], ob.t[:])
                    for t in range(4):
                        vb = vx[t % 2]
                        tile_idx = (t0 // 128) + t
                        k.acq_w('pool', vb)
                        v5 = vb.t[:].rearrange("p (a two) d -> p a two d", two=2)
                        nc.gpsimd.tensor_scalar(out=v5[:, :, 0, 64:128], in0=onesv.t[:], scalar1=vt.t[:, tile_idx:tile_idx + 1], scalar2=None, op0=ALU.mult)
                        tok = k.sig('pool', nc.gpsimd.tensor_scalar(out=v5[:, :, 1, 0:64], in0=onesv.t[:], scalar1=vt.t[:, tile_idx:tile_idx + 1], scalar2=None, op0=ALU.mult))
                        k.set_w(vb, tok)
                        for (c0, ncol, h0, nh) in ((0, 512, 0, 8), (512, 256, 8, 4)):
                            pb = mm[mmc[0] % 4]; mmc[0] += 1
                            k.acq_w('pe', pb); k.acq_r('pe', xT)
                            for kk in range(16):
                                ins = nc.tensor.matmul(pb.t[:, 0:ncol], lhsT=xT.t[:, kk, t * 128:(t + 1) * 128],
                                                       rhs=wv.t[:, c0 // 128:(c0 + ncol) // 128, kk * 128:(kk + 1) * 128],
                                                       start=(kk == 0), stop=(kk == 15))
                            tokp = k.sig('pe', ins)
                            lasttok = tokp
                            k.set_w(pb, tokp)
                            p4 = pb.t[:, 0:ncol].rearrange("p (a two d) -> p a two d", two=2, d=64)
                            d4 = vb.t[:, h0:h0 + nh, :].rearrange("p (a two) d -> p a two d", two=2)
                            k.acq_r('act', pb); k.acq_w('act', vb)
                            tok = k.sig('act', nc.scalar.activation(out=d4[:, :, 0, 0:64], in_=p4[:, :, 0, :], func=AF.Copy))
                            k.add_r(pb, tok); k.set_w(vb, tok, fresh=False)
                            k.acq_r('dve', pb); k.acq_w('dve', vb)
                            tok = k.sig('dve', nc.vector.tensor_copy(out=d4[:, :, 1, 64:128], in_=p4[:, :, 1, :]))
                            k.add_r(pb, tok); k.set_w(vb, tok, fresh=False)
                        k.store('pool', vb, seg.v[t0 + t * 128:t0 + (t + 1) * 128, :], vb.t[:].rearrange("p h d -> p (h d)"))
                    k.add_r(xT, lasttok)
            if stop is not None and stop <= l * 10 + 1:
                continue
            with Phase(k) as ph:
                cw = ph.sb([128, 6, 34], F32, dma=True, name="cw")
                dg = ph.sb([128, 6, 31, 128], BF16, name="dg")
                ut = ph.ring(2, [128, 6, 544], BF16, dma=True, name="ut")
                acc = ph.ring(2, [128, 6, 512], F32, name="acc")
                ysq = ph.ring(2, [128, 512], F32, name="ysq")
                mean = ph.sb([128, 512], F32, name="mean")
                msq = ph.sb([128, 512], F32, name="msq")
                rstd = ph.sb([128, 512], F32, name="rstd")
                zt = ph.ring(2, [128, 512], F32, name="zt")
                co = ph.ring(2, [128, 512], BF16, dma=True, name="co")
                cps = ph.pring(3, [128, 512], F32, name="cps")
                sps = ph.pring(2, [128, 512], F32, name="sps")
                k.load('sp', cw, cw.t[:], cpar[l])
                k.acq_r('dve', cw); k.acq_r('act', cw)
                for c in range(6):
                    for j in range(31):
                        ins = nc.vector.tensor_scalar(out=dg.t[:, c, j, :], in0=ident_f.t[:], scalar1=cw.t[:, c, j:j + 1], scalar2=None, op0=ALU.mult)
                k.set_w(dg, k.sig('dve', ins))
                k.acq_r('pe', dg)
                nblk = (oe - os_) // 512
                cc = 0
                for b in range(nblk):
                    t0 = os_ + b * 512
                    ub = ut[b % 2]; ab = acc[b % 2]
                    lo = max(hs, t0 - 16); hi = min(he, t0 + 528)
                    fresh = True
                    if lo > t0 - 16 or hi < t0 + 528:
                        k.acq_w('pool', ub)
                        k.set_w(ub, k.sig('pool', nc.gpsimd.memset(ub.t[:], 0.0)))
                        fresh = False
                    for c in range(6):
                        k.load('sp', ub, ub.t[:, c, lo - (t0 - 16):hi - (t0 - 16)], seg.u[c * 128:(c + 1) * 128, lo:hi], fresh=(fresh and c == 0))
                    k.acq_r('pe', ub)
                    k.acq_w('pe', sps[0]); k.acq_w('pe', sps[1])
                    k.acq_w('act', ab)
                    for c in range(6):
                        pb = cps[cc % 3]; cc += 1
                        k.acq_w('pe', pb)
                        for j in range(31):
                            ins = nc.tensor.matmul(pb.t[:], lhsT=dg.t[:, c, j, :], rhs=ub.t[:, c, j + 1:j + 513], start=(j == 0), stop=(j == 30))
                        tokc = k.sig('pe', ins)
                        k.set_w(pb, tokc)
                        k.acq_r('act', pb)
                        tok = k.sig('act', nc.scalar.activation(out=ab.t[:, c, :], in_=pb.t[:], func=AF.Identity, bias=cw.t[:, c, 31:32], scale=1.0))
                        k.add_r(pb, tok); k.set_w(ab, tok, fresh=(c == 0))
                        k.wait('act', tok, force=True)
                        yb = ysq[c % 2]
                        k.acq_w('act', yb)
                        toka = k.sig('act', nc.scalar.activation(out=yb.t[:], in_=ab.t[:, c, :], func=AF.Square))
                        k.set_w(yb, toka)
                        k.wait('pe', tok)
                        nc.tensor.matmul(sps[0].t[:], lhsT=ones_f.t[:], rhs=ab.t[:, c, :], start=(c == 0), stop=(c == 5))
                        k.wait('pe', toka)
                        tokp = k.sig('pe', nc.tensor.matmul(sps[1].t[:], lhsT=ones_f.t[:], rhs=yb.t[:], start=(c == 0), stop=(c == 5)))
                        k.add_r(yb, tokp)
                    k.add_r(ub, tokc)
                    k.set_w(sps[0], tokp); k.set_w(sps[1], tokp)
                    k.add_r(ab, tokp)
                    k.wait('act', tokp); k.acq_w('act', mean)
                    tokm = k.sig('act', nc.scalar.activation(out=mean.t[:], in_=sps[0].t[:], func=AF.Copy, scale=1.0 / D_CONV))
                    k.set_w(mean, tokm); k.add_r(sps[0], tokm)
                    k.wait('dve', tokm); k.wait('dve', tokp)
                    nc.vector.tensor_tensor(out=msq.t[:], in0=mean.t[:], in1=mean.t[:], op=ALU.mult)
                    tok = k.sig('dve', nc.vector.scalar_tensor_tensor(out=msq.t[:], in0=sps[1].t[:], scalar=1.0 / D_CONV, in1=msq.t[:], op0=ALU.mult, op1=ALU.subtract))
                    k.add_r(sps[1], tok)
                    k.wait('act', tok)
                    tok = k.sig('act', nc.scalar.activation(out=rstd.t[:], in_=msq.t[:], func=AF.Sqrt, bias=epsb.t[:, 0:1], scale=1.0))
                    k.wait('dve', tok)
                    nc.vector.reciprocal(out=rstd.t[:], in_=rstd.t[:])
                    k.acq_r('dve', ab)
                    for c in range(6):
                        zb = zt[c % 2]; cb = co[c % 2]
                        k.acq_w('dve', zb)
                        nc.vector.tensor_tensor(out=zb.t[:], in0=ab.t[:, c, :], in1=mean.t[:], op=ALU.subtract)
                        tok = k.sig('dve', nc.vector.tensor_tensor(out=zb.t[:], in0=zb.t[:], in1=rstd.t[:], op=ALU.mult))
                        k.set_w(zb, tok)
                        k.acq_r('act', zb); k.acq_w('act', cb)
                        toka = k.sig('act', nc.scalar.activation(out=cb.t[:], in_=zb.t[:], func=AF.Silu, bias=cw.t[:, c, 33:34], scale=cw.t[:, c, 32:33]))
                        k.add_r(zb, toka); k.set_w(cb, toka)
                        k.store('pool', cb, seg.mix[c * 128:(c + 1) * 128, t0:t0 + 512], cb.t[:])
                    k.add_r(ab, tok); k.add_r(mean, tok)
            if stop is not None and stop <= l * 10 + 2:
                continue
            with Phase(k) as ph:
                mk = ph.sb([128, MASK_J], BF16, dma=True, name="mk")
                qt = ph.ring(2, [128, 512], BF16, dma=True, name="qt")
                kt = ph.ring(2, [128, 2560], BF16, dma=True, name="kt")
                vxl = ph.ring(2, [128, 20, 256], BF16, dma=True, name="vxl")
                NS = 6
                LA = 3
                pe_t = ph.ring(NS, [128, 512], BF16, name="pexp")
                pm_t = ph.ring(NS, [128, 512], BF16, name="pmsk")
                rd = ph.ring(2, [128, 512], F32, name="rd")
                rd2 = ph.ring(2, [128, 512], F32, name="rd2")
                att = ph.ring(2, [128, 512], BF16, dma=True, name="att")
                sp_ = ph.pring(NS, [128, 512], F32, name="sps")
                ap_ = ph.pring(2, [128, 512], F32, name="aps")
                k.load('pool', mk, mk.t[:], maskd[:, :])
                k.acq_r('dve', mk)
                it = 0; sc = 0; ac = 0
                nqb = (oe - os_) // 512
                for qb in range(nqb):
                    q0 = os_ + qb * 512
                    k0 = max(hs, q0 - 1024); k1 = min(he, q0 + 512 + 1024)
                    nkb = (k1 - k0) // 128
                    for hp in range(6):
                        qb_ = qt[it % 2]; kb_ = kt[it % 2]; vb_ = vxl[it % 2]; ab_ = att[it % 2]; it += 1
                        bg(4)
                        k.load('sp', qb_, qb_.t[:], seg.q[hp * 128:(hp + 1) * 128, q0:q0 + 512])
                        k.load('sp', kb_, kb_.t[:, 0:k1 - k0], seg.kk[hp * 128:(hp + 1) * 128, k0:k1])
                        k.load('sp', vb_, vb_.t[:, 0:nkb, :], seg.v[k0:k1, hp * 256:(hp + 1) * 256].rearrange("(kb p) c -> p kb c", p=128))
                        k.acq_r('pe', qb_); k.acq_r('pe', kb_); k.acq_r('pe', vb_)
                        for hh in range(2):
                            r0 = hh * 64
                            accb = ap_[ac % 2]; ac += 1
                            k.acq_w('pe', accb)
                            pend = []
                            for kbi in range(nkb):
                                d = (k0 + kbi * 128) - q0
                                j0 = MASK_D0 - d
                                sb_ = sp_[sc % NS]; peb = pe_t[sc % NS]; pmb = pm_t[sc % NS]; sc += 1
                                k.acq_w('pe', sb_)
                                tok = k.sig('pe', nc.tensor.matmul(sb_.t[:], lhsT=kb_.t[r0:r0 + 64, kbi * 128:(kbi + 1) * 128], rhs=qb_.t[r0:r0 + 64, :], start=True, stop=True))
                                k.set_w(sb_, tok)
                                k.acq_r('act', sb_); k.acq_w('act', peb)
                                tok = k.sig('act', nc.scalar.activation(out=peb.t[:], in_=sb_.t[:], func=AF.Exp, scale=0.125))
                                k.add_r(sb_, tok); k.set_w(peb, tok)
                                k.acq_r('dve', peb); k.acq_w('dve', pmb)
                                tok = k.sig('dve', nc.vector.tensor_tensor(out=pmb.t[:], in0=peb.t[:], in1=mk.t[:, j0:j0 + 512], op=ALU.mult))
                                k.add_r(peb, tok); k.set_w(pmb, tok)
                                pend.append((pmb, kbi))
                                if len(pend) > LA:
                                    pb2, kb2 = pend.pop(0)
                                    k.acq_r('pe', pb2)
                                    tok = k.sig('pe', nc.tensor.matmul(accb.t[:], lhsT=vb_.t[:, kb2, hh * 128:(hh + 1) * 128], rhs=pb2.t[:], start=(kb2 == 0), stop=False))
                                    k.add_r(pb2, tok)
                            while pend:
                                pb2, kb2 = pend.pop(0)
                                k.acq_r('pe', pb2)
                                tok = k.sig('pe', nc.tensor.matmul(accb.t[:], lhsT=vb_.t[:, kb2, hh * 128:(hh + 1) * 128], rhs=pb2.t[:], start=(kb2 == 0), stop=(not pend)))
                                k.add_r(pb2, tok)
                            k.set_w(accb, tok)
                            nr = r0; dr = 64 - r0
                            rdb = rd[hh]; rd2b = rd2[hh]
                            k.acq_r('dve', accb); k.acq_w('dve', rdb)
                            tok = k.sig('dve', nc.vector.reciprocal(out=rdb.t[dr:dr + 64, :], in_=accb.t[dr:dr + 64, :]))
                            k.set_w(rdb, tok)
                            k.acq_r('act', rdb); k.acq_w('act', rd2b)
                            tok = k.sig('act', nc.scalar.activation(out=rd2b.t[nr:nr + 64, :], in_=rdb.t[dr:dr + 64, :], func=AF.Copy))
                            k.add_r(rdb, tok); k.set_w(rd2b, tok)
                            k.acq_r('dve', rd2b)
                            if hh == 0:
                                k.acq_w('dve', ab_)
                            tok = k.sig('dve', nc.vector.tensor_tensor(out=ab_.t[nr:nr + 64, :], in0=accb.t[nr:nr + 64, :], in1=rd2b.t[nr:nr + 64, :], op=ALU.mult))
                            k.add_r(accb, tok); k.add_r(rd2b, tok); k.set_w(ab_, tok, fresh=(hh == 0))
                        k.add_r(qb_, tok); k.add_r(kb_, tok); k.add_r(vb_, tok)
                        k.store('pool', ab_, seg.mix[(6 + hp) * 128:(7 + hp) * 128, q0:q0 + 512], ab_.t[:])
            if stop is not None and stop <= l * 10 + 3:
                continue
            with Phase(k) as ph:
                mb = ph.sb([128, 2, 2048], BF16, dma=True, name="mb")
                memT = ph.sb([128, 16, 256], BF16, name="memT")
                wkv = ph.sb([128, 8, 2048], BF16, dma=True, name="wkv")
                kmT = ph.sb([128, 4, 256], BF16, name="kmT")
                vm = ph.sb([128, 2, 512], BF16, name="vm")
                qmt = ph.ring(2, [128, 4, 512], BF16, dma=True, name="qmt")
                pe_t = ph.ring(3, [128, 512], BF16, name="pexp")
                rd = ph.ring(2, [128, 512], F32, name="rd")
                mo = ph.ring(2, [128, 512], BF16, dma=True, name="mo")
                tpm = ph.pring(2, [128, 4, 256], BF16, name="tpm")
                sp_ = ph.pring(2, [128, 512], F32, name="sps")
                np_ = ph.pring(2, [128, 512], F32, name="nps")
                dp_ = ph.pring(2, [128, 512], F32, name="dps")
                k.load('pool', mb, mb.t[:], seg.mem.rearrange("(t p) d -> p t d", p=128))
                for m in range(8):
                    k.load('sp', wkv, wkv.t[:, m, :], wkv_b[l][m], fresh=(m == 0))
                k.acq_r('pe', mb); k.acq_r('pe', wkv)
                for kg in range(4):
                    tp = tpm[kg % 2]
                    k.acq_w('pe', tp)
                    for kk in range(4):
                        for t in range(2):
                            ins = nc.tensor.transpose(tp.t[:, kk, t * 128:(t + 1) * 128], mb.t[:, t, (kg * 4 + kk) * 128:(kg * 4 + kk + 1) * 128], ident_b.t[:])
                    k.set_w(tp, k.sig('pe', ins))
                    k.acq_r('act', tp)
                    tok = k.sig('act', nc.scalar.activation(out=memT.t[:, kg * 4:(kg + 1) * 4, :], in_=tp.t[:], func=AF.Copy))
                    k.add_r(tp, tok); k.set_w(memT, tok, fresh=(kg == 0))
                k.acq_r('pe', memT)
                for mh in range(4):
                    pb = sp_[mh % 2]
                    k.acq_w('pe', pb)
                    for kk in range(16):
                        ins = nc.tensor.matmul(pb.t[:, 0:256], lhsT=wkv.t[:, mh, kk * 128:(kk + 1) * 128], rhs=memT.t[:, kk, :], start=(kk == 0), stop=(kk == 15))
                    k.set_w(pb, k.sig('pe', ins))
                    k.acq_r('act', pb)
                    tok = k.sig('act', nc.scalar.activation(out=kmT.t[:, mh, :], in_=pb.t[:, 0:256], func=AF.Copy))
                    k.add_r(pb, tok); k.set_w(kmT, tok, fresh=(mh == 0))
                i = 0
                for t in range(2):
                    for mh in range(4):
                        pb = np_[i % 2]; i += 1
                        k.acq_w('pe', pb)
                        for kk in range(16):
                            ins = nc.tensor.matmul(pb.t[:, 0:128], lhsT=memT.t[:, kk, t * 128:(t + 1) * 128], rhs=wkv.t[:, 4 + mh, kk * 128:(kk + 1) * 128], start=(kk == 0), stop=(kk == 15))
                        k.set_w(pb, k.sig('pe', ins))
                        k.acq_r('dve', pb)
                        tok = k.sig('dve', nc.vector.tensor_copy(out=vm.t[:, t, mh * 128:(mh + 1) * 128], in_=pb.t[:, 0:128]))
                        k.add_r(pb, tok); k.set_w(vm, tok, fresh=(i == 1))
                k.acq_r('pe', kmT); k.acq_r('pe', vm)
                nqb = (oe - os_) // 512
                sc = 0; it = 0
                for qb in range(nqb):
                    q0 = os_ + qb * 512
                    qb_ = qmt[qb % 2]
                    k.load('sp', qb_, qb_.t[:], seg.qm[:, q0:q0 + 512].rearrange("(h p) t -> p h t", p=128))
                    k.acq_r('pe', qb_)
                    for mh in range(4):
                        nb_ = np_[it % 2]; db_ = dp_[it % 2]; rdb = rd[it % 2]; ob = mo[it % 2]; it += 1
                        k.acq_w('pe', nb_); k.acq_w('pe', db_)
                        for t in range(2):
                            sb_ = sp_[sc % 2]; peb = pe_t[sc % 3]; sc += 1
                            k.acq_w('pe', sb_)
                            tok = k.sig('pe', nc.tensor.matmul(sb_.t[:], lhsT=kmT.t[:, mh, t * 128:(t + 1) * 128], rhs=qb_.t[:, mh, :], start=True, stop=True))
                            k.set_w(sb_, tok)
                            k.acq_r('act', sb_); k.acq_w('act', peb)
                            tok = k.sig('act', nc.scalar.activation(out=peb.t[:], in_=sb_.t[:], func=AF.Exp, scale=float(128 ** -0.5)))
                            k.add_r(sb_, tok); k.set_w(peb, tok)
                            k.acq_r('pe', peb)
                            nc.tensor.matmul(nb_.t[:], lhsT=vm.t[:, t, mh * 128:(mh + 1) * 128], rhs=peb.t[:], start=(t == 0), stop=(t == 1))
                            tok = k.sig('pe', nc.tensor.matmul(db_.t[:], lhsT=ones_b.t[:], rhs=peb.t[:], start=(t == 0), stop=(t == 1)))
                            k.add_r(peb, tok)
                        k.set_w(nb_, tok); k.set_w(db_, tok)
                        k.acq_r('dve', db_); k.acq_w('dve', rdb)
                        k.wait('dve', k.sig('dve', nc.vector.reciprocal(out=rdb.t[:], in_=db_.t[:])), force=True)
                        k.acq_w('dve', ob)
                        tok = k.sig('dve', nc.vector.tensor_tensor(out=ob.t[:], in0=nb_.t[:], in1=rdb.t[:], op=ALU.mult))
                        k.add_r(nb_, tok); k.add_r(db_, tok); k.set_w(ob, tok)
                        k.store('pool', ob, seg.mix[(12 + mh) * 128:(13 + mh) * 128, q0:q0 + 512], ob.t[:])
                    k.add_r(qb_, tok)
            if stop is not None and stop <= l * 10 + 4:
                continue
            with Phase(k) as ph:
                wo = ph.sb([128, 16, 2048], BF16, dma=True, name="wo")
                gb = ph.sb([128, 2, 2048], F32, dma=True, name="gb")
                mt = ph.ring(2, [128, 16, 512], BF16, dma=True, name="mixT")
                xr = ph.ring(2, [128, 2048], F32, dma=True, name="xr")
                yt = ph.ring(2, [128, 2048], F32, dma=True, name="yt")
                st = ph.sb([128, 4, 6], F32); mv = ph.sb([128, 2], F32); rs = ph.sb([128, 1], F32); nb = ph.sb([128, 1], F32)
                ops = ph.pring(8, [128, 512], F32, name="ops")
                k.load('sp', wo, wo.t[:], wout_b[l].rearrange("(k p) n -> p k n", p=128))
                k.load('sp', gb, gb.t[:, 0, :], ln1_g[l].partition_broadcast(128))
                k.load('sp', gb, gb.t[:, 1, :], ln1_b[l].partition_broadcast(128), fresh=False)
                k.acq_r('pe', wo); k.acq_r('dve', gb)
                nblk = (oe - os_) // 512
                oc = 0; ti = 0
                for b in range(nblk):
                    t0 = os_ + b * 512
                    mb_ = mt[b % 2]
                    k.load('sp', mb_, mb_.t[:], seg.mix[:, t0:t0 + 512].rearrange("(k p) t -> p k t", p=128))
                    k.acq_r('pe', mb_)
                    for t in range(4):
                        xb_ = xr[ti % 2]; yb = yt[ti % 2]; ti += 1
                        r0 = t0 + t * 128
                        k.load('sp', xb_, xb_.t[:], xin[r0:r0 + 128, :])
                        pbs = []
                        for ch in range(4):
                            pb = ops[oc % 8]; oc += 1
                            k.acq_w('pe', pb)
                            for kk in range(16):
                                ins = nc.tensor.matmul(pb.t[:], lhsT=mb_.t[:, kk, t * 128:(t + 1) * 128], rhs=wo.t[:, kk, ch * 512:(ch + 1) * 512], start=(kk == 0), stop=(kk == 15))
                            tokp = k.sig('pe', ins)
                            k.set_w(pb, tokp)
                            pbs.append(pb)
                        k.acq_r('dve', xb_); k.acq_w('dve', yb)
                        for ch in range(4):
                            k.acq_r('dve', pbs[ch])
                            tok = k.sig('dve', nc.vector.scalar_tensor_tensor(out=yb.t[:, ch * 512:(ch + 1) * 512], in0=xb_.t[:, ch * 512:(ch + 1) * 512], scalar=float(ALPHA), in1=pbs[ch].t[:], op0=ALU.mult, op1=ALU.add))
                            k.add_r(pbs[ch], tok)
                        k.add_r(xb_, tok)
                        tok = ln_inplace(yb.t[:], gb, (st, mv, rs, nb))
                        k.set_w(yb, tok)
                        k.store('pool', yb, seg.x1[r0:r0 + 128, :], yb.t[:])
                    k.add_r(mb_, tokp)
            if stop is not None and stop <= l * 10 + 5:
                continue
            is_moe = (l % 2 == 1)
            if is_moe:
                with Phase(k) as ph:
                    rt = ph.sb([128, 16, NE], F32, dma=True, name="rt")
                    xr = ph.ring(2, [128, 2048], F32, dma=True, name="xr")
                    xT32 = ph.ring(2, [128, 16, 128], F32, name="xT32")
                    lg = ph.ring(2, [128, NE], F32, name="lg")
                    m8 = ph.sb([128, 8], F32); nv1 = ph.sb([128, 1], F32); ex = ph.sb([128, 8], F32); msk = ph.sb([128, 8], F32)
                    den = ph.sb([128, 1], F32); rden = ph.sb([128, 1], F32)
                    cb = ph.ring(2, [128, NE], F32, dma=True, name="cb")
                    tps = ph.pring(4, [128, 4, 128], F32, name="tps")
                    lps = ph.pring(2, [128, NE], F32, name="lps")
                    k.load('sp', rt, rt.t[:], router[0].rearrange("(k p) e -> p k e", p=128))
                    k.acq_r('pe', rt)
                    ntile = (oe - os_) // 128
                    tc_ = 0
                    for ti in range(ntile):
                        r0 = os_ + ti * 128
                        xb_ = xr[ti % 2]; xtb = xT32[ti % 2]; lgb = lg[ti % 2]; cbb = cb[ti % 2]; lp = lps[ti % 2]
                        k.load('sp', xb_, xb_.t[:], seg.x1[r0:r0 + 128, :])
                        k.acq_r('pe', xb_)
                        for kg in range(4):
                            tp = tps[tc_ % 4]; tc_ += 1
                            k.acq_w('pe', tp)
                            for kk in range(4):
                                ins = nc.tensor.transpose(tp.t[:, kk, :], xb_.t[:, (kg * 4 + kk) * 128:(kg * 4 + kk + 1) * 128], ident_f.t[:])
                            tokp = k.sig('pe', ins)
                            k.set_w(tp, tokp)
                            e = 'act' if kg % 2 == 0 else 'dve'
                            k.acq_r(e, tp); k.acq_w(e, xtb)
                            if e == 'act':
                                ins = nc.scalar.activation(out=xtb.t[:, kg * 4:(kg + 1) * 4, :], in_=tp.t[:], func=AF.Copy)
                            else:
                                ins = nc.vector.tensor_copy(out=xtb.t[:, kg * 4:(kg + 1) * 4, :], in_=tp.t[:])
                            tok = k.sig(e, ins)
                            k.add_r(tp, tok); k.set_w(xtb, tok, fresh=(kg == 0))
                        k.add_r(xb_, tokp)
                        k.acq_r('pe', xtb); k.acq_w('pe', lp)
                        for kk in range(16):
                            ins = nc.tensor.matmul(lp.t[:], lhsT=xtb.t[:, kk, :], rhs=rt.t[:, kk, :], start=(kk == 0), stop=(kk == 15))
                        tokp = k.sig('pe', ins)
                        k.set_w(lp, tokp); k.add_r(xtb, tokp)
                        k.acq_r('dve', lp); k.acq_w('dve', lgb); k.acq_w('dve', cbb)
                        k.wait('dve', k.sig('dve', nc.vector.tensor_copy(out=lgb.t[:], in_=lp.t[:])), force=True)
                        tok = k.sig('dve', nc.vector.max(out=m8.t[:], in_=lgb.t[:]))
                        k.wait('dve', tok, force=True)
                        nc.vector.tensor_scalar(out=msk.t[:], in0=lgb.t[:], scalar1=m8.t[:, 1:2], scalar2=None, op0=ALU.is_ge)
                        tok = k.sig('dve', nc.vector.tensor_scalar(out=nv1.t[:], in0=m8.t[:, 0:1], scalar1=-1.0, scalar2=None, op0=ALU.mult))
                        k.add_r(lp, tok)
                        k.wait('act', tok)
                        toka = k.sig('act', nc.scalar.activation(out=ex.t[:], in_=lgb.t[:], func=AF.Exp, bias=nv1.t[:, 0:1], scale=1.0))
                        k.wait('dve', toka)
                        k.wait('dve', k.sig('dve', nc.vector.tensor_tensor(out=ex.t[:], in0=ex.t[:], in1=msk.t[:], op=ALU.mult)), force=True)
                        k.wait('dve', k.sig('dve', nc.vector.tensor_reduce(out=den.t[:], in_=ex.t[:], axis=mybir.AxisListType.X, op=ALU.add)), force=True)
                        tok = k.sig('dve', nc.vector.reciprocal(out=rden.t[:], in_=den.t[:]))
                        k.wait('dve', tok, force=True)
                        tok = k.sig('dve', nc.vector.tensor_scalar(out=cbb.t[:], in0=ex.t[:], scalar1=rden.t[:, 0:1], scalar2=None, op0=ALU.mult))
                        k.set_w(cbb, tok); k.set_w(lgb, tok)
                        k.store('pool', cbb, seg.comb[r0:r0 + 128, :], cbb.t[:])
            last = (l == DEPTH - 1)
            if is_moe:
                continue
            while bgq and bgq[0][2] is wsemF:
                bg(1)
            for q in ('sp', 'pool'):
                k.wait(q, bgtok.get(wsemF.name))
            with Phase(k) as ph:
                nexp = NE if is_moe else 1
                NF = (E_FF if is_moe else D_FF) // 128
                FG = 4
                xb = ph.sb([128, 4, 2048], BF16, dma=True, name="xb")
                xT = ph.sb([128, 16, 512], BF16, name="xT")
                gT = ph.sb([128, NF, 512], BF16, name="gT")
                accs = ph.sb([128, 4, 2048], F32, dma=True, name="acc")
                w13 = ph.ring(3, [128, 2, 2048], BF16, dma=True, name="w13")
                w2r = ph.ring(4, [128, FG, 512], BF16, dma=True, name="w2r")
                xr = ph.sb([128, 2048], F32, dma=True, name="xr")
                gb = ph.sb([128, 2, 2048], F32, dma=True, name="gb")
                stt = ph.ring(2, [128, 512], F32, name="silu")
                cbt = ph.sb([128, 4, NE], F32, dma=True, name="cbt")
                vt = ph.sb([128, W // 128], F32, dma=True, name="vt")
                st = ph.sb([128, 4, 6], F32); mv = ph.sb([128, 2], F32); rs = ph.sb([128, 1], F32); nb = ph.sb([128, 1], F32)
                tpr = ph.pring(1, [128, 4, 512], BF16, name="tp")
                hps = ph.pring(2, [128, 512], F32, name="hps")
                ops = ph.pring(4, [128, 512], F32, name="ops")
                k.load('sp', vt, vt.t[:], seg.valid[:, :])
                k.load('sp', gb, gb.t[:, 0, :], (ln2_g if True else ln1_g)[l].partition_broadcast(128))
                k.load('sp', gb, gb.t[:, 1, :], ln2_b[l].partition_broadcast(128), fresh=False)
                k.acq_r('dve', gb); k.acq_r('dve', vt)
                nblk = (oe - os_) // 512
                cnt = [0]
                wc = 0; w2c = 0; hc = 0
                for b in range(nblk):
                    t0 = os_ + b * 512
                    load_xT(ph, seg.x1[t0:t0 + 512, :], xb, xT, tpr, cnt)
                    if is_moe:
                        k.load('sp', cbt, cbt.t[:], seg.comb[t0:t0 + 512, :].rearrange("(t p) e -> p t e", p=128))
                        k.acq_r('dve', cbt)
                    for e in range(nexp):
                        w1s = f1_b; w3s = f3_b; w2s = f2_b
                        for f in range(NF):
                            bg(1)
                            wb = w13[wc % 3]; wc += 1
                            k.load('sp', wb, wb.t[:, 0, :], w1s[f])
                            k.load('sp', wb, wb.t[:, 1, :], w3s[f], fresh=False)
                            k.acq_r('pe', wb); k.acq_r('pe', xT)
                            h1 = hps[0]; h3 = hps[1]
                            k.acq_w('pe', h1)
                            for kk in range(16):
                                ins = nc.tensor.matmul(h1.t[:], lhsT=wb.t[:, 0, kk * 128:(kk + 1) * 128], rhs=xT.t[:, kk, :], start=(kk == 0), stop=(kk == 15))
                            k.set_w(h1, k.sig('pe', ins))
                            k.acq_w('pe', h3)
                            for kk in range(16):
                                ins = nc.tensor.matmul(h3.t[:], lhsT=wb.t[:, 1, kk * 128:(kk + 1) * 128], rhs=xT.t[:, kk, :], start=(kk == 0), stop=(kk == 15))
                            tokp = k.sig('pe', ins)
                            k.set_w(h3, tokp); k.add_r(wb, tokp)
                            sb_ = stt[hc % 2]; hc += 1
                            k.acq_r('act', h1); k.acq_w('act', sb_)
                            tok = k.sig('act', nc.scalar.activation(out=sb_.t[:], in_=h1.t[:], func=AF.Silu))
                            k.add_r(h1, tok); k.set_w(sb_, tok)
                            k.acq_r('dve', sb_); k.acq_r('dve', h3)
                            if f == 0:
                                k.acq_w('dve', gT)
                            tok = k.sig('dve', nc.vector.tensor_tensor(out=gT.t[:, f, :], in0=sb_.t[:], in1=h3.t[:], op=ALU.mult))
                            k.add_r(sb_, tok); k.add_r(h3, tok); k.set_w(gT, tok, fresh=(f == 0))
                        if e == nexp - 1:
                            k.add_r(xT, tokp)
                        k.acq_r('pe', gT)
                        for dch in range(4):
                            for t in range(4):
                                k.acq_w('pe', ops[t])
                            for fg in range(NF // FG):
                                wb = w2r[w2c % 4]; w2c += 1
                                k.load('sp', wb, wb.t[:], w2s[fg * FG * 128:(fg + 1) * FG * 128, dch * 512:(dch + 1) * 512].rearrange("(f p) d -> p f d", p=128))
                                k.acq_r('pe', wb)
                                for fi in range(FG):
                                    f = fg * FG + fi
                                    for t in range(4):
                                        ins = nc.tensor.matmul(ops[t].t[:], lhsT=gT.t[:, f, t * 128:(t + 1) * 128], rhs=wb.t[:, fi, :], start=(f == 0), stop=(f == NF - 1))
                                tokp = k.sig('pe', ins)
                                k.add_r(wb, tokp)
                            for t in range(4):
                                k.set_w(ops[t], tokp)
                            if e == 0 and dch == 0:
                                k.acq_w('dve', accs)
                            for t in range(4):
                                k.acq_r('dve', ops[t])
                                dst = accs.t[:, t, dch * 512:(dch + 1) * 512]
                                if not is_moe:
                                    ins = nc.vector.tensor_copy(out=dst, in_=ops[t].t[:])
                                elif e == 0:
                                    ins = nc.vector.tensor_scalar(out=dst, in0=ops[t].t[:], scalar1=cbt.t[:, t, e:e + 1], scalar2=None, op0=ALU.mult)
                                else:
                                    ins = nc.vector.scalar_tensor_tensor(out=dst, in0=ops[t].t[:], scalar=cbt.t[:, t, e:e + 1], in1=dst, op0=ALU.mult, op1=ALU.add)
                                tok = k.sig('dve', ins)
                                k.add_r(ops[t], tok)
                            k.set_w(accs, tok, fresh=(e == 0 and dch == 0))
                        k.add_r(gT, tokp)
                    if is_moe:
                        k.add_r(cbt, tok)
                    for t in range(4):
                        r0 = t0 + t * 128
                        k.load('sp', xr, xr.t[:], seg.x1[r0:r0 + 128, :])
                        k.acq_r('dve', xr)
                        yt = accs.t[:, t, :]
                        tok = k.sig('dve', nc.vector.scalar_tensor_tensor(out=yt, in0=xr.t[:], scalar=float(ALPHA), in1=yt, op0=ALU.mult, op1=ALU.add))
                        k.add_r(xr, tok)
                        va = None if last else vt.t[:, r0 // 128:r0 // 128 + 1]
                        tok = ln_inplace(yt, gb, (st, mv, rs, nb), valid_ap=va)
                        k.set_w(accs, tok, fresh=False)
                        if last:
                            if os_ <= r0 < oe:
                                k.store('pool', accs, seg.yout[r0 - seg.yoff:r0 - seg.yoff + 128, :], yt)
                        else:
                            k.store('pool', accs, seg.x2[r0:r0 + 128, :], yt)

        if l % 2 == 1 and (stop is None or stop > l * 10 + 5):
            I32 = mybir.dt.int32
            AX = mybir.AxisListType.X
            tiles = []
            for seg in segs:
                o0, o1 = seg.orr[l]
                for r0 in range(o0, o1, 128):
                    tiles.append((seg, r0))
            NT = len(tiles)
            NB = NT // 2 + NE
            NROW = NB * 512
            xsort = dscr(f"xsort{l}", [NROW + 128, D], BF16)
            ysort = dscr(f"ysort{l}", [NROW + 128, D], F32)
            outer = ExitStack()

            def osb(shape, dt, name):
                k.uid += 1
                return Buf(outer.enter_context(nc.sbuf_tensor(f"{name}{k.uid}", list(shape), dt)))

            IDX = osb([128, NT * 2], I32, "IDX")
            G12 = osb([128, NT, 2], F32, "G12")
            WI13 = osb([128, NB], I32, "WI13")
            WI2 = osb([128, NB * 4], I32, "WI2")

            def fw(ins):
                tok = k.sig('dve', ins)
                k.wait('dve', tok, force=True)
                return tok

            with Phase(k) as ph:
                U = ph.sb([128, 128], F32, dma=True, name="U")
                iot = ph.sb([128, 1], F32, dma=True, name="iot")
                CB = ph.sb([128, NT, 8], F32, dma=True, name="CB")
                Mt = ph.sb([128, NT, 8], F32, name="Mt")
                RK = ph.sb([128, NT, 8], F32, name="RK")
                carry = ph.sb([128, 8], F32); cmpn = ph.sb([128, 8, 12], F32); pe_ = ph.sb([128, 8], F32)
                o1_ = ph.sb([128, 8], F32); end_ = ph.sb([128, 8], F32)
                cmpE = ph.sb([128, NB, 8], F32); EJ = ph.sb([128, NB], F32); wf = ph.sb([128, NB], F32)
                key = ph.sb([128, 8], F32); m8 = ph.sb([128, 8], F32); eq = ph.sb([128, 8], F32)
                rps = ph.pring(2, [128, 8], F32, name="rps"); cps = ph.pring(2, [128, 8], F32, name="cps")
                k.load('sp', U, U.t[:], triud[:, :])
                k.load('sp', iot, iot.t[:], iotad[:, :])
                i = 0
                for seg in segs:
                    o0, o1 = seg.orr[l]
                    n = (o1 - o0) // 128
                    k.load('sp', CB, CB.t[:, i:i + n, :], seg.comb[o0:o1, :].rearrange("(t p) e -> p t e", p=128), fresh=(i == 0))
                    i += n
                k.acq_r('dve', CB); k.acq_r('dve', iot); k.acq_r('pe', U)
                tokM = fw(nc.vector.tensor_scalar(out=Mt.t[:], in0=CB.t[:], scalar1=0.0, scalar2=None, op0=ALU.is_gt))
                k.wait('pe', tokM)
                fw(nc.vector.memset(carry.t[:], 0.0))
                for i in range(NT):
                    rp = rps[i % 2]; cp = cps[i % 2]
                    k.acq_w('pe', rp); k.acq_w('pe', cp)
                    nc.tensor.matmul(rp.t[:], lhsT=U.t[:], rhs=Mt.t[:, i, :], start=True, stop=True)
                    tok = k.sig('pe', nc.tensor.matmul(cp.t[:], lhsT=ones_f.t[:], rhs=Mt.t[:, i, :], start=True, stop=True))
                    k.set_w(rp, tok); k.set_w(cp, tok)
                    k.acq_r('dve', rp)
                    nc.vector.tensor_tensor(out=RK.t[:, i, :], in0=rp.t[:], in1=carry.t[:], op=ALU.add)
                    tok = fw(nc.vector.tensor_tensor(out=carry.t[:], in0=cp.t[:], in1=carry.t[:], op=ALU.add))
                    k.add_r(rp, tok); k.add_r(cp, tok)
                for j in range(12):
                    ins = nc.vector.tensor_scalar(out=cmpn.t[:, :, j], in0=carry.t[:], scalar1=float(512 * j), scalar2=None, op0=ALU.is_gt)
                fw(ins)
                fw(nc.vector.tensor_reduce(out=pe_.t[:], in_=cmpn.t[:], axis=AX, op=ALU.add))
                fw(nc.vector.tensor_scalar(out=pe_.t[:], in0=pe_.t[:], scalar1=512.0, scalar2=None, op0=ALU.mult))
                fw(nc.vector.tensor_copy(out=end_.t[:, 0:1], in_=pe_.t[:, 0:1]))
                for e in range(1, NE):
                    fw(nc.vector.tensor_tensor(out=end_.t[:, e:e + 1], in0=end_.t[:, e - 1:e], in1=pe_.t[:, e:e + 1], op=ALU.add))
                fw(nc.vector.tensor_tensor(out=o1_.t[:], in0=end_.t[:], in1=pe_.t[:], op=ALU.subtract))
                fw(nc.vector.tensor_scalar(out=o1_.t[:], in0=o1_.t[:], scalar1=1.0, scalar2=None, op0=ALU.add))
                for j in range(NB):
                    ins = nc.vector.tensor_scalar(out=cmpE.t[:, j, :], in0=end_.t[:], scalar1=float(512 * j), scalar2=None, op0=ALU.is_le)
                fw(ins)
                fw(nc.vector.tensor_reduce(out=EJ.t[:], in_=cmpE.t[:], axis=AX, op=ALU.add))
                fw(nc.vector.tensor_scalar(out=EJ.t[:], in0=EJ.t[:], scalar1=float(NE - 1), scalar2=None, op0=ALU.min))
                fw(nc.vector.tensor_scalar(out=wf.t[:], in0=EJ.t[:], scalar1=float(NFH * 128), scalar2=iot.t[:, 0:1], op0=ALU.mult, op1=ALU.add))
                fw(nc.vector.tensor_copy(out=WI13.t[:], in_=wf.t[:]))
                for dch in range(4):
                    fw(nc.vector.tensor_scalar(out=wf.t[:], in0=EJ.t[:], scalar1=float(4 * NFG * 128), scalar2=float(dch * NFG * 128), op0=ALU.mult, op1=ALU.add))
                    fw(nc.vector.tensor_scalar(out=wf.t[:], in0=wf.t[:], scalar1=iot.t[:, 0:1], scalar2=None, op0=ALU.add))
                    fw(nc.vector.tensor_copy(out=WI2.t[:].rearrange("p (b d) -> p b d", d=4)[:, :, dch], in_=wf.t[:]))
                for i in range(NT):
                    fw(nc.vector.tensor_tensor(out=key.t[:], in0=RK.t[:, i, :], in1=o1_.t[:], op=ALU.add))
                    fw(nc.vector.tensor_tensor(out=key.t[:], in0=key.t[:], in1=Mt.t[:, i, :], op=ALU.mult))
                    fw(nc.vector.tensor_scalar(out=key.t[:], in0=key.t[:], scalar1=-1.0, scalar2=None, op0=ALU.add))
                    fw(nc.vector.max(out=m8.t[:], in_=key.t[:]))
                    fw(nc.vector.tensor_scalar(out=eq.t[:, 0:2], in0=m8.t[:, 0:2], scalar1=0.0, scalar2=float(NROW + 1), op0=ALU.is_lt, op1=ALU.mult))
                    fw(nc.vector.tensor_tensor(out=eq.t[:, 0:2], in0=eq.t[:, 0:2], in1=m8.t[:, 0:2], op=ALU.add))
                    fw(nc.vector.tensor_copy(out=IDX.t[:, 2 * i:2 * i + 2], in_=eq.t[:, 0:2]))
                    for c in range(2):
                        fw(nc.vector.tensor_scalar(out=eq.t[:], in0=key.t[:], scalar1=m8.t[:, c:c + 1], scalar2=None, op0=ALU.is_equal))
                        fw(nc.vector.tensor_tensor(out=eq.t[:], in0=eq.t[:], in1=CB.t[:, i, :], op=ALU.mult))
                        fw(nc.vector.tensor_reduce(out=G12.t[:, i, c:c + 1], in_=eq.t[:], axis=AX, op=ALU.add))
            with Phase(k) as ph:
                xr = ph.ring(3, [128, 2048], BF16, dma=True, name="xsc")
                for i, (seg, r0) in enumerate(tiles):
                    b = xr[i % 3]
                    k.load('pool', b, b.t[:], seg.x1[r0:r0 + 128, :])
                    k.acq_r('pool', b)
                    for c in range(2):
                        ins = nc.gpsimd.indirect_dma_start(out=xsort[:, :], out_offset=bass.IndirectOffsetOnAxis(ap=IDX.t[:, 2 * i + c:2 * i + c + 1], axis=0),
                                                           in_=b.t[:], in_offset=None)
                        ins.then_inc(b.sem.h, 16)
                        b.sem.n += 16
                        tok = (b.sem, b.sem.n, None)
                        k.add_r(b, tok)
                        k.pending.append(('pool', tok))
            bg(len(bgq))
            for q in ('sp', 'pool'):
                k.wait(q, bgtok.get(wsemB.name))
            with Phase(k) as ph:
                xbs = ph.ring(2, [128, 4, 2048], BF16, dma=True, name="xb")
                xTs = ph.ring(2, [128, 16, 512], BF16, name="xT")
                gT = ph.sb([128, NFE, 512], BF16, name="gT")
                w13 = ph.ring(3, [128, 4096], BF16, dma=True, name="w13")
                w2r = ph.ring(4, [128, FG, 512], BF16, dma=True, name="w2r")
                yst = ph.ring(2, [128, 4, 512], F32, dma=True, name="yst")
                stt = ph.ring(2, [128, 512], F32, name="silu")
                tpr = ph.pring(1, [128, 4, 512], BF16, name="tp")
                hps = ph.pring(2, [128, 512], F32, name="hps")
                ops = ph.pring(4, [128, 512], F32, name="ops")
                cnt = [0]; wc = 0; w2c = 0; hc = 0; yc = 0
                load_xT(ph, xsort[0:512, :], xbs[0], xTs[0], tpr, cnt)
                for j in range(NB):
                    xT = xTs[j % 2]
                    if j + 1 < NB:
                        xload(xsort[(j + 1) * 512:(j + 2) * 512, :], xbs[(j + 1) % 2])
                    for f in range(NFE):
                        wb = w13[wc % 3]; wc += 1
                        k.iload(wb, wb.t[:], m13_h[f // NFH][:, :], WI13.t[:, j:j + 1], elem_off=(f % NFH) * 128 * 4096)
                        k.acq_r('pe', wb); k.acq_r('pe', xT)
                        h1 = hps[0]; h3 = hps[1]
                        k.acq_w('pe', h1)
                        for kk in range(16):
                            ins = nc.tensor.matmul(h1.t[:], lhsT=wb.t[:, kk * 128:(kk + 1) * 128], rhs=xT.t[:, kk, :], start=(kk == 0), stop=(kk == 15))
                        k.set_w(h1, k.sig('pe', ins))
                        k.acq_w('pe', h3)
                        for kk in range(16):
                            ins = nc.tensor.matmul(h3.t[:], lhsT=wb.t[:, 2048 + kk * 128:2048 + (kk + 1) * 128], rhs=xT.t[:, kk, :], start=(kk == 0), stop=(kk == 15))
                        tokp = k.sig('pe', ins)
                        k.set_w(h3, tokp); k.add_r(wb, tokp)
                        sb_ = stt[hc % 2]; hc += 1
                        k.acq_r('act', h1); k.acq_w('act', sb_)
                        tok = k.sig('act', nc.scalar.activation(out=sb_.t[:], in_=h1.t[:], func=AF.Silu))
                        k.add_r(h1, tok); k.set_w(sb_, tok)
                        k.acq_r('dve', sb_); k.acq_r('dve', h3)
                        if f == 0:
                            k.acq_w('dve', gT)
                        tok = k.sig('dve', nc.vector.tensor_tensor(out=gT.t[:, f, :], in0=sb_.t[:], in1=h3.t[:], op=ALU.mult))
                        k.add_r(sb_, tok); k.add_r(h3, tok); k.set_w(gT, tok, fresh=(f == 0))
                    k.add_r(xT, tokp)
                    if j + 1 < NB:
                        xtrans(xbs[(j + 1) % 2], xTs[(j + 1) % 2], tpr, cnt)
                    k.acq_r('pe', gT)
                    for dch in range(4):
                        for t in range(4):
                            k.acq_w('pe', ops[t])
                        for fg in range(NFG):
                            wb = w2r[w2c % 4]; w2c += 1
                            k.iload(wb, wb.t[:].rearrange("p f c -> p (f c)"), m2_b[:, :], WI2.t[:, 4 * j + dch:4 * j + dch + 1], elem_off=fg * 128 * 2048)
                            k.acq_r('pe', wb)
                            for fi in range(FG):
                                f = fg * FG + fi
                                for t in range(4):
                                    ins = nc.tensor.matmul(ops[t].t[:], lhsT=gT.t[:, f, t * 128:(t + 1) * 128], rhs=wb.t[:, fi, :], start=(f == 0), stop=(f == NFE - 1))
                            tokp = k.sig('pe', ins)
                            k.add_r(wb, tokp)
                        ys = yst[yc % 2]; yc += 1
                        for t in range(4):
                            k.set_w(ops[t], tokp)
                        for t in range(4):
                            e_ = 'act' if t % 2 == 0 else 'dve'
                            k.acq_r(e_, ops[t]); k.acq_w(e_, ys)
                            if e_ == 'act':
                                ins = nc.scalar.activation(out=ys.t[:, t, :], in_=ops[t].t[:], func=AF.Copy)
                            else:
                                ins = nc.vector.tensor_copy(out=ys.t[:, t, :], in_=ops[t].t[:])
                            tok = k.sig(e_, ins)
                            k.add_r(ops[t], tok); k.set_w(ys, tok, fresh=(t == 0))
                        k.store('sp', ys, ysort[j * 512:(j + 1) * 512, dch * 512:(dch + 1) * 512].rearrange("(t p) c -> p t c", p=128), ys.t[:])
                    k.add_r(gT, tokp)
            with Phase(k) as ph:
                gb = ph.sb([128, 2, 2048], F32, dma=True, name="gb")
                Y1 = ph.ring(2, [128, 2048], F32, dma=True, name="Y1")
                Y2 = ph.ring(2, [128, 2048], F32, dma=True, name="Y2")
                xr = ph.ring(2, [128, 2048], F32, dma=True, name="xr")
                st = ph.sb([128, 4, 6], F32); mv = ph.sb([128, 2], F32); rs = ph.sb([128, 1], F32); nb = ph.sb([128, 1], F32)
                k.load('sp', gb, gb.t[:, 0, :], ln2_g[l].partition_broadcast(128))
                k.load('sp', gb, gb.t[:, 1, :], ln2_b[l].partition_broadcast(128), fresh=False)
                k.acq_r('dve', gb)
                zt_ = Y1[0]
                k.set_w(zt_, k.sig('dve', nc.vector.memset(zt_.t[:], 0.0)))
                ztok = k.store('sp', zt_, ysort[NROW:NROW + 128, :], zt_.t[:])
                k.wait('pool', ztok)
                for i, (seg, r0) in enumerate(tiles):
                    y1 = Y1[i % 2]; y2 = Y2[i % 2]; xb_ = xr[i % 2]
                    for yb, c in ((y1, 0), (y2, 1)):
                        k.iload(yb, yb.t[:], ysort[:, :], IDX.t[:, 2 * i + c:2 * i + c + 1])
                    k.load('sp', xb_, xb_.t[:], seg.x1[r0:r0 + 128, :])
                    k.acq_r('dve', y1); k.acq_r('dve', y2); k.acq_r('dve', xb_)
                    nc.vector.tensor_scalar(out=y1.t[:], in0=y1.t[:], scalar1=G12.t[:, i, 0:1], scalar2=None, op0=ALU.mult)
                    nc.vector.scalar_tensor_tensor(out=y1.t[:], in0=y2.t[:], scalar=G12.t[:, i, 1:2], in1=y1.t[:], op0=ALU.mult, op1=ALU.add)
                    tok = k.sig('dve', nc.vector.scalar_tensor_tensor(out=y1.t[:], in0=xb_.t[:], scalar=float(ALPHA), in1=y1.t[:], op0=ALU.mult, op1=ALU.add))
                    k.add_r(y2, tok); k.add_r(xb_, tok)
                    tok = ln_inplace(y1.t[:], gb, (st, mv, rs, nb))
                    k.set_w(y1, tok, fresh=False)
                    k.store('sp', y1, seg.yout[r0 - seg.yoff:r0 - seg.yoff + 128, :], y1.t[:])
            outer.close()
    return nc


def _host_prep(inputs):
    f32 = np.float32
    w_in = np.asarray(inputs["w_in"], f32)
    L = w_in.shape[0]
    idx = np.arange(64)
    perm = idx.copy()
    perm[0:8] = idx[8:16]
    perm[8:16] = idx[0:8]
    qcols = 1536 + (np.arange(12)[:, None] * 64 + perm[None, :]).reshape(-1)
    kcols = 2304 + (np.arange(12)[:, None] * 64 + perm[None, :]).reshape(-1)
    w_in_ext = np.concatenate([w_in, w_in[:, :, qcols], w_in[:, :, kcols]], axis=-1)
    cpar = np.zeros((L, 128, 6, 34), f32)
    for l in range(L):
        cpar[l, :, :, 0:31] = np.asarray(inputs["conv_w"], f32)[l].T.reshape(6, 128, 31).transpose(1, 0, 2)
        cpar[l, :, :, 31] = np.asarray(inputs["conv_b"], f32)[l].reshape(6, 128).T
        cpar[l, :, :, 32] = np.asarray(inputs["conv_ln_g"], f32)[l].reshape(6, 128).T
        cpar[l, :, :, 33] = np.asarray(inputs["conv_ln_b"], f32)[l].reshape(6, 128).T
    shared = {
        "w_in_ext": np.ascontiguousarray(w_in_ext), "cpar": cpar,
        "maskd": _mult_mask(), "identd": np.eye(128, dtype=f32),
        "triud": np.triu(np.ones((128, 128), f32), 1), "iotad": np.arange(128, dtype=f32).reshape(128, 1),
        "cs_p": _rope_tables(np.arange(PW)), "valid_p": np.ones((128, PW // 128), f32),
    }
    for n in ("w_mem_kv", "w_out", "ln1_g", "ln1_b", "ln2_g", "ln2_b", "ffn_w1", "ffn_w3", "ffn_w2",
              "moe_router", "moe_w1", "moe_w3", "moe_w2"):
        shared[n] = np.ascontiguousarray(np.asarray(inputs[n], f32))
    xp = np.asarray(inputs["x_prompt"], f32)
    xs = np.asarray(inputs["x_sample"], f32)
    mp = np.asarray(inputs["mem_prompt"], f32)
    ms = np.asarray(inputs["mem_sample"], f32)
    in_maps = []
    for c in range(NCORE):
        sq, j = c // 4, c % 4
        a = j * 4096
        lo = a - 2048
        pos = np.arange(lo, lo + SW)
        ok = (pos >= 0) & (pos < 16384)
        xw = np.zeros((SW, D), f32)
        xw[ok] = xs[sq, pos[ok]]
        m = dict(shared)
        m["xp"] = np.ascontiguousarray(xp[c])
        m["xs"] = xw
        m["memp"] = np.ascontiguousarray(mp[c])
        m["mems"] = np.ascontiguousarray(ms[sq])
        m["valid_s"] = np.ascontiguousarray(ok.astype(f32).reshape(SW // 128, 128).T)
        m["cs_s"] = _rope_tables(np.where(ok, pos, 0))
        in_maps.append(m)
    return in_maps


def kernel(**inputs):
    in_maps = _host_prep(inputs)
    nc = build()
    res = run_bass_kernel_spmd(nc, in_maps, core_ids=list(range(NCORE)))
    yp = np.stack([res.results[c]["yp"] for c in range(NCORE)], axis=0)
    ysf = np.zeros((2, 16384, D), np.float32)
    for c in range(NCORE):
        sq, j = c // 4, c % 4
        ysf[sq, j * 4096:(j + 1) * 4096] = res.results[c]["ys"]
    return (yp.astype(np.float32), ysf)
```
